# Optimizing a Trainium2 kernel written in Bass

```python
import jax, jax.numpy as jnp
from jax import lax
import numpy as np

D_MODEL = 1024
BATCH = 4
SEQ = 4096
DEPTH = 2

N_HEADS = 4
HEAD_DIM = 64
MIX_W = N_HEADS * HEAD_DIM
N_MIXERS = 4
MIX_WIDTH = N_MIXERS * MIX_W
ROPE_THETA = 500000.0
ROPE_DIM = HEAD_DIM // 4
Q_BLOCK = 128
EPS = 1e-6
NEG = -1e30
BIG = 1e9
DIL_CONFIGS = ((128, 1), (512, 4), (2048, 16))
CMP_LEN = 32
CMP_STRIDE = 16
CMP_HID = 256
SEL_LEN = 64
SEL_TOPN = 16
WIN_LEN = 512
MOBA_BLOCK = 256
MOBA_TOPK = 3

IN_SPLITS = (
    MIX_W, MIX_W, MIX_W, MIX_W,
    MIX_W, MIX_W, MIX_W, MIX_W,
    MIX_W, HEAD_DIM, HEAD_DIM, HEAD_DIM, HEAD_DIM, HEAD_DIM, HEAD_DIM,
    3 * N_HEADS, MIX_W,
    MIX_W, MIX_W, MIX_W, MIX_W,
)
N_IN = sum(IN_SPLITS)

kernel_name = 'hybrid_parallel_sparse_mixers'


def rms_norm(x, g):
    xf = x.astype(jnp.float32)
    y = xf * lax.rsqrt(jnp.mean(xf * xf, axis=-1, keepdims=True) + EPS)
    return (y * g.astype(jnp.float32)).astype(x.dtype)


def rope_tables(pos):
    inv_freq = 1.0 / (ROPE_THETA ** (np.arange(0, ROPE_DIM, 2, dtype=np.float32) / ROPE_DIM))
    ang = jnp.asarray(pos, jnp.float32)[:, None] * jnp.asarray(inv_freq, jnp.float32)[None, :]
    return jnp.cos(ang), jnp.sin(ang)


def apply_rope(t, cos, sin):
    half = ROPE_DIM // 2
    c = cos.astype(t.dtype)
    s = sin.astype(t.dtype)
    t1 = t[..., :half]
    t2 = t[..., half:ROPE_DIM]
    return jnp.concatenate([t1 * c - t2 * s, t2 * c + t1 * s, t[..., ROPE_DIM:]], axis=-1)


def to_heads(t):
    B, S, _ = t.shape
    return t.reshape(B, S, N_HEADS, HEAD_DIM).transpose(0, 2, 1, 3)


def from_heads(t):
    B, H, S, dh = t.shape
    return t.transpose(0, 2, 1, 3).reshape(B, S, H * dh)


def sweep_query_blocks(block_fn, n_blocks):
    out = lax.map(block_fn, jnp.arange(n_blocks))
    nb, B, H, qb, dh = out.shape
    return out.transpose(1, 2, 0, 3, 4).reshape(B, H, nb * qb, dh)


def query_block(q, i):
    s0 = i * Q_BLOCK
    return s0, s0 + jnp.arange(Q_BLOCK), lax.dynamic_slice_in_dim(q, s0, Q_BLOCK, axis=2)


def stick_breaking_attention(q, k, v):
    S = q.shape[2]
    scale = HEAD_DIM ** -0.5
    kpos = jnp.arange(S)

    def block(i):
        s0, qpos, qb = query_block(q, i)
        z = jnp.einsum('bhqd,bhkd->bhqk', qb, k).astype(jnp.float32) * scale
        past = kpos[None, :] < qpos[:, None]
        log_rest = jnp.where(past, jax.nn.log_sigmoid(-z), 0.0)
        between = lax.cumsum(log_rest, axis=3, reverse=True) - log_rest
        a = jnp.where(past, jnp.exp(jax.nn.log_sigmoid(z) + between), 0.0)
        return jnp.einsum('bhqk,bhkd->bhqd', a.astype(v.dtype), v)

    return sweep_query_blocks(block, S // Q_BLOCK)


def dilated_window_attention(q, k, v):
    B, H, S, dh = q.shape
    scale = HEAD_DIM ** -0.5

    def block(i):
        s0, qpos, qb = query_block(q, i)
        outs, lses = [], []
        for window, dil in DIL_CONFIGS:
            n_keys = window // dil + 1
            idx = qpos[:, None] - dil * jnp.arange(n_keys)[None, :]
            valid = idx >= 0
            flat = jnp.maximum(idx, 0).reshape(-1)
            kg = jnp.take(k, flat, axis=2).reshape(B, H, Q_BLOCK, n_keys, dh)
            vg = jnp.take(v, flat, axis=2).reshape(B, H, Q_BLOCK, n_keys, dh)
            s = jnp.einsum('bhqd,bhqnd->bhqn', qb, kg).astype(jnp.float32) * scale
            s = jnp.where(valid, s, NEG)
            lse = jax.nn.logsumexp(s, axis=-1, keepdims=True)
            p = jnp.exp(s - lse)
            outs.append(jnp.einsum('bhqn,bhqnd->bhqd', p.astype(v.dtype), vg))
            lses.append(lse)
        w = jax.nn.softmax(jnp.concatenate(lses, axis=-1), axis=-1)
        o = jnp.stack(outs, axis=-1)
        return jnp.einsum('bhqdc,bhqc->bhqd', o, w.astype(o.dtype))

    return sweep_query_blocks(block, S // Q_BLOCK)


def compress_tokens(t, pos_emb, w1, b1, w2):
    B, S, dh = t.shape
    n_cmp = (S - CMP_LEN) // CMP_STRIDE + 1
    idx = np.arange(n_cmp)[:, None] * CMP_STRIDE + np.arange(CMP_LEN)[None, :]
    blocks = t[:, idx] + pos_emb
    hid = jax.nn.gelu(blocks.reshape(B, n_cmp, CMP_LEN * dh) @ w1 + b1)
    return hid @ w2


def nsa_attention(q, kc, vc, ks, vs, kw, vw, gates):
    B, H, S, dh = q.shape
    scale = HEAD_DIM ** -0.5
    n_cmp = kc.shape[1]
    n_sel = S // SEL_LEN
    n_top = min(SEL_TOPN, n_sel)
    c_start = np.arange(n_cmp) * CMP_STRIDE
    c_end = jnp.asarray(c_start + CMP_LEN - 1)
    s_start = np.arange(n_sel) * SEL_LEN
    overlap = np.clip(np.minimum(c_start[:, None] + CMP_LEN, s_start[None, :] + SEL_LEN)
                      - np.maximum(c_start[:, None], s_start[None, :]), 0, None) / CMP_LEN
    overlap = jnp.asarray(overlap, jnp.float32)
    ks_blk = ks.reshape(B, n_sel, SEL_LEN, dh)
    vs_blk = vs.reshape(B, n_sel, SEL_LEN, dh)
    kw_pad = jnp.pad(kw, ((0, 0), (WIN_LEN, 0), (0, 0)))
    vw_pad = jnp.pad(vw, ((0, 0), (WIN_LEN, 0), (0, 0)))
    blk_ids = jnp.arange(n_sel)
    gather_blocks = jax.vmap(lambda tb, idx: tb[idx])
    n_sel_keys = n_top * SEL_LEN

    def block(i):
        s0, qpos, qb = query_block(q, i)
        sc = jnp.einsum('bhqd,bnd->bhqn', qb, kc).astype(jnp.float32) * scale
        c_valid = c_end[None, :] <= qpos[:, None]
        pc = jax.nn.softmax(jnp.where(c_valid, sc, NEG), axis=-1) * c_valid
        o_cmp = jnp.einsum('bhqn,bnd->bhqd', pc.astype(vc.dtype), vc)
        imp = jnp.einsum('bhqn,ns->bqs', pc, overlap)
        own = qpos // SEL_LEN
        s_valid = blk_ids[None, :] <= own[:, None]
        forced = (blk_ids[None, :] == 0) | (blk_ids[None, :] >= own[:, None] - 1)
        imp = jnp.where(s_valid, jnp.where(forced, BIG, imp), NEG)
        _, sel = lax.top_k(imp, n_top)
        kg = gather_blocks(ks_blk, sel).reshape(B, Q_BLOCK, n_sel_keys, dh)
        vg = gather_blocks(vs_blk, sel).reshape(B, Q_BLOCK, n_sel_keys, dh)
        key_pos = (sel[..., None] * SEL_LEN + jnp.arange(SEL_LEN)).reshape(B, Q_BLOCK, n_sel_keys)
        sel_valid = key_pos <= qpos[None, :, None]
        ss = jnp.einsum('bhqd,bqmd->bhqm', qb, kg).astype(jnp.float32) * scale
        ps = jax.nn.softmax(jnp.where(sel_valid[:, None], ss, NEG), axis=-1)
        o_sel = jnp.einsum('bhqm,bqmd->bhqd', ps.astype(vg.dtype), vg)
        kwb = lax.dynamic_slice_in_dim(kw_pad, s0, Q_BLOCK + WIN_LEN, axis=1)
        vwb = lax.dynamic_slice_in_dim(vw_pad, s0, Q_BLOCK + WIN_LEN, axis=1)
        kpos = s0 - WIN_LEN + jnp.arange(Q_BLOCK + WIN_LEN)
        dist = qpos[:, None] - kpos[None, :]
        w_valid = (dist >= 0) & (dist < WIN_LEN) & (kpos[None, :] >= 0)
        sw = jnp.einsum('bhqd,bkd->bhqk', qb, kwb).astype(jnp.float32) * scale
        pw = jax.nn.softmax(jnp.where(w_valid, sw, NEG), axis=-1)
        o_win = jnp.einsum('bhqk,bkd->bhqd', pw.astype(vwb.dtype), vwb)
        g = lax.dynamic_slice_in_dim(gates, s0, Q_BLOCK, axis=2)
        return g[..., 0:1] * o_cmp + g[..., 1:2] * o_sel + g[..., 2:3] * o_win

    return sweep_query_blocks(block, S // Q_BLOCK)


def moba_attention(q, k, v):
    B, H, S, dh = q.shape
    scale = HEAD_DIM ** -0.5
    n_blk = -(-S // MOBA_BLOCK)
    pad = n_blk * MOBA_BLOCK - S
    kp = jnp.pad(k, ((0, 0), (0, 0), (0, pad), (0, 0)))
    vp = jnp.pad(v, ((0, 0), (0, 0), (0, pad), (0, 0)))
    k_blk = kp.reshape(B, H, n_blk, MOBA_BLOCK, dh)
    v_blk = vp.reshape(B, H, n_blk, MOBA_BLOCK, dh)
    k_mean = jnp.mean(k_blk.astype(jnp.float32), axis=3)
    n_top = min(MOBA_TOPK, max(n_blk - 1, 1))
    n_sel_keys = n_top * MOBA_BLOCK
    blk_ids = jnp.arange(n_blk)
    gather_blocks = jax.vmap(jax.vmap(lambda tb, idx: tb[idx]))

    def block(i):
        s0, qpos, qb = query_block(q, i)
        own = s0 // MOBA_BLOCK
        gsc = jnp.einsum('bhqd,bhnd->bhqn', qb.astype(jnp.float32), k_mean)
        gsc = jnp.where(blk_ids[None, :] < own, gsc, NEG)
        _, sel = lax.top_k(gsc, n_top)
        sel_valid = jnp.repeat(sel < own, MOBA_BLOCK, axis=-1)
        kg = gather_blocks(k_blk, sel).reshape(B, H, Q_BLOCK, n_sel_keys, dh)
        vg = gather_blocks(v_blk, sel).reshape(B, H, Q_BLOCK, n_sel_keys, dh)
        ss = jnp.einsum('bhqd,bhqmd->bhqm', qb, kg).astype(jnp.float32) * scale
        ss = jnp.where(sel_valid, ss, NEG)
        own_start = own * MOBA_BLOCK
        ko = lax.dynamic_slice_in_dim(kp, own_start, MOBA_BLOCK, axis=2)
        vo = lax.dynamic_slice_in_dim(vp, own_start, MOBA_BLOCK, axis=2)
        opos = own_start + jnp.arange(MOBA_BLOCK)
        so = jnp.einsum('bhqd,bhkd->bhqk', qb, ko).astype(jnp.float32) * scale
        so = jnp.where(opos[None, :] <= qpos[:, None], so, NEG)
        p = jax.nn.softmax(jnp.concatenate([ss, so], axis=-1), axis=-1)
        o_sel = jnp.einsum('bhqm,bhqmd->bhqd', p[..., :n_sel_keys].astype(vg.dtype), vg)
        o_own = jnp.einsum('bhqk,bhkd->bhqd', p[..., n_sel_keys:].astype(vo.dtype), vo)
        return o_sel + o_own

    return sweep_query_blocks(block, S // Q_BLOCK)


def hybrid_layer(x, c, norm_pre, norm_post, w_mod, b_mod, w_in, w_out,
                 cmp_pos, cmp_w1, cmp_b1, cmp_w2, cos, sin, cos_c, sin_c):
    B, S, _ = x.shape
    mod = jax.nn.silu(c) @ w_mod + b_mod
    shift, scale, gate = jnp.split(mod[:, None, :], 3, axis=-1)
    h = rms_norm(x, norm_pre) * (1 + scale) + shift
    proj = h @ w_in
    (qa, ka, va, za, qb, kb, vb, zb, qc, kcr, vcr, ksr, vsr, kwr, vwr, gc, zc,
     qd, kd, vd, zd) = jnp.split(proj, np.cumsum(IN_SPLITS)[:-1].tolist(), axis=-1)
    oa = stick_breaking_attention(to_heads(qa), to_heads(ka), to_heads(va))
    ob = dilated_window_attention(apply_rope(to_heads(qb), cos, sin),
                                  apply_rope(to_heads(kb), cos, sin), to_heads(vb))
    kc = apply_rope(compress_tokens(kcr, cmp_pos[0], cmp_w1[0], cmp_b1[0], cmp_w2[0]), cos_c, sin_c)
    vc = compress_tokens(vcr, cmp_pos[1], cmp_w1[1], cmp_b1[1], cmp_w2[1])
    gates = jax.nn.sigmoid(gc.astype(jnp.float32)).reshape(B, S, N_HEADS, 3)
    gates = gates.transpose(0, 2, 1, 3).astype(x.dtype)
    oc = nsa_attention(apply_rope(to_heads(qc), cos, sin), kc, vc,
                       apply_rope(ksr, cos, sin), vsr, apply_rope(kwr, cos, sin), vwr, gates)
    od = moba_attention(apply_rope(to_heads(qd), cos, sin),
                        apply_rope(to_heads(kd), cos, sin), to_heads(vd))
    mixed = jnp.concatenate([from_heads(oa) * jax.nn.silu(za), from_heads(ob) * jax.nn.silu(zb),
                             from_heads(oc) * jax.nn.silu(zc), from_heads(od) * jax.nn.silu(zd)],
                            axis=-1)
    y = rms_norm(mixed @ w_out, norm_post)
    return x + gate * y


def setup_inputs(seed: int = 0) -> dict:
    key = jax.random.key(seed)
    ks = jax.random.split(key, 12)
    f = jnp.float32
    nrm = jax.random.normal
    x = nrm(ks[0], (BATCH, SEQ, D_MODEL), f)
    c = nrm(ks[1], (BATCH, D_MODEL), f)
    norm_pre = 1.0 + 0.05 * nrm(ks[2], (DEPTH, D_MODEL), f)
    norm_post = 1.0 + 0.05 * nrm(ks[3], (DEPTH, D_MODEL), f)
    w_mod = nrm(ks[4], (DEPTH, D_MODEL, 3 * D_MODEL), f) * (0.5 * D_MODEL ** -0.5)
    b_mod = 0.01 * nrm(ks[5], (DEPTH, 3 * D_MODEL), f)
    w_in = nrm(ks[6], (DEPTH, D_MODEL, N_IN), f) * D_MODEL ** -0.5
    w_out = nrm(ks[7], (DEPTH, MIX_WIDTH, D_MODEL), f) * MIX_WIDTH ** -0.5
    cmp_pos = 0.1 * nrm(ks[8], (DEPTH, 2, CMP_LEN, HEAD_DIM), f)
    cmp_w1 = nrm(ks[9], (DEPTH, 2, CMP_LEN * HEAD_DIM, CMP_HID), f) * (CMP_LEN * HEAD_DIM) ** -0.5
    cmp_b1 = 0.01 * nrm(ks[10], (DEPTH, 2, CMP_HID), f)
    cmp_w2 = nrm(ks[11], (DEPTH, 2, CMP_HID, HEAD_DIM), f) * CMP_HID ** -0.5
    return {'x': x, 'c': c, 'norm_pre': norm_pre, 'norm_post': norm_post,
            'w_mod': w_mod, 'b_mod': b_mod, 'w_in': w_in, 'w_out': w_out,
            'cmp_pos': cmp_pos, 'cmp_w1': cmp_w1, 'cmp_b1': cmp_b1, 'cmp_w2': cmp_w2}


def reference(x, c, norm_pre, norm_post, w_mod, b_mod, w_in, w_out, cmp_pos, cmp_w1, cmp_b1, cmp_w2):
    S = x.shape[1]
    cos, sin = rope_tables(np.arange(S))
    n_cmp = (S - CMP_LEN) // CMP_STRIDE + 1
    cos_c, sin_c = rope_tables(np.arange(n_cmp) * CMP_STRIDE + CMP_LEN - 1)
    for l in range(DEPTH):
        x = hybrid_layer(x, c, norm_pre[l], norm_post[l], w_mod[l], b_mod[l], w_in[l], w_out[l],
                         cmp_pos[l], cmp_w1[l], cmp_b1[l], cmp_w2[l], cos, sin, cos_c, sin_c)
    return x
```

```python
import numpy as np
from contextlib import ExitStack
import concourse.bass as bass
import concourse.mybir as mybir
from concourse.bass_utils import run_bass_kernel_spmd
import ml_dtypes

F32 = mybir.dt.float32
BF16 = mybir.dt.bfloat16
AF = mybir.ActivationFunctionType
ALU = mybir.AluOpType
AX = mybir.AxisListType

S_LEN = 4096
D = 1024
NSLOT = 32
L = 2
EPS = 1e-6
SCALE = 0.125
BIG = 1e9
NEGB = -1e30


class Sched:
    EPOCH = 20000

    def __init__(self, nc, es, same_engine_sync=True):
        self.nc = nc
        self.es = es
        self.eng = dict(pe=nc.tensor, act=nc.scalar, dve=nc.vector, pool=nc.gpsimd, sp=nc.sync)
        self.cur = {}
        self.nsem = 0
        self.bufs = {}
        self.seen = {e: {} for e in self.eng}
        self.streams = {}
        self.same = same_engine_sync
        self.nwait = 0
        self.nins = 0
        self.last_tok = {}

    def _newsem(self, name):
        self.nsem += 1
        return self.es.enter_context(self.nc.semaphore(f"s{self.nsem}_{name}"))

    def _tick(self, e):
        c = self.cur.get(e)
        if c is None or c[1] >= self.EPOCH:
            c = [self._newsem(e), 0]
            self.cur[e] = c
        c[1] += 1
        return ('c', c[0], c[1], e)

    def _wait(self, e, tok):
        if tok[0] == 'c':
            _, sem, val, src = tok
            if src == e and (e == 'pe' or not self.same):
                return
        else:
            _, st = tok
            sem = st[0]
            val = 16 * st[1]
        k = sem.name
        if self.seen[e].get(k, 0) >= val:
            return
        self.eng[e].wait_ge(sem, val)
        self.nwait += 1
        self.seen[e][k] = val

    def _deps(self, reads, writes):
        deps = []
        for r in reads:
            b = self.bufs.get(r)
            if b and b['w'] is not None:
                deps.append(b['w'])
        for w in writes:
            b = self.bufs.get(w)
            if b:
                if b['w'] is not None:
                    deps.append(b['w'])
                deps.extend(b['r'].values())
        return deps

    def _record(self, e, tok, reads, writes):
        for r in reads:
            self.bufs.setdefault(r, dict(w=None, r={}))['r'][e] = tok
        for w in writes:
            self.bufs[w] = dict(w=tok, r={})
        self.last_tok[e] = tok

    @staticmethod
    def _is_psum(k):
        return isinstance(k, str) and k[:2] in ('pR', 'pA', 'pT')

    def op(self, e, fn, reads=(), writes=()):
        psr = [r for r in reads if self._is_psum(r)]
        if psr:
            reads = [r for r in reads if not self._is_psum(r)]
            writes = list(writes) + [r for r in psr if r not in writes]
        for t in self._deps(reads, writes):
            self._wait(e, t)
        ins = fn(self.eng[e])
        tok = self._tick(e)
        ins.then_inc(tok[1], 1)
        self.nins += 1
        self._record(e, tok, reads, writes)
        return tok

    def dma(self, e, out, in_, stream, reads=(), writes=(), **kw):
        for t in self._deps(reads, writes):
            self._wait(e, t)
        st = self.streams.get(stream)
        if st is None:
            st = [self._newsem('d' + stream), 0]
            self.streams[stream] = st
        elif st[1] > 0:
            self._wait(e, ('d', st))
        ins = self.eng[e].dma_start(out=out, in_=in_, **kw)
        ins.then_inc(st[0], 16)
        st[1] += 1
        tok = ('d', st)
        self.nins += 1
        self._record('dma:' + stream, tok, reads, writes)
        return tok

    def barrier(self, drop_prefixes=()):
        toks = [t for k, t in self.last_tok.items()]
        for e in self.eng:
            for t in toks:
                self._wait(e, t)
        if drop_prefixes:
            for k in list(self.bufs.keys()):
                ks = k if isinstance(k, str) else k[0]
                if any(ks.startswith(p) for p in drop_prefixes):
                    del self.bufs[k]

    def finish(self):
        for stream, st in self.streams.items():
            self._wait('sp', ('d', st))
        for e, t in list(self.last_tok.items()):
            if not e.startswith('dma:'):
                self._wait('sp', t)


class Rot:
    def __init__(self, items):
        self.items = items
        self.i = 0

    def __call__(self):
        it = self.items[self.i % len(self.items)]
        self.i += 1
        return it


def _consts():
    bf = ml_dtypes.bfloat16
    c = {}
    k = np.arange(128)[:, None]
    q = np.arange(128)[None, :]
    c['ident'] = np.eye(128, dtype=np.float32).astype(bf)
    c['triS'] = (k < q).astype(np.float32).astype(bf)
    c['triI'] = (k <= q).astype(np.float32).astype(bf)
    c['w4'] = (q < k).astype(np.float32).astype(bf)
    c['umat'] = (k >= q).astype(np.float32).astype(bf)
    c['ones'] = np.ones((128, 128), np.float32).astype(bf)
    c['ntri4'] = np.tile(-240.0 * (1.0 - (k <= q).astype(np.float32)), (1, 4)).astype(bf)
    c['nw44'] = np.tile(-240.0 * (1.0 - (q < k).astype(np.float32)), (1, 4)).astype(bf)
    mb = np.zeros((128, 17, 128), np.float32)
    for d in range(17):
        j = 128 * d + q - k
        m = ((j >= 0) & (j <= 128)).astype(np.float32)
        m += ((j >= 0) & (j <= 512) & (j % 4 == 0)).astype(np.float32)
        m += ((j >= 0) & (j <= 2048) & (j % 16 == 0)).astype(np.float32)
        mb[:, d, :] = m
    with np.errstate(divide='ignore'):
        lb = np.where(mb > 0, np.log(np.maximum(mb, 1.0)) / SCALE, -240.0).astype(np.float32)
    hi = lb.astype(bf)
    c['lmbh'] = hi
    c['lmbl'] = (lb[:, 0:5, :] - hi[:, 0:5, :].astype(np.float32)).astype(bf)
    inv_freq = (1.0 / (500000.0 ** (np.arange(0, 16, 2, dtype=np.float32) / 16))).astype(np.float32)

    def tabs(pos):
        ang = pos.astype(np.float32)[:, None] * inv_freq[None, :]
        cs, sn = np.cos(ang).astype(np.float32), np.sin(ang).astype(np.float32)
        return np.concatenate([cs, cs], 1), np.concatenate([-sn, sn], 1)
    cc, ss = tabs(np.arange(S_LEN))
    c['rcc'] = np.ascontiguousarray(cc.reshape(32, 128, 16).transpose(1, 0, 2))
    c['rss'] = np.ascontiguousarray(ss.reshape(32, 128, 16).transpose(1, 0, 2))
    ccc, ssc = tabs(np.arange(256) * 16 + 31)
    c['rccc'] = np.ascontiguousarray(ccc.reshape(2, 128, 16).transpose(1, 0, 2))
    c['rssc'] = np.ascontiguousarray(ssc.reshape(2, 128, 16).transpose(1, 0, 2))
    cv = np.zeros((128, 17, 128), np.float32)
    for u in range(17):
        cv[:, u, :] = (16 * k + 31 - q <= 128 * u)
    c['cv'] = cv.astype(bf)
    n_cmp, n_sel = 255, 64
    c_start = np.arange(n_cmp) * 16
    s_start = np.arange(n_sel) * 64
    ov = np.clip(np.minimum(c_start[:, None] + 32, s_start[None, :] + 64)
                 - np.maximum(c_start[:, None], s_start[None, :]), 0, None) / 32
    ovp = np.zeros((256, 64), np.float32)
    ovp[:255] = ov
    c['ovl'] = np.ascontiguousarray(ovp.reshape(2, 128, 64).transpose(1, 0, 2)).astype(bf)
    qq = np.arange(128)[:, None]
    cidx = np.arange(128)[None, :]
    hi = qq // 64
    V = (cidx - 64 <= hi)
    Fm = (cidx - 64 >= hi - 1)
    c['t1'] = (V & ~Fm).astype(np.float32)
    c['t2'] = (BIG * (V & Fm) + NEGB * (~V)).astype(np.float32)
    c['vt'] = V.astype(np.float32)
    return c


GROUPS = None


def _w_groups(w_in_l):
    o = {}
    names = ['qa', 'ka', 'va', 'za', 'qb', 'kb', 'vb', 'zb', 'qc', 'kcr', 'vcr', 'ksr', 'vsr', 'kwr', 'vwr',
             'gc', 'zc', 'qd', 'kd', 'vd', 'zd']
    sizes = [256] * 8 + [256, 64, 64, 64, 64, 64, 64, 12, 256] + [256] * 4
    off = 0
    for n, s in zip(names, sizes):
        o[n] = w_in_l[:, off:off + s]
        off += s
    assert off == 3980
    pad = np.zeros((1024, 512 - 396), np.float32)
    g = [np.concatenate([o['ka'], o['va']], 1), np.concatenate([o['qa'], o['za']], 1),
         np.concatenate([o['kb'], o['vb']], 1), np.concatenate([o['qb'], o['zb']], 1),
         np.concatenate([o['kcr'], o['vcr'], o['ksr'], o['kwr'], o['vsr'], o['vwr'], o['gc'], pad], 1),
         np.concatenate([o['qc'], o['zc']], 1),
         np.concatenate([o['kd'], o['vd']], 1), np.concatenate([o['qd'], o['zd']], 1)]
    return np.stack(g, 0)


def build(n_layers=2, mixers='ABCD', dbg=False, nq_groups=8):
    nc = bass.Bass("TRN2", target_bir_lowering=False)
    C = _consts()

    def din(name, shape, dt=F32):
        return nc.dram_tensor(name, list(shape), dt, kind="ExternalInput").ap()
    x_d = din("x", [S_LEN, D])
    cT_d = din("cT", [128, 8])
    npre_d = din("npre", [L, 128, D])
    npost_d = din("npost", [L, 128, D])
    bmod_d = din("bmod", [L, 128, 3 * D])
    wmod_d = din("wmod", [L, D, 3 * D])
    wg_d = din("wg", [L, 8, D, 512])
    wout_d = din("wout", [L, D, D])
    w1_d = din("w1", [L, 2, 2048, 256])
    w2_d = din("w2", [L, 2, 256, 64])
    b1_d = din("b1", [L, 128, 4])
    posT_d = din("posT", [L, 64, 2, 32])
    cd = {}
    for k_, v_ in C.items():
        cd[k_] = din("c_" + k_, v_.shape, BF16 if v_.dtype == ml_dtypes.bfloat16 else F32)
    out_d = nc.dram_tensor("out", [S_LEN, D], F32, kind="ExternalOutput").ap()
    x1_d = nc.dram_tensor("x1s", [S_LEN, D], F32, kind="Internal").ap()
    mixed_d = nc.dram_tensor("mixed", [S_LEN, D], BF16, kind="ExternalOutput" if dbg else "Internal").ap()

    with ExitStack() as es:
        S = Sched(nc, es)

        tcount = [0]

        def T(name, shape, dt, st=es):
            tcount[0] += 1
            return st.enter_context(nc.sbuf_tensor(f"{name}_u{tcount[0]}", list(shape), dt))

        def P(name, shape, dt):
            return es.enter_context(nc.psum_tensor(name, list(shape), dt))

        hT = T("hT", [128, 8, S_LEN], BF16)
        Gt = T("Gt", [128, D], F32)
        sht = T("sht", [128, D], F32)
        gpt = T("gpt", [128, D], F32)
        ct = {}
        for k_, v_ in C.items():
            ct[k_] = T("k_" + k_, v_.shape, BF16 if v_.dtype == ml_dtypes.bfloat16 else F32)
            S.dma('sp', ct[k_][:], cd[k_], 'const', writes=['c_' + k_])
        ident = ct['ident']
        wbf = [T(f"wbf{i}", [128, 8, 512], BF16) for i in range(2)]
        wst = [T(f"wst{i}", [128, 4, 512], F32) for i in range(2)]
        wbf_rot = Rot([0, 1])
        wst_rot = Rot([0, 1])
        wf = [T(f"wf{i}", [128, 512], F32) for i in range(3)]
        wb = [T(f"wb{i}", [128, 512], BF16) for i in range(6)]
        wf_rot = Rot(list(range(3)))
        wb_rot = Rot(list(range(6)))
        qbp = [T(f"qbp{i}", [128, 256], BF16) for i in range(2)]
        qb_rot = Rot([0, 1])
        sm2 = [T(f"sz{i}", [128, 256], F32) for i in range(1)]
        sm2_rot = Rot([0])

        def SM2():
            i = sm2_rot()
            return sm2[i], f"sz{i}"
        sm = [T(f"sm{i}", [128, 64], F32) for i in range(8)]
        sm_rot = Rot(list(range(8)))
        pT = [P(f"pT{i}", [128, 8, 128], BF16) for i in range(2)]
        pR = [P(f"pR{i}", [128, 512], F32) for i in range(4)]
        pA = [P(f"pA{i}", [128, 512], F32) for i in range(2)]
        pT_rot = Rot([0, 1])
        pR_rot = Rot([0, 1, 2, 3])

        def WF():
            i = wf_rot()
            return wf[i], f"wf{i}"

        def WB():
            i = wb_rot()
            return wb[i], f"wb{i}"

        def SM():
            i = sm_rot()
            return sm[i], f"sm{i}"

        def PR():
            i = pR_rot()
            return pR[i], f"pR{i}"

        def PT():
            i = pT_rot()
            return pT[i], f"pT{i}"

        def load_wgroup(l, g):
            bi = wbf_rot()
            wt, wk = wbf[bi], f"wbf{bi}"
            src = wg_d[l, g].rearrange("(k p) c -> p k c", p=128)
            for half in range(2):
                si = wst_rot()
                S.dma('sp', wst[si][:], src[:, half * 4:(half + 1) * 4, :], f"wst{si}", writes=[f"wst{si}"])
                S.op('pool', lambda e, si=si, half=half: e.tensor_copy(wt[:, half * 4:(half + 1) * 4, :], wst[si][:]),
                     reads=[f"wst{si}"], writes=[(wk, half)])
            return wt, wk

        def proj_slot(slot, wt, wk, ncols=512):
            ps, pk = PR()

            def f(e):
                for k in range(8):
                    ins = e.matmul(ps[:, 0:ncols], hT[:, k, slot * 128:(slot + 1) * 128], wt[:, k, 0:ncols],
                                   start=(k == 0), stop=(k == 7))
                return ins
            S.op('pe', f, reads=[('hT', slot), (wk, 0), (wk, 1)], writes=[pk])
            return ps, pk

        def rope(src, srckey, dst, dstkey, nh, cc_ap, ss_ap, tabkeys):
            S.op('act', lambda e: e.copy(dst[:, :, 16:64], src[:, :, 16:64]), reads=[srckey], writes=[dstkey])
            ta, tak = SM()
            tb, tbk = SM()
            tav = ta[:, 0:nh * 16].rearrange("p (h c) -> p h c", c=16)
            tbv = tb[:, 0:nh * 16].rearrange("p (h c) -> p h c", c=16)
            S.op('dve', lambda e: e.tensor_tensor(tav, src[:, :, 0:16], cc_ap.unsqueeze(1).to_broadcast([128, nh, 16]), ALU.mult),
                 reads=[srckey] + tabkeys, writes=[tak])
            S.op('dve', lambda e: e.tensor_tensor(tbv[:, :, 0:8], src[:, :, 8:16], ss_ap[:, 0:8].unsqueeze(1).to_broadcast([128, nh, 8]), ALU.mult),
                 reads=[srckey] + tabkeys, writes=[tbk])
            S.op('dve', lambda e: e.tensor_tensor(tbv[:, :, 8:16], src[:, :, 0:8], ss_ap[:, 8:16].unsqueeze(1).to_broadcast([128, nh, 8]), ALU.mult),
                 reads=[srckey] + tabkeys, writes=[tbk])
            S.op('dve', lambda e: e.tensor_tensor(dst[:, :, 0:16], tav, tbv, ALU.add),
                 reads=[tak, tbk], writes=[dstkey])

        def run_pipeline(its, nst):
            n = len(its)
            for step in range(n + nst - 1):
                for k in range(nst):
                    t = step - k
                    if 0 <= t < n and its[t][k] is not None:
                        its[t][k]()

        for l in range(n_layers):
            xin = x_d if l == 0 else x1_d
            xin_key = 'xin' if l == 0 else 'x1'
            xout = out_d if l == n_layers - 1 else x1_d
            xout_key = 'out' if l == n_layers - 1 else 'x1'

            with ExitStack() as ps0:
                cTt = T("cTt", [128, 8], F32, ps0)
                sc = T("sc", [128, 8], F32, ps0)
                crep = T("crep", [128, 8, 128], F32, ps0)
                wm = [T(f"wm{i}", [128, 8, 512], F32, ps0) for i in range(1)]
                modb = T("modb", [128, 3 * D], F32, ps0)
                bmt = T("bmt", [128, 3 * D], F32, ps0)
                npre_t = T("npre_t", [128, D], F32, ps0)
                npost_t = T("npost_t", [128, D], F32, ps0)
                S.dma('sp', cTt[:], cT_d, 'p0a', writes=['cTt'])
                S.dma('sp', bmt[:], bmod_d[l], 'p0b', writes=['bmt'])
                S.dma('sp', npre_t[:], npre_d[l], 'p0c', writes=['npre_t'])
                S.dma('sp', npost_t[:], npost_d[l], 'p0d', writes=['npost_t'])
                S.op('act', lambda e: e.activation(sc[:], cTt[:], AF.Silu), reads=['cTt'], writes=['sc'])
                S.op('dve', lambda e: e.tensor_copy(crep[:], sc[:].unsqueeze(2).to_broadcast([128, 8, 128])), reads=['sc'], writes=['crep'])
                for cg in range(6):
                    wmt, wmk = wm[0], "wm0"
                    S.dma('sp', wmt[:], wmod_d[l][:, cg * 512:(cg + 1) * 512].rearrange("(k p) c -> p k c", p=128), wmk, writes=[wmk])
                    ps, pk = PR()

                    def f(e, ps=ps, wmt=wmt):
                        for k in range(8):
                            ins = e.matmul(ps[:], crep[:, k, :], wmt[:, k, :], start=(k == 0), stop=(k == 7))
                        return ins
                    S.op('pe', f, reads=['crep', wmk], writes=[pk])
                    S.op('dve', lambda e, ps=ps, cg=cg: e.tensor_tensor(modb[:, cg * 512:(cg + 1) * 512], ps[:], bmt[:, cg * 512:(cg + 1) * 512], ALU.add),
                         reads=[pk, 'bmt'], writes=[('modb', cg)])
                S.op('act', lambda e: e.copy(sht[:], modb[:, 0:D]), reads=[('modb', 0), ('modb', 1)], writes=['sht'])
                S.op('dve', lambda e: e.scalar_tensor_tensor(out=Gt[:], in0=modb[:, D:2 * D], scalar=1.0, in1=npre_t[:], op0=ALU.add, op1=ALU.mult),
                     reads=[('modb', 2), ('modb', 3), 'npre_t'], writes=['Gt'])
                S.op('dve', lambda e: e.tensor_tensor(gpt[:], modb[:, 2 * D:3 * D], npost_t[:], ALU.mult),
                     reads=[('modb', 4), ('modb', 5), 'npost_t'], writes=['gpt'])
                S.barrier(drop_prefixes=('cTt', 'sc', 'crep', 'wm', 'modb', 'bmt', 'npre_t', 'npost_t'))

            with ExitStack() as ps1:
                xt = [T(f"xt{i}", [128, D], F32, ps1) for i in range(2)]
                junk = T("junk", [128, D], BF16, ps1)
                ht32 = T("ht32", [128, D], F32, ps1)
                hb = [T(f"hb{i}", [128, D], BF16, ps1) for i in range(2)]
                st1 = [T(f"st1_{i}", [128, 4], F32, ps1) for i in range(2)]
                def p1_iter(slot):
                    i2 = slot % 2
                    stt, stk = st1[i2], f"st1_{i2}"
                    c = {}

                    def s0():
                        S.dma('sp', xt[i2][:], xin[slot * 128:(slot + 1) * 128, :], f"xt{i2}", reads=[(xin_key, slot)], writes=[f"xt{i2}"])

                    def s1():
                        S.op('act', lambda e: e.activation(junk[:], xt[i2][:], AF.Square, accum_out=stt[:, 0:1]),
                             reads=[f"xt{i2}"], writes=['junk', (stk, 0)])
                        S.op('dve', lambda e: e.tensor_scalar(out=stt[:, 3:4], in0=stt[:, 0:1], scalar1=1.0 / D, scalar2=EPS, op0=ALU.mult, op1=ALU.add),
                             reads=[(stk, 0)], writes=[(stk, 3)])
                        S.op('act', lambda e: e.activation(stt[:, 1:2], stt[:, 3:4], AF.Sqrt), reads=[(stk, 3)], writes=[(stk, 1)])
                        S.op('dve', lambda e: e.reciprocal(stt[:, 2:3], stt[:, 1:2]), reads=[(stk, 1)], writes=[(stk, 2)])
                        S.op('dve', lambda e: e.scalar_tensor_tensor(out=ht32[:], in0=xt[i2][:], scalar=stt[:, 2:3], in1=Gt[:], op0=ALU.mult, op1=ALU.mult),
                             reads=[f"xt{i2}", (stk, 2), 'Gt'], writes=['ht32'])
                        S.op('pool', lambda e: e.tensor_tensor(hb[i2][:], ht32[:], sht[:], ALU.add),
                             reads=['ht32', 'sht'], writes=[f"hb{i2}"])

                    def s2():
                        pt, ptk = PT()
                        c['pt'], c['ptk'] = pt, ptk

                        def f(e):
                            for k in range(8):
                                ins = e.transpose(pt[:, k, :], hb[i2][:, k * 128:(k + 1) * 128], ident[:])
                            return ins
                        S.op('pe', f, reads=[f"hb{i2}", 'c_ident'], writes=[ptk])

                    def s3():
                        pt, ptk = c['pt'], c['ptk']
                        if slot % 2:
                            S.op('act', lambda e: e.copy(hT[:, :, slot * 128:(slot + 1) * 128], pt[:]), reads=[ptk], writes=[('hT', slot)])
                        else:
                            S.op('dve', lambda e: e.tensor_copy(hT[:, :, slot * 128:(slot + 1) * 128], pt[:]), reads=[ptk], writes=[('hT', slot)])
                    return [s0, s1, s2, s3]
                run_pipeline([p1_iter(slot) for slot in range(NSLOT)], 4)
                S.barrier(drop_prefixes=('xt', 'junk', 'ht32', 'hb', 'st1_'))

            def qk_transpose(src_bf, srckeys, dst_fn, dstkey, npair):
                pt, ptk = PT()

                def f(e):
                    for p in range(npair):
                        ins = e.transpose(pt[:, p, :], src_bf[:, p * 128:(p + 1) * 128], ident[:])
                    return ins
                S.op('pe', f, reads=list(srckeys) + ['c_ident'], writes=[ptk])
                return pt, ptk

            mixrr = [0]

            def store_mixed(m, col0, o_ap_fn, okeys, zt, ztk, i):
                mo, mok = WB()
                S.op('dve', lambda e: e.tensor_tensor(mo[:, 0:256], o_ap_fn(), zt[:, i, :], ALU.mult),
                     reads=list(okeys) + [(ztk, i)], writes=[mok])
                mixrr[0] += 1
                S.dma('pool', mixed_d[m * 128:(m + 1) * 128, col0:col0 + 256], mo[:, 0:256], f'mixst{mixrr[0] % 4}',
                      reads=[mok], writes=[('mixed', m, col0)])

            def qs_iters(wq, wqk, qg, qT2, qT2k, zt, ztk, do_rope, pairs=True, qcT=None):
                def one(i):
                    slot = qg * 4 + i
                    c = {}

                    def s0():
                        c['ps'], c['pk'] = proj_slot(slot, wq, wqk)

                    def s1():
                        ps, pk = c['ps'], c['pk']
                        ez, ezk = SM2()
                        S.op('act', lambda e: e.activation(ez[:], ps[:, 256:512], AF.Exp, scale=-1.0), reads=[pk], writes=[ezk])
                        S.op('dve', lambda e: e.tensor_scalar(out=ez[:], in0=ez[:], scalar1=1.0, scalar2=None, op0=ALU.add), reads=[ezk], writes=[ezk])
                        S.op('dve', lambda e: e.reciprocal(ez[:], ez[:]), reads=[ezk], writes=[ezk])
                        S.op('dve', lambda e: e.tensor_tensor(zt[:, i, :], ps[:, 256:512], ez[:], ALU.mult), reads=[pk, ezk], writes=[(ztk, i)])
                        qi = qb_rot()
                        qb_, qbk = qbp[qi], f"qbp{qi}"
                        c['qb'], c['qbk'] = qb_, qbk
                        if do_rope:
                            rope(ps[:, 0:256].rearrange("p (h c) -> p h c", c=64), pk,
                                 qb_[:, 0:256].rearrange("p (h c) -> p h c", c=64), qbk, 4,
                                 ct['rcc'][:, slot, :], ct['rss'][:, slot, :], ['c_rcc', 'c_rss'])
                        else:
                            S.op('dve', lambda e: e.tensor_copy(qb_[:, 0:256], ps[:, 0:256]), reads=[pk], writes=[qbk])

                    def s2():
                        qb_, qbk = c['qb'], c['qbk']
                        if pairs:
                            c['pt'], c['ptk'] = qk_transpose(qb_, [qbk], None, None, 2)
                        else:
                            pt, ptk = PT()
                            c['pt'], c['ptk'] = pt, ptk

                            def f(e):
                                for h in range(4):
                                    ins = e.transpose(pt[0:64, h, :], qb_[:, h * 64:(h + 1) * 64], ident[:])
                                return ins
                            S.op('pe', f, reads=[qbk, 'c_ident'], writes=[ptk])

                    def s3():
                        pt, ptk = c['pt'], c['ptk']
                        if pairs:
                            S.op('dve', lambda e: e.tensor_copy(qT2[0:64, :, i, 0, :], pt[0:64, 0:2, :]), reads=[ptk], writes=[(qT2k, i, 0)])
                            S.op('act', lambda e: e.copy(qT2[64:128, :, i, 1, :], pt[64:128, 0:2, :]), reads=[ptk], writes=[(qT2k, i, 1)])
                        else:
                            S.op('dve', lambda e: e.tensor_copy(qcT[:, i, :, :], pt[0:64, 0:4, :]), reads=[ptk], writes=[('qcT', i)])
                    return [s0, s1, s2, s3]
                return [one(i) for i in range(4)]

            def interleave(its, extra):
                its = [it + [None] * (4 - len(it)) for it in its]
                if not extra:
                    return its
                n = len(its)
                out = []
                pos = [((j + 1) * n) // (len(extra) + 1) for j in range(len(extra))]
                j = 0
                for t, it in enumerate(its):
                    while j < len(extra) and pos[j] == t:
                        out.append(extra[j])
                        j += 1
                    out.append(it)
                out.extend(extra[j:])
                return out

            def s_pairs(kT, kTk, kb, qT2, qT2k, i):
                ps, pk = PR()

                def f(e):
                    for p in range(2):
                        ins = e.matmul(ps[:, p * 256:(p + 1) * 256], kT[:, p, kb * 128:(kb + 1) * 128],
                                       qT2[:, p, i, :, :].rearrange("p a b -> p (a b)"), start=True, stop=True)
                    return ins
                S.op('pe', f, reads=[(kTk, kb), (qT2k, i, 0), (qT2k, i, 1)], writes=[pk])
                return ps, pk

            def av(acc, acck, pb, pbk, v_fn, vkey, first, last, width):
                def f(e):
                    for h in range(4):
                        ins = e.matmul(acc[:, h * width:(h + 1) * width], pb[:, h * 128:(h + 1) * 128], v_fn(h), start=(first and h == 0), stop=last,
                                       skip_group_check=True)
                    return ins
                S.op('pe', f, reads=[pbk] + (vkey if isinstance(vkey, list) else [vkey]), writes=[acck])

            def normalize(acc, acck, width=65):
                accv = acc[:, 0:4 * width].rearrange("p (h c) -> p h c", c=width)
                rd, rdk = SM()
                S.op('dve', lambda e: e.reciprocal(rd[:, 0:4], accv[:, :, 64]), reads=[acck], writes=[rdk])
                o, ok = WF()
                S.op('dve', lambda e: e.tensor_tensor(o[:, 0:256].rearrange("p (h c) -> p h c", c=64), accv[:, :, 0:64],
                                                      rd[:, 0:4].unsqueeze(2).to_broadcast([128, 4, 64]), ALU.mult),
                     reads=[acck, rdk], writes=[ok])
                return o, ok

            if 'A' in mixers:
                with ExitStack() as ma:
                    kaT = T("kaT", [128, 2, S_LEN], BF16, ma)
                    va = T("va", [128, NSLOT, 256], BF16, ma)
                    qT2s = [T(f"qT2a{b_}", [128, 2, 4, 2, 128], BF16, ma) for b_ in range(2)]
                    zts = [T(f"zta{b_}", [128, 4, 256], BF16, ma) for b_ in range(2)]
                    for b_ in range(2):
                        S.op('pool', lambda e, b_=b_: e.memset(qT2s[b_][:].rearrange("p a b c d -> p (a b c d)"), 0.0), writes=[(f'qT2_{b_}', i, j) for i in range(4) for j in range(2)])
                    wk_, wkk = load_wgroup(l, 0)
                    wq_, wqk = load_wgroup(l, 1)
                    def a_k_iter(slot):
                        c = {}

                        def s0():
                            c['ps'], c['pk'] = proj_slot(slot, wk_, wkk)

                        def s1():
                            ps, pk = c['ps'], c['pk']
                            S.op('act', lambda e: e.copy(va[:, slot, :], ps[:, 256:512]), reads=[pk], writes=[('va', slot)])
                            c['kb'], c['kbk'] = WB()
                            S.op('dve', lambda e: e.tensor_copy(c['kb'][:, 0:256], ps[:, 0:256]), reads=[pk], writes=[c['kbk']])

                        def s2():
                            c['pt'], c['ptk'] = qk_transpose(c['kb'], [c['kbk']], None, None, 2)

                        def s3():
                            S.op('dve', lambda e: e.tensor_copy(kaT[:, :, slot * 128:(slot + 1) * 128], c['pt'][:, 0:2, :]),
                                 reads=[c['ptk']], writes=[('kaT', slot)])
                        return [s0, s1, s2, s3]
                    run_pipeline([a_k_iter(slot) for slot in range(NSLOT)], 4)
                    wa = [T(f"wa{j}", [128, 512], BF16, ma) for j in range(16)]
                    wa_rot = Rot(list(range(16)))

                    def WA():
                        j = wa_rot()
                        return wa[j], f"wa{j}"

                    def a_iter(m, i, kb, st, qT2, qT2k, zt, ztk):
                        c = {}
                        acc, acck = pA[m % 2], f"pA{m % 2}"
                        diag = (kb == m)

                        def s0():
                            c['ps'], c['pk'] = s_pairs(kaT, 'kaT', kb, qT2, qT2k, i)

                        def s1():
                            ps, pk = c['ps'], c['pk']
                            et, etk = WA()
                            c['et'], c['etk'] = et, etk
                            S.op('act', lambda e: e.activation(et[:], ps[:], AF.Exp, scale=SCALE), reads=[pk], writes=[etk])
                            spb, spbk = WA()
                            S.op('act', lambda e: e.activation(spb[:], et[:], AF.Ln, bias=1.0), reads=[etk], writes=[spbk])
                            if diag:
                                S.op('dve', lambda e: e.tensor_tensor(
                                    spb[:].rearrange("p (h c) -> p h c", c=128), spb[:].rearrange("p (h c) -> p h c", c=128),
                                    ct['triS'][:].unsqueeze(1).to_broadcast([128, 4, 128]), ALU.mult),
                                    reads=[spbk, 'c_triS'], writes=[spbk])
                            bw, bwk = PR()
                            c['bw'], c['bwk'] = bw, bwk
                            lsum, lsumk = st['lsum'], st['lsumk']

                            def fb(e):
                                ins = e.matmul(bw[:], ct['umat'][:], spb[:], start=True, stop=diag)
                                if not diag:
                                    ins = e.matmul(bw[:], ct['ones'][:], lsum[:], start=False, stop=True)
                                return ins
                            S.op('pe', fb, reads=[spbk, 'c_umat', 'c_ones'] + ([lsumk] if not diag else []), writes=[bwk])
                            if kb > 0:
                                if diag:
                                    st['lsum'], st['lsumk'] = spb, spbk
                                else:
                                    nl_, nlk = WA()
                                    S.op('pool', lambda e: e.tensor_tensor(nl_[:], lsum[:], spb[:], ALU.add), reads=[lsumk, spbk], writes=[nlk])
                                    st['lsum'], st['lsumk'] = nl_, nlk

                        def s2():
                            et, etk, bw, bwk = c['et'], c['etk'], c['bw'], c['bwk']
                            xt_, xtk = WA()
                            S.op('act', lambda e: e.activation(xt_[:], bw[:], AF.Exp, scale=-1.0), reads=[bwk], writes=[xtk])
                            ab_, abk = WA()
                            S.op('dve', lambda e: e.tensor_tensor(ab_[:], et[:], xt_[:], ALU.mult), reads=[etk, xtk], writes=[abk])
                            if diag:
                                S.op('dve', lambda e: e.tensor_tensor(
                                    ab_[:].rearrange("p (h c) -> p h c", c=128), ab_[:].rearrange("p (h c) -> p h c", c=128),
                                    ct['triS'][:].unsqueeze(1).to_broadcast([128, 4, 128]), ALU.mult),
                                    reads=[abk, 'c_triS'], writes=[abk])
                            av(acc, acck, ab_, abk, lambda h: va[:, kb, h * 64:(h + 1) * 64], ('va', kb), diag, kb == 0, 64)
                            if kb == 0:
                                store_mixed(m, 0, lambda: acc[:, 0:256], [acck], zt, ztk, i)
                        return [s0, s1, s2]
                    run_pipeline(qs_iters(wq_, wqk, 0, qT2s[0], 'qT2_0', zts[0], 'zta_0', do_rope=False), 4)
                    for qg in range(nq_groups):
                        b_ = qg % 2
                        st = {'lsum': None, 'lsumk': None}
                        its = []
                        for i in range(4):
                            m = qg * 4 + i
                            for kb in range(m, -1, -1):
                                its.append(a_iter(m, i, kb, st, qT2s[b_], f'qT2_{b_}', zts[b_], f'zta_{b_}'))
                        extra = qs_iters(wq_, wqk, qg + 1, qT2s[1 - b_], f'qT2_{1 - b_}', zts[1 - b_], f'zta_{1 - b_}', do_rope=False) if qg + 1 < nq_groups else []
                        run_pipeline(interleave(its, extra), 4)
                    S.barrier(drop_prefixes=('kaT', 'va', 'qT2', 'zta', 'wa'))

            if 'B' in mixers:
                with ExitStack() as mbs:
                    kT = T("kbT", [128, 2, S_LEN], BF16, mbs)
                    vaug = T("vbaug", [128, NSLOT, 4, 65], BF16, mbs)
                    qT2s = [T(f"qT2b{b_}", [128, 2, 4, 2, 128], BF16, mbs) for b_ in range(2)]
                    zts = [T(f"ztb{b_}", [128, 4, 256], BF16, mbs) for b_ in range(2)]
                    for b_ in range(2):
                        S.op('pool', lambda e, b_=b_: e.memset(qT2s[b_][:].rearrange("p a b c d -> p (a b c d)"), 0.0), writes=[(f'qT2_{b_}', i, j) for i in range(4) for j in range(2)])
                    S.op('pool', lambda e: e.memset(vaug[:].rearrange("p a b c -> p (a b c)"), 1.0), writes=[('vaug', s_) for s_ in range(NSLOT)])
                    wk_, wkk = load_wgroup(l, 2)
                    wq_, wqk = load_wgroup(l, 3)
                    def rk_iter(slot, kT_, kTkey, vaug_, wk__, wkk__):
                        c = {}

                        def s0():
                            c['ps'], c['pk'] = proj_slot(slot, wk__, wkk__)

                        def s1():
                            ps, pk = c['ps'], c['pk']
                            S.op('act', lambda e: e.copy(vaug_[:, slot, :, 0:64], ps[:, 256:512].rearrange("p (h c) -> p h c", c=64)),
                                 reads=[pk], writes=[('vaug', slot)])
                            c['kb'], c['kbk'] = WB()
                            rope(ps[:, 0:256].rearrange("p (h c) -> p h c", c=64), pk,
                                 c['kb'][:, 0:256].rearrange("p (h c) -> p h c", c=64), c['kbk'], 4,
                                 ct['rcc'][:, slot, :], ct['rss'][:, slot, :], ['c_rcc', 'c_rss'])

                        def s2():
                            c['pt'], c['ptk'] = qk_transpose(c['kb'], [c['kbk']], None, None, 2)

                        def s3():
                            S.op('dve', lambda e: e.tensor_copy(kT_[:, :, slot * 128:(slot + 1) * 128], c['pt'][:, 0:2, :]),
                                 reads=[c['ptk']], writes=[(kTkey, slot)])
                        return [s0, s1, s2, s3]
                    run_pipeline([rk_iter(slot, kT, 'kT', vaug, wk_, wkk) for slot in range(NSLOT)], 4)
                    def b_iter(m, i, d, dmax, qT2, qT2k, zt, ztk):
                        c = {}
                        acc, acck = pA[m % 2], f"pA{m % 2}"
                        kb = m - d

                        def s0():
                            ps, pk = PR()
                            c['ps'], c['pk'] = ps, pk

                            def f(e):
                                for p in range(2):
                                    e.matmul(ps[:, p * 256:(p + 1) * 256], kT[:, p, kb * 128:(kb + 1) * 128],
                                             qT2[:, p, i, :, :].rearrange("p a b -> p (a b)"), start=(p == 0), stop=False, skip_group_check=True)
                                for h in range(4):
                                    ins = e.matmul(ps[:, h * 128:(h + 1) * 128], ident[:], ct['lmbh'][:, d, :], start=False, stop=(d > 4 and h == 3), skip_group_check=True)
                                if d <= 4:
                                    for h in range(4):
                                        ins = e.matmul(ps[:, h * 128:(h + 1) * 128], ident[:], ct['lmbl'][:, d, :], start=False, stop=(h == 3), skip_group_check=True)
                                return ins
                            S.op('pe', f, reads=[('kT', kb), (qT2k, i, 0), (qT2k, i, 1), 'c_ident', 'c_lmbh', 'c_lmbl'], writes=[pk])

                        def s1():
                            c['pb'], c['pbk'] = WB()
                            S.op('act', lambda e: e.activation(c['pb'][:], c['ps'][:], AF.Exp, scale=SCALE), reads=[c['pk']], writes=[c['pbk']])

                        def s2():
                            av(acc, acck, c['pb'], c['pbk'], lambda h: vaug[:, kb, h, :], ('vaug', kb), d == 0, d == dmax, 65)
                            if d == dmax:
                                o, ok = normalize(acc, acck)
                                store_mixed(m, 256, lambda: o[:, 0:256], [ok], zt, ztk, i)
                        return [s0, s1, s2]
                    run_pipeline(qs_iters(wq_, wqk, 0, qT2s[0], 'qT2_0', zts[0], 'ztb_0', do_rope=True), 4)
                    for qg in range(nq_groups):
                        b_ = qg % 2
                        its = []
                        for i in range(4):
                            m = qg * 4 + i
                            dmax = min(16, m)
                            for d in range(0, dmax + 1):
                                its.append(b_iter(m, i, d, dmax, qT2s[b_], f'qT2_{b_}', zts[b_], f'ztb_{b_}'))
                        extra = qs_iters(wq_, wqk, qg + 1, qT2s[1 - b_], f'qT2_{1 - b_}', zts[1 - b_], f'ztb_{1 - b_}', do_rope=True) if qg + 1 < nq_groups else []
                        run_pipeline(interleave(its, extra), 4)
                    S.barrier(drop_prefixes=('kT', 'vaug', 'qT2', 'ztb'))

            if 'C' in mixers:
                with ExitStack() as mcs:
                    kcrT = T("kcrT", [64, S_LEN], BF16, mcs)
                    vcrT = T("vcrT", [64, S_LEN], BF16, mcs)
                    kvT = [kcrT, vcrT]
                    ksT = T("ksT", [64, S_LEN], BF16, mcs)
                    kwT = T("kwT", [64, S_LEN], BF16, mcs)
                    vsa = T("vsa", [128, NSLOT, 65], BF16, mcs)
                    vwa = T("vwa", [128, NSLOT, 65], BF16, mcs)
                    gsig = T("gsig", [128, NSLOT, 12], F32, mcs)
                    kcT = T("kcT", [64, 256], BF16, mcs)
                    vcE = T("vcE", [128, 2, 128], BF16, mcs)
                    qcT = T("qcT", [64, 4, 4, 128], BF16, mcs)
                    zt = T("ztc", [128, 4, 256], BF16, mcs)
                    w1b = T("w1b", [64, 2, 8, 256], BF16, mcs)
                    w2f = T("w2f", [128, 2, 2, 64], F32, mcs)
                    w2b = T("w2b", [128, 2, 2, 64], BF16, mcs)
                    b1t = T("b1t", [128, 4], F32, mcs)
                    posf = T("posf", [64, 2, 32], F32, mcs)
                    posb = T("posb", [64, 2, 32], BF16, mcs)
                    biasv = T("biasv", [128, 4], F32, mcs)
                    hidT = T("hidT", [128, 4, 256], BF16, mcs)
                    ocacc = T("ocacc", [128, 256], F32, mcs)
                    impt = T("impt", [128, 64], F32, mcs)
                    imps = T("imps", [128, 64], F32, mcs)
                    selm = T("selm", [128, 64], BF16, mcs)
                    mx8 = T("mx8", [128, 16], F32, mcs)
                    cf = T("cf", [128, 16], F32, mcs)
                    S.op('pool', lambda e: e.memset(vsa[:].rearrange("p a b -> p (a b)"), 1.0), writes=[('vsa', s_) for s_ in range(NSLOT)])
                    S.op('pool', lambda e: e.memset(vwa[:].rearrange("p a b -> p (a b)"), 1.0), writes=[('vwa', s_) for s_ in range(NSLOT)])
                    wk_, wkk = load_wgroup(l, 4)
                    wq_, wqk = load_wgroup(l, 5)
                    for t in range(2):
                        S.dma('sp', w2f[:, t, :, :], w2_d[l, t].rearrange("(c p) d -> p c d", p=128), f'cw{t}', writes=[('w2f', t)])
                    S.op('pool', lambda e: e.tensor_copy(w2b[:], w2f[:]), reads=[('w2f', 0), ('w2f', 1)], writes=['w2b'])
                    S.dma('sp', b1t[:], b1_d[l], 'cw2', writes=['b1t'])
                    S.dma('sp', posf[:], posT_d[l], 'cw3', writes=['posf'])
                    S.op('pool', lambda e: e.tensor_copy(posb[:], posf[:]), reads=['posf'], writes=['posb'])
                    S.dma('sp', vcE[:, :, 64:128], cd['ovl'], 'cw4', writes=[('vcE', 'ov')])
                    for slot in range(NSLOT):
                        ps, pk = proj_slot(slot, wk_, wkk, ncols=396)
                        tsl = slice(slot * 128, (slot + 1) * 128)
                        S.op('act', lambda e, ps=ps, slot=slot: e.copy(vsa[:, slot, 0:64], ps[:, 256:320]), reads=[pk], writes=[('vsa', slot)])
                        S.op('act', lambda e, ps=ps, slot=slot: e.copy(vwa[:, slot, 0:64], ps[:, 320:384]), reads=[pk], writes=[('vwa', slot)])
                        S.op('act', lambda e, ps=ps, slot=slot: e.activation(gsig[:, slot, :], ps[:, 384:396], AF.Sigmoid), reads=[pk], writes=[('gsig', slot)])
                        kb_, kbk = WB()
                        S.op('dve', lambda e, ps=ps, kb_=kb_: e.tensor_copy(kb_[:, 0:128], ps[:, 0:128]), reads=[pk], writes=[kbk])
                        rope(ps[:, 128:256].rearrange("p (h c) -> p h c", c=64), pk,
                             kb_[:, 128:256].rearrange("p (h c) -> p h c", c=64), kbk, 2,
                             ct['rcc'][:, slot, :], ct['rss'][:, slot, :], ['c_rcc', 'c_rss'])
                        pt, ptk = PT()

                        def f(e, pt=pt, kb_=kb_):
                            e.transpose(pt[0:64, 0, :], kb_[:, 0:64], ident[:])
                            e.transpose(pt[0:64, 3, :], kb_[:, 64:128], ident[:])
                            e.transpose(pt[0:64, 1, :], kb_[:, 128:192], ident[:])
                            return e.transpose(pt[0:64, 2, :], kb_[:, 192:256], ident[:])
                        S.op('pe', f, reads=[kbk, 'c_ident'], writes=[ptk])
                        S.op('dve', lambda e, pt=pt, tsl=tsl: e.tensor_copy(kcrT[:, tsl], pt[0:64, 0, :]), reads=[ptk], writes=[('kcrT', slot)])
                        S.op('act', lambda e, pt=pt, tsl=tsl: e.copy(vcrT[:, tsl], pt[0:64, 3, :]), reads=[ptk], writes=[('vcrT', slot)])
                        S.op('act', lambda e, pt=pt, tsl=tsl: e.copy(ksT[:, tsl], pt[0:64, 1, :]), reads=[ptk], writes=[('ksT', slot)])
                        S.op('dve', lambda e, pt=pt, tsl=tsl: e.tensor_copy(kwT[:, tsl], pt[0:64, 2, :]), reads=[ptk], writes=[('kwT', slot)])
                    allkv = [[('kcrT', s_) for s_ in range(NSLOT)], [('vcrT', s_) for s_ in range(NSLOT)]]
                    bps, bpk = PR()
                    kv3 = [kvT[t][:].rearrange("p (n r) -> p n r", r=16) for t in range(2)]
                    for piece in range(4):
                        for t in range(2):
                            si = wst_rot()
                            stv = wst[si][:].rearrange("p a b -> p (a b)").rearrange("p (a b) -> p a b", b=256)
                            S.dma('sp', stv[0:64], w1_d[l, t, piece * 512:(piece + 1) * 512, :].rearrange("(l d) c -> d l c", d=64),
                                  f"wst{si}", writes=[f"wst{si}"])
                            S.op('pool', lambda e, t=t, stv=stv: e.tensor_copy(w1b[:, t, :, :], stv[0:64]),
                                 reads=[f"wst{si}"], writes=[("w1b", t)])
                        for t in range(2):
                            def f(e, t=t, piece=piece):
                                ins = None
                                for li in range(8):
                                    lg = piece * 8 + li
                                    for c in range(2):
                                        o_ = pA[t][:, c * 256:(c + 1) * 256]
                                        w_ = w1b[:, t, li, c * 128:(c + 1) * 128]
                                        first = (lg == 0 and c == 0)
                                        last = (lg == 31)
                                        if lg < 16:
                                            ins = e.matmul(o_, w_, kv3[t][:, 0:256, lg], start=first, stop=False, skip_group_check=True)
                                        else:
                                            ins = e.matmul(o_[:, 0:255], w_, kv3[t][:, 1:256, lg - 16], start=False, stop=last, skip_group_check=True)
                                        ins = e.matmul(bps[:, (t * 2 + c):(t * 2 + c) + 1], w_, posb[:, t, lg:lg + 1],
                                                       start=(t == 0 and first), stop=last, skip_group_check=True)
                                return ins
                            S.op('pe', f, reads=allkv[t] + [("w1b", t), 'posb'], writes=[f"pA{t}", bpk])
                    S.op('dve', lambda e: e.tensor_tensor(biasv[:], bps[:, 0:4], b1t[:], ALU.add), reads=[bpk, 'b1t'], writes=['biasv'])
                    for t in range(2):
                        for c in range(2):
                            j = t * 2 + c
                            (xs_, xsk), (x2_, x2k), (u__, uk) = WF(), WF(), WF()
                            xs, x2, u_ = xs_[:, 0:256], x2_[:, 0:256], u__[:, 0:256]
                            S.op('dve', lambda e, t=t, c=c, j=j, xs=xs: e.tensor_scalar(out=xs, in0=pA[t][:, c * 256:(c + 1) * 256], scalar1=biasv[:, j:j + 1], scalar2=None, op0=ALU.add),
                                 reads=[f"pA{t}", 'biasv'], writes=[xsk])
                            S.op('dve', lambda e, xs=xs, x2=x2: e.tensor_tensor(x2, xs, xs, ALU.mult), reads=[xsk], writes=[x2k])
                            S.op('dve', lambda e, x2=x2: e.tensor_scalar(out=x2, in0=x2, scalar1=0.044715, scalar2=1.0, op0=ALU.mult, op1=ALU.add), reads=[x2k], writes=[x2k])
                            S.op('dve', lambda e, xs=xs, x2=x2, u_=u_: e.tensor_tensor(u_, x2, xs, ALU.mult), reads=[x2k, xsk], writes=[uk])
                            S.op('act', lambda e, u_=u_: e.activation(u_, u_, AF.Sigmoid, scale=1.5957691216057308), reads=[uk], writes=[uk])
                            S.op('dve', lambda e, j=j, xs=xs, u_=u_: e.tensor_tensor(hidT[:, j, :], xs, u_, ALU.mult), reads=[xsk, uk], writes=[('hidT', j)])
                    for nchunk in range(2):
                        ps, pk = PR()

                        def f(e, ps=ps, nchunk=nchunk):
                            for t in range(2):
                                for c in range(2):
                                    ins = e.matmul(ps[:, t * 64:(t + 1) * 64], hidT[:, t * 2 + c, nchunk * 128:(nchunk + 1) * 128], w2b[:, t, c, :],
                                                   start=(c == 0), stop=(c == 1))
                            return ins
                        S.op('pe', f, reads=[('hidT', j) for j in range(4)] + ['w2b'], writes=[pk])
                        S.op('act', lambda e, ps=ps, nchunk=nchunk: e.copy(vcE[:, nchunk, 0:64], ps[:, 64:128]), reads=[pk], writes=[('vcE', nchunk)])
                        kb_, kbk = WB()
                        rope(ps[:, 0:64].rearrange("p (h c) -> p h c", c=64), pk,
                             kb_[:, 0:64].rearrange("p (h c) -> p h c", c=64), kbk, 1,
                             ct['rccc'][:, nchunk, :], ct['rssc'][:, nchunk, :], ['c_rccc', 'c_rssc'])
                        pt, ptk = PT()
                        S.op('pe', lambda e, pt=pt, kb_=kb_: e.transpose(pt[0:64, 0, :], kb_[:, 0:64], ident[:]),
                             reads=[kbk, 'c_ident'], writes=[ptk])
                        S.op('dve', lambda e, pt=pt, nchunk=nchunk: e.tensor_copy(kcT[:, nchunk * 128:(nchunk + 1) * 128], pt[0:64, 0, :]),
                             reads=[ptk], writes=[('kcT', nchunk)])
                    for qg in range(nq_groups):
                        run_pipeline(qs_iters(wq_, wqk, qg, None, None, zt, 'ztc', do_rope=True, pairs=False, qcT=qcT), 4)
                        for i in range(4):
                            m = qg * 4 + i
                            qv = qcT[:, i, :, :].rearrange("p h q -> p (h q)")
                            acc, acck = pA[0], 'pA0'
                            nch = 1 if m < 16 else 2
                            for c in range(nch):
                                ps, pk = PR()
                                S.op('pe', lambda e, ps=ps, c=c, qv=qv: e.matmul(ps[:], kcT[:, c * 128:(c + 1) * 128], qv, start=True, stop=True),
                                     reads=[('kcT', c), ('qcT', i)], writes=[pk])
                                pb, pbk = WB()
                                S.op('act', lambda e, pb=pb, ps=ps: e.activation(pb[:], ps[:], AF.Exp, scale=SCALE), reads=[pk], writes=[pbk])
                                u = m - 16 * c
                                if u < 17:
                                    S.op('dve', lambda e, pb=pb, u=u: e.tensor_tensor(
                                        pb[:].rearrange("p (h c) -> p h c", c=128), pb[:].rearrange("p (h c) -> p h c", c=128),
                                        ct['cv'][:, u, :].unsqueeze(1).to_broadcast([128, 4, 128]), ALU.mult),
                                        reads=[pbk, 'c_cv'], writes=[pbk])
                                av(acc, acck, pb, pbk, lambda h, c=c: vcE[:, c, :], [('vcE', c), ('vcE', 'ov')], c == 0, c == nch - 1, 128)
                            accv = acc[:].rearrange("p (h c) -> p h c", c=128)
                            S.op('dve', lambda e, accv=accv: e.tensor_reduce(out=cf[:, 0:4], in_=accv[:, :, 64:128], axis=AX.X, op=ALU.add), reads=[acck, ('vcE', 'ov')], writes=[('cf', 0)])
                            S.op('dve', lambda e: e.tensor_scalar(out=cf[:, 0:4], in0=cf[:, 0:4], scalar1=1e-30, scalar2=None, op0=ALU.max), reads=[('cf', 0)], writes=[('cf', 0)])
                            S.op('dve', lambda e: e.reciprocal(cf[:, 4:8], cf[:, 0:4]), reads=[('cf', 0)], writes=[('cf', 1)])
                            for h in range(4):
                                if h == 0:
                                    S.op('dve', lambda e, accv=accv: e.tensor_scalar(out=impt[:], in0=accv[:, 0, 64:128], scalar1=cf[:, 4:5], scalar2=None, op0=ALU.mult),
                                         reads=[acck, ('cf', 1)], writes=['impt'])
                                else:
                                    S.op('dve', lambda e, accv=accv, h=h: e.scalar_tensor_tensor(out=impt[:], in0=accv[:, h, 64:128], scalar=cf[:, 4 + h:5 + h], in1=impt[:], op0=ALU.mult, op1=ALU.add),
                                         reads=[acck, ('cf', 1), 'impt'], writes=['impt'])
                            gv = gsig[:, m, :].rearrange("p (h b) -> p h b", b=3)
                            S.op('dve', lambda e, gv=gv: e.tensor_tensor(cf[:, 8:12], cf[:, 4:8], gv[:, :, 0], ALU.mult), reads=[('cf', 1), ('gsig', m)], writes=[('cf', 2)])
                            ocv = ocacc[:].rearrange("p (h c) -> p h c", c=64)
                            S.op('dve', lambda e, accv=accv, ocv=ocv: e.tensor_tensor(ocv, accv[:, :, 0:64], cf[:, 8:12].unsqueeze(2).to_broadcast([128, 4, 64]), ALU.mult),
                                 reads=[acck, ('cf', 2)], writes=['ocacc'])
                            vsl = ct['vt'][:, 64 - 2 * m:128 - 2 * m]
                            if m <= 7:
                                S.op('dve', lambda e, vsl=vsl: e.tensor_copy(selm[:], vsl), reads=['c_vt'], writes=['selm'])
                            else:
                                t1s = ct['t1'][:, 64 - 2 * m:128 - 2 * m]
                                t2s = ct['t2'][:, 64 - 2 * m:128 - 2 * m]
                                S.op('dve', lambda e, t1s=t1s: e.tensor_tensor(imps[:], impt[:], t1s, ALU.mult), reads=['impt', 'c_t1'], writes=['imps'])
                                S.op('dve', lambda e, t2s=t2s: e.tensor_tensor(imps[:], imps[:], t2s, ALU.add), reads=['imps', 'c_t2'], writes=['imps'])
                                S.op('dve', lambda e: e.memset(imps[:, 0:1], BIG), reads=['imps'], writes=['imps'])
                                S.op('dve', lambda e: e.max(out=mx8[:, 0:8], in_=imps[:]), reads=['imps'], writes=[('mx8', 0)])
                                S.op('dve', lambda e: e.match_replace(out=impt[:], in_to_replace=mx8[:, 0:8], in_values=imps[:], imm_value=-3e38),
                                     reads=[('mx8', 0), 'imps'], writes=['impt'])
                                S.op('dve', lambda e: e.max(out=mx8[:, 8:16], in_=impt[:]), reads=['impt'], writes=[('mx8', 1)])
                                S.op('dve', lambda e: e.tensor_scalar(out=imps[:], in0=imps[:], scalar1=mx8[:, 15:16], scalar2=None, op0=ALU.is_ge),
                                     reads=['imps', ('mx8', 1)], writes=['imps'])
                                S.op('dve', lambda e, vsl=vsl: e.tensor_tensor(selm[:], imps[:], vsl, ALU.mult), reads=['imps', 'c_vt'], writes=['selm'])
                            def c_fin(acc, acck, gcol, m=m, gv=gv):
                                accv = acc[:, 0:260].rearrange("p (h c) -> p h c", c=65)
                                S.op('dve', lambda e: e.reciprocal(cf[:, 12:16], accv[:, :, 64]), reads=[acck], writes=[('cf', 3)])
                                S.op('dve', lambda e: e.tensor_tensor(cf[:, 12:16], cf[:, 12:16], gv[:, :, gcol], ALU.mult), reads=[('cf', 3), ('gsig', m)], writes=[('cf', 3)])
                                tmpo, tmpk = WF()
                                S.op('dve', lambda e: e.tensor_tensor(tmpo[:, 0:256].rearrange("p (h c) -> p h c", c=64), accv[:, :, 0:64],
                                                                      cf[:, 12:16].unsqueeze(2).to_broadcast([128, 4, 64]), ALU.mult),
                                     reads=[acck, ('cf', 3)], writes=[tmpk])
                                S.op('pool', lambda e: e.tensor_tensor(ocacc[:], ocacc[:], tmpo[:, 0:256], ALU.add), reads=['ocacc', tmpk], writes=['ocacc'])

                            def c_iter(kind, kb, first, last, m=m, i=i, qv=qv, c_fin=c_fin):
                                c = {}
                                sel = (kind == 'sel')
                                acc, acck = (pA[1], 'pA1') if sel else (pA[0], 'pA0')
                                kT_, kTk = (ksT, 'ksT') if sel else (kwT, 'kwT')
                                vA_, vAk = (vsa, 'vsa') if sel else (vwa, 'vwa')
                                d = m - kb
                                bias = None
                                if (sel and kb == m) or ((not sel) and d == 0):
                                    bias = 'ntri4'
                                elif (not sel) and d == 4:
                                    bias = 'nw44'

                                def s0():
                                    ps, pk = PR()
                                    c['ps'], c['pk'] = ps, pk

                                    def f(e):
                                        ins = e.matmul(ps[:], kT_[:, kb * 128:(kb + 1) * 128], qv, start=True, stop=not bias)
                                        if bias:
                                            ins = e.matmul(ps[:], ident[:], ct[bias][:], start=False, stop=True)
                                        return ins
                                    S.op('pe', f, reads=[(kTk, kb), ('qcT', i), 'c_ident'] + (['c_' + bias] if bias else []), writes=[pk])
                                    if sel:
                                        mp, mpk = PR()
                                        c['mp'], c['mpk'] = mp, mpk

                                        def fm(e):
                                            e.matmul(mp[0:64, 0:128], selm[:, 2 * kb:2 * kb + 1].to_broadcast([128, 64]), ident[:], start=True, stop=True)
                                            return e.matmul(mp[64:128, 0:128], selm[:, 2 * kb + 1:2 * kb + 2].to_broadcast([128, 64]), ident[:], start=True, stop=True)
                                        S.op('pe', fm, reads=['selm', 'c_ident'], writes=[mpk])

                                def s1():
                                    c['pb'], c['pbk'] = WB()
                                    pb, pbk = c['pb'], c['pbk']
                                    S.op('act', lambda e: e.activation(pb[:], c['ps'][:], AF.Exp, scale=SCALE), reads=[c['pk']], writes=[pbk])
                                    if sel:
                                        S.op('dve', lambda e: e.tensor_tensor(
                                            pb[:].rearrange("p (h c) -> p h c", c=128), pb[:].rearrange("p (h c) -> p h c", c=128),
                                            c['mp'][:, 0:128].unsqueeze(1).to_broadcast([128, 4, 128]), ALU.mult),
                                            reads=[pbk, c['mpk']], writes=[pbk])

                                def s2():
                                    av(acc, acck, c['pb'], c['pbk'], lambda h: vA_[:, kb, :], (vAk, kb), first, last, 65)
                                    if last:
                                        c_fin(acc, acck, 1 if sel else 2)
                                        if not sel:
                                            store_mixed(m, 512, lambda: ocacc[:], ['ocacc'], zt, 'ztc', i)
                                return [s0, s1, s2]
                            its = [c_iter('sel', kb, kb == 0, kb == m) for kb in range(0, m + 1)]
                            dmax = min(4, m)
                            its += [c_iter('win', m - d, d == 0, d == dmax) for d in range(0, dmax + 1)]
                            run_pipeline(its, 3)
                    S.barrier(drop_prefixes=('kcrT', 'vcrT', 'ksT', 'kwT', 'vsa', 'vwa', 'gsig', 'kcT', 'vcE', 'qcT', 'ztc', 'w1b', 'w2', 'b1t', 'pos',
                                             'biasv', 'hidT', 'gx', 'ocacc', 'imp', 'selm', 'selb', 'mx8', 'cf'))

            if 'D' in mixers:
                with ExitStack() as mds:
                    kT = T("kdT", [128, 2, S_LEN], BF16, mds)
                    vaug = T("vdaug", [128, NSLOT, 4, 65], BF16, mds)
                    qT2s = [T(f"qT2d{b_}", [128, 2, 4, 2, 128], BF16, mds) for b_ in range(2)]
                    zts = [T(f"ztd{b_}", [128, 4, 256], BF16, mds) for b_ in range(2)]
                    kmf = T("kmf", [128, 2, 16], F32, mds)
                    kmb = T("kmb", [128, 2, 16], BF16, mds)
                    gm = T("gm", [128, 4, 16], F32, mds)
                    gmx = T("gmx", [128, 4, 8], F32, mds)
                    isel = T("isel", [128, 4, 16], BF16, mds)
                    for b_ in range(2):
                        S.op('pool', lambda e, b_=b_: e.memset(qT2s[b_][:].rearrange("p a b c d -> p (a b c d)"), 0.0), writes=[(f'qT2_{b_}', i, j) for i in range(4) for j in range(2)])
                    S.op('pool', lambda e: e.memset(vaug[:].rearrange("p a b c -> p (a b c)"), 1.0), writes=[('vaug', s_) for s_ in range(NSLOT)])
                    wk_, wkk = load_wgroup(l, 6)
                    wq_, wqk = load_wgroup(l, 7)
                    def rk_iter(slot, kT_, kTkey, vaug_, wk__, wkk__):
                        c = {}

                        def s0():
                            c['ps'], c['pk'] = proj_slot(slot, wk__, wkk__)

                        def s1():
                            ps, pk = c['ps'], c['pk']
                            S.op('act', lambda e: e.copy(vaug_[:, slot, :, 0:64], ps[:, 256:512].rearrange("p (h c) -> p h c", c=64)),
                                 reads=[pk], writes=[('vaug', slot)])
                            c['kb'], c['kbk'] = WB()
                            rope(ps[:, 0:256].rearrange("p (h c) -> p h c", c=64), pk,
                                 c['kb'][:, 0:256].rearrange("p (h c) -> p h c", c=64), c['kbk'], 4,
                                 ct['rcc'][:, slot, :], ct['rss'][:, slot, :], ['c_rcc', 'c_rss'])

                        def s2():
                            c['pt'], c['ptk'] = qk_transpose(c['kb'], [c['kbk']], None, None, 2)

                        def s3():
                            S.op('dve', lambda e: e.tensor_copy(kT_[:, :, slot * 128:(slot + 1) * 128], c['pt'][:, 0:2, :]),
                                 reads=[c['ptk']], writes=[(kTkey, slot)])
                        return [s0, s1, s2, s3]
                    run_pipeline([rk_iter(slot, kT, 'kT', vaug, wk_, wkk) for slot in range(NSLOT)], 4)
                    allk = [('kT', s_) for s_ in range(NSLOT)]
                    for p in range(2):
                        S.op('dve', lambda e, p=p: e.tensor_reduce(out=kmf[:, p, :], in_=kT[:, p, :].rearrange("p (n r) -> p n r", r=256), axis=AX.X, op=ALU.add),
                             reads=allk, writes=[('kmf', p)])
                    S.op('dve', lambda e: e.tensor_scalar(out=kmb[:], in0=kmf[:], scalar1=1.0 / 256, scalar2=None, op0=ALU.mult),
                         reads=[('kmf', 0), ('kmf', 1)], writes=['kmb'])
                    isel2 = [isel, T("isel2", [128, 4, 16], BF16, mds)]

                    def d_prep(m, i, own, qT2, qT2k):
                        isl = isel2[m % 2]
                        islk = f"isel{m % 2}"
                        gp_, gpk = PR()

                        def fg(e):
                            for h in range(4):
                                ins = e.matmul(gp_[:, h * 16:(h + 1) * 16], qT2[:, h // 2, i, h % 2, :], kmb[:, h // 2, :], start=True, stop=True)
                            return ins
                        S.op('pe', fg, reads=[(qT2k, i, 0), (qT2k, i, 1), 'kmb'], writes=[gpk])
                        S.op('dve', lambda e: e.memset(gm[:].rearrange("p a b -> p (a b)"), NEGB), writes=['gm'])
                        S.op('dve', lambda e: e.tensor_copy(gm[:, :, 0:own], gp_[:, 0:64].rearrange("p (h n) -> p h n", n=16)[:, :, 0:own]),
                             reads=[gpk, 'gm'], writes=['gm'])
                        for h in range(4):
                            S.op('dve', lambda e, h=h: e.max(out=gmx[:, h, :], in_=gm[:, h, :]), reads=['gm'], writes=[('gmx', h)])
                        for h in range(4):
                            S.op('dve', lambda e, h=h: e.tensor_scalar(out=gm[:, h, :], in0=gm[:, h, :], scalar1=gmx[:, h, 2:3], scalar2=None, op0=ALU.is_ge),
                                 reads=['gm', ('gmx', h)], writes=['gm'])
                        S.op('dve', lambda e: e.tensor_scalar(out=isl[:].rearrange("p a b -> p (a b)"), in0=gm[:].rearrange("p a b -> p (a b)"), scalar1=-1.0, scalar2=240.0, op0=ALU.add, op1=ALU.mult),
                             reads=['gm'], writes=[islk])

                    def d_iter(m, i, kb, mt, first, last, own, qT2, qT2k, zt, ztk):
                        c = {}
                        acc, acck = pA[m % 2], f"pA{m % 2}"
                        isl = isel2[m % 2]
                        islk = f"isel{m % 2}"

                        def s0():
                            if first and own > 3:
                                d_prep(m, i, own, qT2, qT2k)
                            ps, pk = PR()
                            c['ps'], c['pk'] = ps, pk
                            n_ = kb // 2

                            def f(e):
                                for p in range(2):
                                    ins = e.matmul(ps[:, p * 256:(p + 1) * 256], kT[:, p, kb * 128:(kb + 1) * 128],
                                                   qT2[:, p, i, :, :].rearrange("p a b -> p (a b)"), start=(p == 0), stop=(mt == 'none' and p == 1),
                                                   skip_group_check=True)
                                if mt == 'sel':
                                    for h in range(4):
                                        ins = e.matmul(ps[:, h * 128:(h + 1) * 128], isl[:, h, n_:n_ + 1].to_broadcast([128, 128]), ident[:],
                                                       start=False, stop=(h == 3), skip_group_check=True)
                                elif mt == 'diag':
                                    ins = e.matmul(ps[:], ident[:], ct['ntri4'][:], start=False, stop=True, skip_group_check=True)
                                return ins
                            S.op('pe', f, reads=[('kT', kb), (qT2k, i, 0), (qT2k, i, 1), 'c_ident', 'c_ntri4'] + ([islk] if mt == 'sel' else []), writes=[pk])

                        def s1():
                            c['pb'], c['pbk'] = WB()
                            S.op('act', lambda e: e.activation(c['pb'][:], c['ps'][:], AF.Exp, scale=SCALE), reads=[c['pk']], writes=[c['pbk']])

                        def s2():
                            av(acc, acck, c['pb'], c['pbk'], lambda h: vaug[:, kb, h, :], ('vaug', kb), first, last, 65)
                            if last:
                                o, ok = normalize(acc, acck)
                                store_mixed(m, 768, lambda: o[:, 0:256], [ok], zt, ztk, i)
                        return [s0, s1, s2]
                    run_pipeline(qs_iters(wq_, wqk, 0, qT2s[0], 'qT2_0', zts[0], 'ztd_0', do_rope=True), 4)
                    for qg in range(nq_groups):
                        b_ = qg % 2
                        its = []
                        for i in range(4):
                            m = qg * 4 + i
                            own = m // 2
                            steps = [(kb, 'sel' if own > 3 else 'none') for kb in range(0, 2 * own)]
                            if m % 2 == 1:
                                steps.append((m - 1, 'none'))
                            steps.append((m, 'diag'))
                            for si_, (kb, mt) in enumerate(steps):
                                its.append(d_iter(m, i, kb, mt, si_ == 0, si_ == len(steps) - 1, own, qT2s[b_], f'qT2_{b_}', zts[b_], f'ztd_{b_}'))
                        extra = qs_iters(wq_, wqk, qg + 1, qT2s[1 - b_], f'qT2_{1 - b_}', zts[1 - b_], f'ztd_{1 - b_}', do_rope=True) if qg + 1 < nq_groups else []
                        run_pipeline(interleave(its, extra), 4)
                    S.barrier(drop_prefixes=('kT', 'vaug', 'qT2', 'ztd', 'kmf', 'kmb', 'gm', 'isel'))

            with ExitStack() as ps3:
                if dbg and (mixers != 'ABCD' or nq_groups != 8):
                    break
                wo = T("wo", [128, 8, D], BF16, ps3)
                mx = [T(f"mxl{i}", [128, D], BF16, ps3) for i in range(2)]
                mT = [T(f"mTl{i}", [128, 8, 128], BF16, ps3) for i in range(2)]
                xr = [T(f"xr{i}", [128, D], F32, ps3) for i in range(2)]
                ot = [T(f"ot{i}", [128, D], F32, ps3) for i in range(2)]
                junk3 = T("junk3", [128, D], BF16, ps3)
                st3 = [T(f"st3_{i}", [128, 8], F32, ps3) for i in range(2)]
                wsrc = wout_d[l].rearrange("(k p) c -> p k c", p=128)
                for q4 in range(4):
                    si = wst_rot()
                    stv = wst[si][:].rearrange("p a b -> p (a b)").rearrange("p (a b) -> p a b", b=D)
                    S.dma('sp', stv, wsrc[:, q4 * 2:(q4 + 1) * 2, :], f"wst{si}", writes=[f"wst{si}"])
                    S.op('pool', lambda e, q4=q4, stv=stv: e.tensor_copy(wo[:, q4 * 2:(q4 + 1) * 2, :], stv), reads=[f"wst{si}"], writes=[('wo', q4)])
                def p3_iter(slot):
                    i2 = slot % 2
                    tsl = slice(slot * 128, (slot + 1) * 128)
                    stt, stk = st3[i2], f"st3_{i2}"
                    if slot % 2:
                        (y0, y0k), (y1, y1k) = (pR[0], 'pR0'), (pR[1], 'pR1')
                    else:
                        (y0, y0k), (y1, y1k) = (pA[0], 'pA0'), (pA[1], 'pA1')

                    def s0():
                        S.dma('sp', mx[i2][:], mixed_d[tsl, :], f"mxl{i2}", reads=[('mixed', slot, c0) for c0 in (0, 256, 512, 768)], writes=[f"mxl{i2}"])

                    def s1():
                        pt, ptk = PT()

                        def f(e):
                            for k in range(8):
                                ins = e.transpose(pt[:, k, :], mx[i2][:, k * 128:(k + 1) * 128], ident[:])
                            return ins
                        S.op('pe', f, reads=[f"mxl{i2}", 'c_ident'], writes=[ptk])
                        S.op('act', lambda e: e.copy(mT[i2][:], pt[:]), reads=[ptk], writes=[f"mTl{i2}"])

                    def s2():
                        S.dma('sp', xr[i2][:], xin[tsl, :], f"xr{i2}", reads=[(xin_key, slot)], writes=[f"xr{i2}"])

                        def fy(e):
                            for hh, y in enumerate((y0, y1)):
                                for k in range(8):
                                    ins = e.matmul(y[:], mT[i2][:, k, :], wo[:, k, hh * 512:(hh + 1) * 512], start=(k == 0), stop=(k == 7))
                            return ins
                        S.op('pe', fy, reads=[f"mTl{i2}"] + [('wo', q4) for q4 in range(4)], writes=[y0k, y1k])

                    def s3():
                        S.op('act', lambda e: e.activation(junk3[:, 0:512], y0[:], AF.Square, accum_out=stt[:, 0:1]), reads=[y0k], writes=[('junk3', 0), (stk, 0)])
                        S.op('act', lambda e: e.activation(junk3[:, 512:1024], y1[:], AF.Square, accum_out=stt[:, 1:2]), reads=[y1k], writes=[('junk3', 1), (stk, 1)])
                        S.op('dve', lambda e: e.tensor_tensor(stt[:, 2:3], stt[:, 0:1], stt[:, 1:2], ALU.add), reads=[(stk, 0), (stk, 1)], writes=[(stk, 2)])
                        S.op('dve', lambda e: e.tensor_scalar(out=stt[:, 5:6], in0=stt[:, 2:3], scalar1=1.0 / D, scalar2=EPS, op0=ALU.mult, op1=ALU.add), reads=[(stk, 2)], writes=[(stk, 5)])
                        S.op('act', lambda e: e.activation(stt[:, 3:4], stt[:, 5:6], AF.Sqrt), reads=[(stk, 5)], writes=[(stk, 3)])
                        S.op('dve', lambda e: e.reciprocal(stt[:, 4:5], stt[:, 3:4]), reads=[(stk, 3)], writes=[(stk, 4)])
                        for hh, (y, yk) in enumerate(((y0, y0k), (y1, y1k))):
                            S.op('dve', lambda e, y=y, hh=hh: e.scalar_tensor_tensor(out=ot[i2][:, hh * 512:(hh + 1) * 512], in0=y[:], scalar=stt[:, 4:5],
                                                                                   in1=gpt[:, hh * 512:(hh + 1) * 512], op0=ALU.mult, op1=ALU.mult),
                                 reads=[yk, (stk, 4), 'gpt'], writes=[(f"ot{i2}", hh)])
                        S.op('pool', lambda e: e.tensor_tensor(ot[i2][:], ot[i2][:], xr[i2][:], ALU.add),
                             reads=[(f"ot{i2}", 0), (f"ot{i2}", 1), f"xr{i2}"], writes=[(f"ot{i2}", 0), (f"ot{i2}", 1)])
                        S.dma('pool', xout[tsl, :], ot[i2][:], f'outst{i2}', reads=[(f"ot{i2}", 0), (f"ot{i2}", 1)], writes=[(xout_key, slot)])
                    return [s0, s1, s2, s3]
                run_pipeline([p3_iter(slot) for slot in range(NSLOT)], 4)
                S.barrier(drop_prefixes=('wo', 'mxl', 'mTl', 'xr', 'ot', 'junk3', 'st3_'))
        S.finish()
        build.stats = dict(nins=S.nins, nwait=S.nwait, nsem=S.nsem)
    return nc


def _prep_inputs(inputs):
    f = np.float32
    x = np.asarray(inputs['x'], f)
    c = np.asarray(inputs['c'], f)
    w_in = np.asarray(inputs['w_in'], f)
    shared = {}
    shared['npre'] = np.ascontiguousarray(np.broadcast_to(np.asarray(inputs['norm_pre'], f)[:, None, :], (L, 128, D)))
    shared['npost'] = np.ascontiguousarray(np.broadcast_to(np.asarray(inputs['norm_post'], f)[:, None, :], (L, 128, D)))
    shared['bmod'] = np.ascontiguousarray(np.broadcast_to(np.asarray(inputs['b_mod'], f)[:, None, :], (L, 128, 3 * D)))
    shared['wmod'] = np.ascontiguousarray(np.asarray(inputs['w_mod'], f))
    shared['wg'] = np.ascontiguousarray(np.stack([_w_groups(w_in[l]) for l in range(L)], 0))
    shared['wout'] = np.ascontiguousarray(np.asarray(inputs['w_out'], f))
    shared['w1'] = np.ascontiguousarray(np.asarray(inputs['cmp_w1'], f))
    shared['w2'] = np.ascontiguousarray(np.asarray(inputs['cmp_w2'], f))
    b1 = np.asarray(inputs['cmp_b1'], f)
    shared['b1'] = np.ascontiguousarray(b1.reshape(L, 2, 2, 128).transpose(0, 3, 1, 2).reshape(L, 128, 4))
    pos = np.asarray(inputs['cmp_pos'], f)
    shared['posT'] = np.ascontiguousarray(pos.transpose(0, 3, 1, 2))
    for k_, v_ in _consts().items():
        shared['c_' + k_] = v_
    maps = []
    for b in range(x.shape[0]):
        m = dict(shared)
        m['x'] = np.ascontiguousarray(x[b])
        m['cT'] = np.ascontiguousarray(c[b].reshape(8, 128).T)
        maps.append(m)
    return maps


_NC_CACHE = {}


def kernel(**inputs):
    maps = _prep_inputs(inputs)
    if 'nc' not in _NC_CACHE:
        _NC_CACHE['nc'] = build()
    nc = _NC_CACHE['nc']
    res = run_bass_kernel_spmd(nc, maps, core_ids=list(range(len(maps))))
    out = np.stack([np.asarray(r['out'], np.float32) for r in res.results], 0)
    return out
```

```python
import numpy as np
from contextlib import ExitStack
import concourse.bass as bass
import concourse.mybir as mybir
from concourse.bass_utils import run_bass_kernel_spmd
import ml_dtypes

F32 = mybir.dt.float32
BF16 = mybir.dt.bfloat16
AF = mybir.ActivationFunctionType
ALU = mybir.AluOpType
AX = mybir.AxisListType

S_LEN = 4096
D = 1024
NSLOT = 32
L = 2
EPS = 1e-6
SCALE = 0.125
BIG = 1e9
NEGB = -1e30


class Sched:
    EPOCH = 20000

    def __init__(self, nc, es, same_engine_sync=True):
        self.nc = nc
        self.es = es
        self.eng = dict(pe=nc.tensor, act=nc.scalar, dve=nc.vector, pool=nc.gpsimd, sp=nc.sync)
        self.cur = {}
        self.nsem = 0
        self.bufs = {}
        self.seen = {e: {} for e in self.eng}
        self.streams = {}
        self.same = same_engine_sync
        self.nwait = 0
        self.nins = 0
        self.last_tok = {}

    def _newsem(self, name):
        self.nsem += 1
        return self.es.enter_context(self.nc.semaphore(f"s{self.nsem}_{name}"))

    def _tick(self, e):
        c = self.cur.get(e)
        if c is None or c[1] >= self.EPOCH:
            c = [self._newsem(e), 0]
            self.cur[e] = c
        c[1] += 1
        return ('c', c[0], c[1], e)

    def _wait(self, e, tok):
        if tok[0] == 'c':
            _, sem, val, src = tok
            if src == e and (e == 'pe' or not self.same):
                return
        else:
            _, st = tok
            sem = st[0]
            val = 16 * st[1]
        k = sem.name
        if self.seen[e].get(k, 0) >= val:
            return
        self.eng[e].wait_ge(sem, val)
        self.nwait += 1
        self.seen[e][k] = val

    def _deps(self, reads, writes):
        deps = []
        for r in reads:
            b = self.bufs.get(r)
            if b and b['w'] is not None:
                deps.append(b['w'])
        for w in writes:
            b = self.bufs.get(w)
            if b:
                if b['w'] is not None:
                    deps.append(b['w'])
                deps.extend(b['r'].values())
        return deps

    def _record(self, e, tok, reads, writes):
        for r in reads:
            self.bufs.setdefault(r, dict(w=None, r={}))['r'][e] = tok
        for w in writes:
            self.bufs[w] = dict(w=tok, r={})
        self.last_tok[e] = tok

    @staticmethod
    def _is_psum(k):
        return isinstance(k, str) and k[:2] in ('pR', 'pA', 'pT')

    def op(self, e, fn, reads=(), writes=()):
        psr = [r for r in reads if self._is_psum(r)]
        if psr:
            reads = [r for r in reads if not self._is_psum(r)]
            writes = list(writes) + [r for r in psr if r not in writes]
        for t in self._deps(reads, writes):
            self._wait(e, t)
        ins = fn(self.eng[e])
        tok = self._tick(e)
        ins.then_inc(tok[1], 1)
        self.nins += 1
        self._record(e, tok, reads, writes)
        return tok

    def dma(self, e, out, in_, stream, reads=(), writes=(), **kw):
        for t in self._deps(reads, writes):
            self._wait(e, t)
        st = self.streams.get(stream)
        if st is None:
            st = [self._newsem('d' + stream), 0]
            self.streams[stream] = st
        elif st[1] > 0:
            self._wait(e, ('d', st))
        ins = self.eng[e].dma_start(out=out, in_=in_, **kw)
        ins.then_inc(st[0], 16)
        st[1] += 1
        tok = ('d', st)
        self.nins += 1
        self._record('dma:' + stream, tok, reads, writes)
        return tok

    def barrier(self, drop_prefixes=()):
        toks = [t for k, t in self.last_tok.items()]
        for e in self.eng:
            for t in toks:
                self._wait(e, t)
        if drop_prefixes:
            for k in list(self.bufs.keys()):
                ks = k if isinstance(k, str) else k[0]
                if any(ks.startswith(p) for p in drop_prefixes):
                    del self.bufs[k]

    def finish(self):
        for stream, st in self.streams.items():
            self._wait('sp', ('d', st))
        for e, t in list(self.last_tok.items()):
            if not e.startswith('dma:'):
                self._wait('sp', t)


class Rot:
    def __init__(self, items):
        self.items = items
        self.i = 0

    def __call__(self):
        it = self.items[self.i % len(self.items)]
        self.i += 1
        return it


def _consts():
    bf = ml_dtypes.bfloat16
    c = {}
    k = np.arange(128)[:, None]
    q = np.arange(128)[None, :]
    c['ident'] = np.eye(128, dtype=np.float32).astype(bf)
    c['triS'] = (k < q).astype(np.float32).astype(bf)
    c['triI'] = (k <= q).astype(np.float32).astype(bf)
    c['w4'] = (q < k).astype(np.float32).astype(bf)
    c['umat'] = (k >= q).astype(np.float32).astype(bf)
    c['ones'] = np.ones((128, 128), np.float32).astype(bf)
    c['ntri4'] = np.tile(-240.0 * (1.0 - (k <= q).astype(np.float32)), (1, 4)).astype(bf)
    c['nw44'] = np.tile(-240.0 * (1.0 - (q < k).astype(np.float32)), (1, 4)).astype(bf)
    mb = np.zeros((128, 17, 128), np.float32)
    for d in range(17):
        j = 128 * d + q - k
        m = ((j >= 0) & (j <= 128)).astype(np.float32)
        m += ((j >= 0) & (j <= 512) & (j % 4 == 0)).astype(np.float32)
        m += ((j >= 0) & (j <= 2048) & (j % 16 == 0)).astype(np.float32)
        mb[:, d, :] = m
    with np.errstate(divide='ignore'):
        lb = np.where(mb > 0, np.log(np.maximum(mb, 1.0)) / SCALE, -240.0).astype(np.float32)
    hi = lb.astype(bf)
    c['lmbh'] = hi
    c['lmbl'] = (lb[:, 0:5, :] - hi[:, 0:5, :].astype(np.float32)).astype(bf)
    inv_freq = (1.0 / (500000.0 ** (np.arange(0, 16, 2, dtype=np.float32) / 16))).astype(np.float32)

    def tabs(pos):
        ang = pos.astype(np.float32)[:, None] * inv_freq[None, :]
        cs, sn = np.cos(ang).astype(np.float32), np.sin(ang).astype(np.float32)
        return np.concatenate([cs, cs], 1), np.concatenate([-sn, sn], 1)
    cc, ss = tabs(np.arange(S_LEN))
    c['rcc'] = np.ascontiguousarray(cc.reshape(32, 128, 16).transpose(1, 0, 2))
    c['rss'] = np.ascontiguousarray(ss.reshape(32, 128, 16).transpose(1, 0, 2))
    ccc, ssc = tabs(np.arange(256) * 16 + 31)
    c['rccc'] = np.ascontiguousarray(ccc.reshape(2, 128, 16).transpose(1, 0, 2))
    c['rssc'] = np.ascontiguousarray(ssc.reshape(2, 128, 16).transpose(1, 0, 2))
    cv = np.zeros((128, 17, 128), np.float32)
    for u in range(17):
        cv[:, u, :] = (16 * k + 31 - q <= 128 * u)
    c['cv'] = cv.astype(bf)
    n_cmp, n_sel = 255, 64
    c_start = np.arange(n_cmp) * 16
    s_start = np.arange(n_sel) * 64
    ov = np.clip(np.minimum(c_start[:, None] + 32, s_start[None, :] + 64)
                 - np.maximum(c_start[:, None], s_start[None, :]), 0, None) / 32
    ovp = np.zeros((256, 64), np.float32)
    ovp[:255] = ov
    c['ovl'] = np.ascontiguousarray(ovp.reshape(2, 128, 64).transpose(1, 0, 2)).astype(bf)
    qq = np.arange(128)[:, None]
    cidx = np.arange(128)[None, :]
    hi = qq // 64
    V = (cidx - 64 <= hi)
    Fm = (cidx - 64 >= hi - 1)
    c['t1'] = (V & ~Fm).astype(np.float32)
    c['t2'] = (BIG * (V & Fm) + NEGB * (~V)).astype(np.float32)
    c['vt'] = V.astype(np.float32)
    return c


GROUPS = None


def _w_groups(w_in_l):
    o = {}
    names = ['qa', 'ka', 'va', 'za', 'qb', 'kb', 'vb', 'zb', 'qc', 'kcr', 'vcr', 'ksr', 'vsr', 'kwr', 'vwr',
             'gc', 'zc', 'qd', 'kd', 'vd', 'zd']
    sizes = [256] * 8 + [256, 64, 64, 64, 64, 64, 64, 12, 256] + [256] * 4
    off = 0
    for n, s in zip(names, sizes):
        o[n] = w_in_l[:, off:off + s]
        off += s
    assert off == 3980
    pad = np.zeros((1024, 512 - 396), np.float32)
    g = [np.concatenate([o['ka'], o['va']], 1), np.concatenate([o['qa'], o['za']], 1),
         np.concatenate([o['kb'], o['vb']], 1), np.concatenate([o['qb'], o['zb']], 1),
         np.concatenate([o['kcr'], o['vcr'], o['ksr'], o['kwr'], o['vsr'], o['vwr'], o['gc'], pad], 1),
         np.concatenate([o['qc'], o['zc']], 1),
         np.concatenate([o['kd'], o['vd']], 1), np.concatenate([o['qd'], o['zd']], 1)]
    return np.stack(g, 0)


def build(n_layers=2, mixers='ABCD', dbg=False, nq_groups=8):
    nc = bass.Bass("TRN2", target_bir_lowering=False)
    C = _consts()

    def din(name, shape, dt=F32):
        return nc.dram_tensor(name, list(shape), dt, kind="ExternalInput").ap()
    x_d = din("x", [S_LEN, D])
    cT_d = din("cT", [128, 8])
    npre_d = din("npre", [L, 128, D])
    npost_d = din("npost", [L, 128, D])
    bmod_d = din("bmod", [L, 128, 3 * D])
    wmod_d = din("wmod", [L, D, 3 * D])
    wg_d = din("wg", [L, 8, D, 512])
    wout_d = din("wout", [L, D, D])
    w1_d = din("w1", [L, 2, 2048, 256])
    w2_d = din("w2", [L, 2, 256, 64])
    b1_d = din("b1", [L, 128, 4])
    posT_d = din("posT", [L, 64, 2, 32])
    cd = {}
    for k_, v_ in C.items():
        cd[k_] = din("c_" + k_, v_.shape, BF16 if v_.dtype == ml_dtypes.bfloat16 else F32)
    out_d = nc.dram_tensor("out", [S_LEN, D], F32, kind="ExternalOutput").ap()
    x1_d = nc.dram_tensor("x1s", [S_LEN, D], F32, kind="Internal").ap()
    mixed_d = nc.dram_tensor("mixed", [S_LEN, D], BF16, kind="ExternalOutput" if dbg else "Internal").ap()

    with ExitStack() as es:
        S = Sched(nc, es)

        tcount = [0]

        def T(name, shape, dt, st=es):
            tcount[0] += 1
            return st.enter_context(nc.sbuf_tensor(f"{name}_u{tcount[0]}", list(shape), dt))

        def P(name, shape, dt):
            return es.enter_context(nc.psum_tensor(name, list(shape), dt))

        hT = T("hT", [128, 8, S_LEN], BF16)
        Gt = T("Gt", [128, D], F32)
        sht = T("sht", [128, D], F32)
        gpt = T("gpt", [128, D], F32)
        ct = {}
        for k_, v_ in C.items():
            ct[k_] = T("k_" + k_, v_.shape, BF16 if v_.dtype == ml_dtypes.bfloat16 else F32)
            S.dma('sp', ct[k_][:], cd[k_], 'const', writes=['c_' + k_])
        ident = ct['ident']
        wbf = [T(f"wbf{i}", [128, 8, 512], BF16) for i in range(3)]
        wloaded = {}
        wf = [T(f"wf{i}", [128, 512], F32) for i in range(3)]
        wb = [T(f"wb{i}", [128, 512], BF16) for i in range(6)]
        wf_rot = Rot(list(range(3)))
        wb_rot = Rot(list(range(6)))
        qbp = [T(f"qbp{i}", [128, 256], BF16) for i in range(2)]
        qb_rot = Rot([0, 1])
        sm2 = [T(f"sz{i}", [128, 256], F32) for i in range(1)]
        sm2_rot = Rot([0])

        def SM2():
            i = sm2_rot()
            return sm2[i], f"sz{i}"
        sm = [T(f"sm{i}", [128, 64], F32) for i in range(8)]
        sm_rot = Rot(list(range(8)))
        pT = [P(f"pT{i}", [128, 8, 128], BF16) for i in range(2)]
        pR = [P(f"pR{i}", [128, 512], F32) for i in range(4)]
        pA = [P(f"pA{i}", [128, 512], F32) for i in range(2)]
        pT_rot = Rot([0, 1])
        pR_rot = Rot([0, 1, 2, 3])

        def WF():
            i = wf_rot()
            return wf[i], f"wf{i}"

        def WB():
            i = wb_rot()
            return wb[i], f"wb{i}"

        def SM():
            i = sm_rot()
            return sm[i], f"sm{i}"

        def PR():
            i = pR_rot()
            return pR[i], f"pR{i}"

        def PT():
            i = pT_rot()
            return pT[i], f"pT{i}"

        def wload(idx):
            if idx in wloaded or idx >= 8 * n_layers:
                return
            bi = idx % 3
            wt, wk = wbf[bi], f"wbf{bi}"
            src = wg_d[idx // 8, idx % 8].rearrange("(k p) c -> p k c", p=128)
            for half in range(2):
                S.dma('pool', wt[:, half * 4:(half + 1) * 4, :], src[:, half * 4:(half + 1) * 4, :], f"wl{bi}{half}", writes=[(wk, half)])
            wloaded[idx] = (wt, wk)

        def load_wgroup(l, g):
            idx = 8 * l + g
            wload(idx)
            return wloaded[idx]

        def proj_slot(slot, wt, wk, ncols=512):
            ps, pk = PR()

            def f(e):
                for k in range(8):
                    ins = e.matmul(ps[:, 0:ncols], hT[:, k, slot * 128:(slot + 1) * 128], wt[:, k, 0:ncols],
                                   start=(k == 0), stop=(k == 7))
                return ins
            S.op('pe', f, reads=[('hT', slot), (wk, 0), (wk, 1)], writes=[pk])
            return ps, pk

        def rope(src, srckey, dst, dstkey, nh, cc_ap, ss_ap, tabkeys):
            S.op('act', lambda e: e.copy(dst[:, :, 16:64], src[:, :, 16:64]), reads=[srckey], writes=[dstkey])
            ta, tak = SM()
            tb, tbk = SM()
            tav = ta[:, 0:nh * 16].rearrange("p (h c) -> p h c", c=16)
            tbv = tb[:, 0:nh * 16].rearrange("p (h c) -> p h c", c=16)
            S.op('dve', lambda e: e.tensor_tensor(tav, src[:, :, 0:16], cc_ap.unsqueeze(1).to_broadcast([128, nh, 16]), ALU.mult),
                 reads=[srckey] + tabkeys, writes=[tak])
            S.op('dve', lambda e: e.tensor_tensor(tbv[:, :, 0:8], src[:, :, 8:16], ss_ap[:, 0:8].unsqueeze(1).to_broadcast([128, nh, 8]), ALU.mult),
                 reads=[srckey] + tabkeys, writes=[tbk])
            S.op('dve', lambda e: e.tensor_tensor(tbv[:, :, 8:16], src[:, :, 0:8], ss_ap[:, 8:16].unsqueeze(1).to_broadcast([128, nh, 8]), ALU.mult),
                 reads=[srckey] + tabkeys, writes=[tbk])
            S.op('dve', lambda e: e.tensor_tensor(dst[:, :, 0:16], tav, tbv, ALU.add),
                 reads=[tak, tbk], writes=[dstkey])

        def run_pipeline(its, nst):
            n = len(its)
            for step in range(n + nst - 1):
                for k in range(nst):
                    t = step - k
                    if 0 <= t < n and its[t][k] is not None:
                        its[t][k]()

        for l in range(n_layers):
            xin = x_d if l == 0 else x1_d
            xin_key = 'xin' if l == 0 else 'x1'
            xout = out_d if l == n_layers - 1 else x1_d
            xout_key = 'out' if l == n_layers - 1 else 'x1'

            with ExitStack() as ps0:
                cTt = T("cTt", [128, 8], F32, ps0)
                sc = T("sc", [128, 8], F32, ps0)
                crep = T("crep", [128, 8, 128], F32, ps0)
                wm = [T(f"wm{i}", [128, 8, 512], F32, ps0) for i in range(2)]
                modb = T("modb", [128, 3 * D], F32, ps0)
                bmt = T("bmt", [128, 3 * D], F32, ps0)
                npre_t = T("npre_t", [128, D], F32, ps0)
                npost_t = T("npost_t", [128, D], F32, ps0)
                S.dma('sp', cTt[:], cT_d, 'p0a', writes=['cTt'])
                S.dma('sp', bmt[:], bmod_d[l], 'p0b', writes=['bmt'])
                S.dma('sp', npre_t[:], npre_d[l], 'p0c', writes=['npre_t'])
                S.dma('sp', npost_t[:], npost_d[l], 'p0d', writes=['npost_t'])
                S.op('act', lambda e: e.activation(sc[:], cTt[:], AF.Silu), reads=['cTt'], writes=['sc'])
                S.op('dve', lambda e: e.tensor_copy(crep[:], sc[:].unsqueeze(2).to_broadcast([128, 8, 128])), reads=['sc'], writes=['crep'])
                for cg in range(6):
                    wmt, wmk = wm[cg % 2], f"wm{cg % 2}"
                    S.dma('sp', wmt[:], wmod_d[l][:, cg * 512:(cg + 1) * 512].rearrange("(k p) c -> p k c", p=128), wmk, writes=[wmk])
                    ps, pk = PR()

                    def f(e, ps=ps, wmt=wmt):
                        for k in range(8):
                            ins = e.matmul(ps[:], crep[:, k, :], wmt[:, k, :], start=(k == 0), stop=(k == 7))
                        return ins
                    S.op('pe', f, reads=['crep', wmk], writes=[pk])
                    S.op('dve', lambda e, ps=ps, cg=cg: e.tensor_tensor(modb[:, cg * 512:(cg + 1) * 512], ps[:], bmt[:, cg * 512:(cg + 1) * 512], ALU.add),
                         reads=[pk, 'bmt'], writes=[('modb', cg)])
                S.op('act', lambda e: e.copy(sht[:], modb[:, 0:D]), reads=[('modb', 0), ('modb', 1)], writes=['sht'])
                S.op('dve', lambda e: e.scalar_tensor_tensor(out=Gt[:], in0=modb[:, D:2 * D], scalar=1.0, in1=npre_t[:], op0=ALU.add, op1=ALU.mult),
                     reads=[('modb', 2), ('modb', 3), 'npre_t'], writes=['Gt'])
                S.op('dve', lambda e: e.tensor_tensor(gpt[:], modb[:, 2 * D:3 * D], npost_t[:], ALU.mult),
                     reads=[('modb', 4), ('modb', 5), 'npost_t'], writes=['gpt'])
                S.barrier(drop_prefixes=('cTt', 'sc', 'crep', 'wm', 'modb', 'bmt', 'npre_t', 'npost_t'))

            with ExitStack() as ps1:
                xt = [T(f"xt{i}", [128, D], F32, ps1) for i in range(2)]
                junk = T("junk", [128, D], BF16, ps1)
                ht32 = T("ht32", [128, D], F32, ps1)
                hb = [T(f"hb{i}", [128, D], BF16, ps1) for i in range(2)]
                st1 = [T(f"st1_{i}", [128, 4], F32, ps1) for i in range(2)]
                def p1_iter(slot):
                    i2 = slot % 2
                    stt, stk = st1[i2], f"st1_{i2}"
                    c = {}

                    def s0():
                        S.dma('sp', xt[i2][:], xin[slot * 128:(slot + 1) * 128, :], f"xt{i2}", reads=[(xin_key, slot)], writes=[f"xt{i2}"])

                    def s1():
                        S.op('act', lambda e: e.activation(junk[:], xt[i2][:], AF.Square, accum_out=stt[:, 0:1]),
                             reads=[f"xt{i2}"], writes=['junk', (stk, 0)])
                        S.op('dve', lambda e: e.tensor_scalar(out=stt[:, 3:4], in0=stt[:, 0:1], scalar1=1.0 / D, scalar2=EPS, op0=ALU.mult, op1=ALU.add),
                             reads=[(stk, 0)], writes=[(stk, 3)])
                        S.op('act', lambda e: e.activation(stt[:, 1:2], stt[:, 3:4], AF.Sqrt), reads=[(stk, 3)], writes=[(stk, 1)])
                        S.op('dve', lambda e: e.reciprocal(stt[:, 2:3], stt[:, 1:2]), reads=[(stk, 1)], writes=[(stk, 2)])
                        S.op('dve', lambda e: e.scalar_tensor_tensor(out=ht32[:], in0=xt[i2][:], scalar=stt[:, 2:3], in1=Gt[:], op0=ALU.mult, op1=ALU.mult),
                             reads=[f"xt{i2}", (stk, 2), 'Gt'], writes=['ht32'])
                        S.op('pool', lambda e: e.tensor_tensor(hb[i2][:], ht32[:], sht[:], ALU.add),
                             reads=['ht32', 'sht'], writes=[f"hb{i2}"])

                    def s2():
                        pt, ptk = PT()
                        c['pt'], c['ptk'] = pt, ptk

                        def f(e):
                            for k in range(8):
                                ins = e.transpose(pt[:, k, :], hb[i2][:, k * 128:(k + 1) * 128], ident[:])
                            return ins
                        S.op('pe', f, reads=[f"hb{i2}", 'c_ident'], writes=[ptk])

                    def s3():
                        pt, ptk = c['pt'], c['ptk']
                        if slot % 2:
                            S.op('act', lambda e: e.copy(hT[:, :, slot * 128:(slot + 1) * 128], pt[:]), reads=[ptk], writes=[('hT', slot)])
                        else:
                            S.op('dve', lambda e: e.tensor_copy(hT[:, :, slot * 128:(slot + 1) * 128], pt[:]), reads=[ptk], writes=[('hT', slot)])
                    return [s0, s1, s2, s3]
                run_pipeline([p1_iter(slot) for slot in range(NSLOT)], 4)
                S.barrier(drop_prefixes=('xt', 'junk', 'ht32', 'hb', 'st1_'))

            def qk_transpose(src_bf, srckeys, dst_fn, dstkey, npair):
                pt, ptk = PT()

                def f(e):
                    for p in range(npair):
                        ins = e.transpose(pt[:, p, :], src_bf[:, p * 128:(p + 1) * 128], ident[:])
                    return ins
                S.op('pe', f, reads=list(srckeys) + ['c_ident'], writes=[ptk])
                return pt, ptk

            mixrr = [0]

            def store_mixed(m, col0, o_ap_fn, okeys, zt, ztk, i):
                mo, mok = WB()
                S.op('dve', lambda e: e.tensor_tensor(mo[:, 0:256], o_ap_fn(), zt[:, i, :], ALU.mult),
                     reads=list(okeys) + [(ztk, i)], writes=[mok])
                mixrr[0] += 1
                S.dma('pool', mixed_d[m * 128:(m + 1) * 128, col0:col0 + 256], mo[:, 0:256], f'mixst{mixrr[0] % 4}',
                      reads=[mok], writes=[('mixed', m, col0)])

            def qs_iters(wq, wqk, qg, qT2, qT2k, zt, ztk, do_rope, pairs=True, qcT=None):
                def one(i):
                    slot = qg * 4 + i
                    c = {}

                    def s0():
                        c['ps'], c['pk'] = proj_slot(slot, wq, wqk)

                    def s1():
                        ps, pk = c['ps'], c['pk']
                        ez, ezk = SM2()
                        S.op('act', lambda e: e.activation(ez[:], ps[:, 256:512], AF.Exp, scale=-1.0), reads=[pk], writes=[ezk])
                        S.op('dve', lambda e: e.tensor_scalar(out=ez[:], in0=ez[:], scalar1=1.0, scalar2=None, op0=ALU.add), reads=[ezk], writes=[ezk])
                        S.op('dve', lambda e: e.reciprocal(ez[:], ez[:]), reads=[ezk], writes=[ezk])
                        S.op('dve', lambda e: e.tensor_tensor(zt[:, i, :], ps[:, 256:512], ez[:], ALU.mult), reads=[pk, ezk], writes=[(ztk, i)])
                        qi = qb_rot()
                        qb_, qbk = qbp[qi], f"qbp{qi}"
                        c['qb'], c['qbk'] = qb_, qbk
                        if do_rope:
                            rope(ps[:, 0:256].rearrange("p (h c) -> p h c", c=64), pk,
                                 qb_[:, 0:256].rearrange("p (h c) -> p h c", c=64), qbk, 4,
                                 ct['rcc'][:, slot, :], ct['rss'][:, slot, :], ['c_rcc', 'c_rss'])
                        else:
                            S.op('dve', lambda e: e.tensor_copy(qb_[:, 0:256], ps[:, 0:256]), reads=[pk], writes=[qbk])

                    def s2():
                        qb_, qbk = c['qb'], c['qbk']
                        if pairs:
                            c['pt'], c['ptk'] = qk_transpose(qb_, [qbk], None, None, 2)
                        else:
                            pt, ptk = PT()
                            c['pt'], c['ptk'] = pt, ptk

                            def f(e):
                                for h in range(4):
                                    ins = e.transpose(pt[0:64, h, :], qb_[:, h * 64:(h + 1) * 64], ident[:])
                                return ins
                            S.op('pe', f, reads=[qbk, 'c_ident'], writes=[ptk])

                    def s3():
                        pt, ptk = c['pt'], c['ptk']
                        if pairs:
                            S.op('dve', lambda e: e.tensor_copy(qT2[0:64, :, i, 0, :], pt[0:64, 0:2, :]), reads=[ptk], writes=[(qT2k, i, 0)])
                            S.op('act', lambda e: e.copy(qT2[64:128, :, i, 1, :], pt[64:128, 0:2, :]), reads=[ptk], writes=[(qT2k, i, 1)])
                        else:
                            S.op('dve', lambda e: e.tensor_copy(qcT[:, i, :, :], pt[0:64, 0:4, :]), reads=[ptk], writes=[('qcT', i)])
                    return [s0, s1, s2, s3]
                return [one(i) for i in range(4)]

            def interleave(its, extra):
                its = [it + [None] * (4 - len(it)) for it in its]
                if not extra:
                    return its
                n = len(its)
                out = []
                pos = [((j + 1) * n) // (len(extra) + 1) for j in range(len(extra))]
                j = 0
                for t, it in enumerate(its):
                    while j < len(extra) and pos[j] == t:
                        out.append(extra[j])
                        j += 1
                    out.append(it)
                out.extend(extra[j:])
                return out

            def s_pairs(kT, kTk, kb, qT2, qT2k, i):
                ps, pk = PR()

                def f(e):
                    for p in range(2):
                        ins = e.matmul(ps[:, p * 256:(p + 1) * 256], kT[:, p, kb * 128:(kb + 1) * 128],
                                       qT2[:, p, i, :, :].rearrange("p a b -> p (a b)"), start=True, stop=True)
                    return ins
                S.op('pe', f, reads=[(kTk, kb), (qT2k, i, 0), (qT2k, i, 1)], writes=[pk])
                return ps, pk

            def av(acc, acck, pb, pbk, v_fn, vkey, first, last, width):
                def f(e):
                    for h in range(4):
                        ins = e.matmul(acc[:, h * width:(h + 1) * width], pb[:, h * 128:(h + 1) * 128], v_fn(h), start=(first and h == 0), stop=last,
                                       skip_group_check=True)
                    return ins
                S.op('pe', f, reads=[pbk] + (vkey if isinstance(vkey, list) else [vkey]), writes=[acck])

            def normalize(acc, acck, width=65):
                accv = acc[:, 0:4 * width].rearrange("p (h c) -> p h c", c=width)
                rd, rdk = SM()
                S.op('dve', lambda e: e.reciprocal(rd[:, 0:4], accv[:, :, 64]), reads=[acck], writes=[rdk])
                o, ok = WF()
                S.op('dve', lambda e: e.tensor_tensor(o[:, 0:256].rearrange("p (h c) -> p h c", c=64), accv[:, :, 0:64],
                                                      rd[:, 0:4].unsqueeze(2).to_broadcast([128, 4, 64]), ALU.mult),
                     reads=[acck, rdk], writes=[ok])
                return o, ok

            if 'A' in mixers:
                with ExitStack() as ma:
                    kaT = T("kaT", [128, 2, S_LEN], BF16, ma)
                    va = T("va", [128, NSLOT, 256], BF16, ma)
                    qT2s = [T(f"qT2a{b_}", [128, 2, 4, 2, 128], BF16, ma) for b_ in range(2)]
                    zts = [T(f"zta{b_}", [128, 4, 256], BF16, ma) for b_ in range(2)]
                    for b_ in range(2):
                        S.op('pool', lambda e, b_=b_: e.memset(qT2s[b_][:].rearrange("p a b c d -> p (a b c d)"), 0.0), writes=[(f'qT2_{b_}', i, j) for i in range(4) for j in range(2)])
                    wk_, wkk = load_wgroup(l, 0)
                    wq_, wqk = load_wgroup(l, 1)
                    wload(8 * l + 2)
                    def a_k_iter(slot):
                        c = {}

                        def s0():
                            c['ps'], c['pk'] = proj_slot(slot, wk_, wkk)

                        def s1():
                            ps, pk = c['ps'], c['pk']
                            S.op('act', lambda e: e.copy(va[:, slot, :], ps[:, 256:512]), reads=[pk], writes=[('va', slot)])
                            c['kb'], c['kbk'] = WB()
                            S.op('dve', lambda e: e.tensor_copy(c['kb'][:, 0:256], ps[:, 0:256]), reads=[pk], writes=[c['kbk']])

                        def s2():
                            c['pt'], c['ptk'] = qk_transpose(c['kb'], [c['kbk']], None, None, 2)

                        def s3():
                            S.op('dve', lambda e: e.tensor_copy(kaT[:, :, slot * 128:(slot + 1) * 128], c['pt'][:, 0:2, :]),
                                 reads=[c['ptk']], writes=[('kaT', slot)])
                        return [s0, s1, s2, s3]
                    run_pipeline([a_k_iter(slot) for slot in range(NSLOT)], 4)
                    wload(8 * l + 3)
                    wa = [T(f"wa{j}", [128, 512], BF16, ma) for j in range(16)]
                    wa_rot = Rot(list(range(16)))

                    def WA():
                        j = wa_rot()
                        return wa[j], f"wa{j}"

                    def a_iter(m, i, kb, st, qT2, qT2k, zt, ztk):
                        c = {}
                        acc, acck = pA[m % 2], f"pA{m % 2}"
                        diag = (kb == m)

                        def s0():
                            c['ps'], c['pk'] = s_pairs(kaT, 'kaT', kb, qT2, qT2k, i)

                        def s1():
                            ps, pk = c['ps'], c['pk']
                            et, etk = WA()
                            c['et'], c['etk'] = et, etk
                            S.op('act', lambda e: e.activation(et[:], ps[:], AF.Exp, scale=SCALE), reads=[pk], writes=[etk])
                            spb, spbk = WA()
                            S.op('act', lambda e: e.activation(spb[:], et[:], AF.Ln, bias=1.0), reads=[etk], writes=[spbk])
                            if diag:
                                S.op('dve', lambda e: e.tensor_tensor(
                                    spb[:].rearrange("p (h c) -> p h c", c=128), spb[:].rearrange("p (h c) -> p h c", c=128),
                                    ct['triS'][:].unsqueeze(1).to_broadcast([128, 4, 128]), ALU.mult),
                                    reads=[spbk, 'c_triS'], writes=[spbk])
                            bw, bwk = PR()
                            c['bw'], c['bwk'] = bw, bwk
                            lsum, lsumk = st['lsum'], st['lsumk']

                            def fb(e):
                                ins = e.matmul(bw[:], ct['umat'][:], spb[:], start=True, stop=diag)
                                if not diag:
                                    ins = e.matmul(bw[:], ct['ones'][:], lsum[:], start=False, stop=True)
                                return ins
                            S.op('pe', fb, reads=[spbk, 'c_umat', 'c_ones'] + ([lsumk] if not diag else []), writes=[bwk])
                            if kb > 0:
                                if diag:
                                    st['lsum'], st['lsumk'] = spb, spbk
                                else:
                                    nl_, nlk = WA()
                                    S.op('pool', lambda e: e.tensor_tensor(nl_[:], lsum[:], spb[:], ALU.add), reads=[lsumk, spbk], writes=[nlk])
                                    st['lsum'], st['lsumk'] = nl_, nlk

                        def s2():
                            et, etk, bw, bwk = c['et'], c['etk'], c['bw'], c['bwk']
                            xt_, xtk = WA()
                            S.op('act', lambda e: e.activation(xt_[:], bw[:], AF.Exp, scale=-1.0), reads=[bwk], writes=[xtk])
                            ab_, abk = WA()
                            S.op('dve', lambda e: e.tensor_tensor(ab_[:], et[:], xt_[:], ALU.mult), reads=[etk, xtk], writes=[abk])
                            if diag:
                                S.op('dve', lambda e: e.tensor_tensor(
                                    ab_[:].rearrange("p (h c) -> p h c", c=128), ab_[:].rearrange("p (h c) -> p h c", c=128),
                                    ct['triS'][:].unsqueeze(1).to_broadcast([128, 4, 128]), ALU.mult),
                                    reads=[abk, 'c_triS'], writes=[abk])
                            av(acc, acck, ab_, abk, lambda h: va[:, kb, h * 64:(h + 1) * 64], ('va', kb), diag, kb == 0, 64)
                            if kb == 0:
                                store_mixed(m, 0, lambda: acc[:, 0:256], [acck], zt, ztk, i)
                        return [s0, s1, s2]
                    run_pipeline(qs_iters(wq_, wqk, 0, qT2s[0], 'qT2_0', zts[0], 'zta_0', do_rope=False), 4)
                    for qg in range(nq_groups):
                        b_ = qg % 2
                        st = {'lsum': None, 'lsumk': None}
                        its = []
                        for i in range(4):
                            m = qg * 4 + i
                            for kb in range(m, -1, -1):
                                its.append(a_iter(m, i, kb, st, qT2s[b_], f'qT2_{b_}', zts[b_], f'zta_{b_}'))
                        extra = qs_iters(wq_, wqk, qg + 1, qT2s[1 - b_], f'qT2_{1 - b_}', zts[1 - b_], f'zta_{1 - b_}', do_rope=False) if qg + 1 < nq_groups else []
                        run_pipeline(interleave(its, extra), 4)
                    S.barrier(drop_prefixes=('kaT', 'va', 'qT2', 'zta', 'wa'))

            if 'B' in mixers:
                with ExitStack() as mbs:
                    kT = T("kbT", [128, 2, S_LEN], BF16, mbs)
                    vaug = T("vbaug", [128, NSLOT, 4, 65], BF16, mbs)
                    qT2s = [T(f"qT2b{b_}", [128, 2, 4, 2, 128], BF16, mbs) for b_ in range(2)]
                    zts = [T(f"ztb{b_}", [128, 4, 256], BF16, mbs) for b_ in range(2)]
                    for b_ in range(2):
                        S.op('pool', lambda e, b_=b_: e.memset(qT2s[b_][:].rearrange("p a b c d -> p (a b c d)"), 0.0), writes=[(f'qT2_{b_}', i, j) for i in range(4) for j in range(2)])
                    S.op('pool', lambda e: e.memset(vaug[:].rearrange("p a b c -> p (a b c)"), 1.0), writes=[('vaug', s_) for s_ in range(NSLOT)])
                    wk_, wkk = load_wgroup(l, 2)
                    wq_, wqk = load_wgroup(l, 3)
                    wload(8 * l + 4)
                    def rk_iter(slot, kT_, kTkey, vaug_, wk__, wkk__):
                        c = {}

                        def s0():
                            c['ps'], c['pk'] = proj_slot(slot, wk__, wkk__)

                        def s1():
                            ps, pk = c['ps'], c['pk']
                            S.op('act', lambda e: e.copy(vaug_[:, slot, :, 0:64], ps[:, 256:512].rearrange("p (h c) -> p h c", c=64)),
                                 reads=[pk], writes=[('vaug', slot)])
                            c['kb'], c['kbk'] = WB()
                            rope(ps[:, 0:256].rearrange("p (h c) -> p h c", c=64), pk,
                                 c['kb'][:, 0:256].rearrange("p (h c) -> p h c", c=64), c['kbk'], 4,
                                 ct['rcc'][:, slot, :], ct['rss'][:, slot, :], ['c_rcc', 'c_rss'])

                        def s2():
                            c['pt'], c['ptk'] = qk_transpose(c['kb'], [c['kbk']], None, None, 2)

                        def s3():
                            S.op('dve', lambda e: e.tensor_copy(kT_[:, :, slot * 128:(slot + 1) * 128], c['pt'][:, 0:2, :]),
                                 reads=[c['ptk']], writes=[(kTkey, slot)])
                        return [s0, s1, s2, s3]
                    run_pipeline([rk_iter(slot, kT, 'kT', vaug, wk_, wkk) for slot in range(NSLOT)], 4)
                    wload(8 * l + 5)
                    def b_iter(m, i, d, dmax, qT2, qT2k, zt, ztk):
                        c = {}
                        acc, acck = pA[m % 2], f"pA{m % 2}"
                        kb = m - d

                        def s0():
                            ps, pk = PR()
                            c['ps'], c['pk'] = ps, pk

                            def f(e):
                                for p in range(2):
                                    e.matmul(ps[:, p * 256:(p + 1) * 256], kT[:, p, kb * 128:(kb + 1) * 128],
                                             qT2[:, p, i, :, :].rearrange("p a b -> p (a b)"), start=(p == 0), stop=False, skip_group_check=True)
                                for h in range(4):
                                    ins = e.matmul(ps[:, h * 128:(h + 1) * 128], ident[:], ct['lmbh'][:, d, :], start=False, stop=(d > 4 and h == 3), skip_group_check=True)
                                if d <= 4:
                                    for h in range(4):
                                        ins = e.matmul(ps[:, h * 128:(h + 1) * 128], ident[:], ct['lmbl'][:, d, :], start=False, stop=(h == 3), skip_group_check=True)
                                return ins
                            S.op('pe', f, reads=[('kT', kb), (qT2k, i, 0), (qT2k, i, 1), 'c_ident', 'c_lmbh', 'c_lmbl'], writes=[pk])

                        def s1():
                            c['pb'], c['pbk'] = WB()
                            S.op('act', lambda e: e.activation(c['pb'][:], c['ps'][:], AF.Exp, scale=SCALE), reads=[c['pk']], writes=[c['pbk']])

                        def s2():
                            av(acc, acck, c['pb'], c['pbk'], lambda h: vaug[:, kb, h, :], ('vaug', kb), d == 0, d == dmax, 65)
                            if d == dmax:
                                o, ok = normalize(acc, acck)
                                store_mixed(m, 256, lambda: o[:, 0:256], [ok], zt, ztk, i)
                        return [s0, s1, s2]
                    run_pipeline(qs_iters(wq_, wqk, 0, qT2s[0], 'qT2_0', zts[0], 'ztb_0', do_rope=True), 4)
                    for qg in range(nq_groups):
                        b_ = qg % 2
                        its = []
                        for i in range(4):
                            m = qg * 4 + i
                            dmax = min(16, m)
                            for d in range(0, dmax + 1):
                                its.append(b_iter(m, i, d, dmax, qT2s[b_], f'qT2_{b_}', zts[b_], f'ztb_{b_}'))
                        extra = qs_iters(wq_, wqk, qg + 1, qT2s[1 - b_], f'qT2_{1 - b_}', zts[1 - b_], f'ztb_{1 - b_}', do_rope=True) if qg + 1 < nq_groups else []
                        run_pipeline(interleave(its, extra), 4)
                    S.barrier(drop_prefixes=('kT', 'vaug', 'qT2', 'ztb'))

            if 'C' in mixers:
                with ExitStack() as mcs:
                    kcrT = T("kcrT", [64, S_LEN], BF16, mcs)
                    vcrT = T("vcrT", [64, S_LEN], BF16, mcs)
                    kvT = [kcrT, vcrT]
                    ksT = T("ksT", [64, S_LEN], BF16, mcs)
                    kwT = T("kwT", [64, S_LEN], BF16, mcs)
                    vsa = T("vsa", [128, NSLOT, 65], BF16, mcs)
                    vwa = T("vwa", [128, NSLOT, 65], BF16, mcs)
                    gsig = T("gsig", [128, NSLOT, 12], F32, mcs)
                    kcT = T("kcT", [64, 256], BF16, mcs)
                    vcE = T("vcE", [128, 2, 128], BF16, mcs)
                    qcT = T("qcT", [64, 4, 4, 128], BF16, mcs)
                    zt = T("ztc", [128, 4, 256], BF16, mcs)
                    w1bs = [T(f"w1b{j}", [64, 2, 8, 256], BF16, mcs) for j in range(2)]
                    w2b = T("w2b", [128, 2, 2, 64], BF16, mcs)
                    b1t = T("b1t", [128, 4], F32, mcs)
                    posb = T("posb", [64, 2, 32], BF16, mcs)
                    biasv = T("biasv", [128, 4], F32, mcs)
                    hidT = T("hidT", [128, 4, 256], BF16, mcs)
                    ocacc = T("ocacc", [128, 256], F32, mcs)
                    impt = T("impt", [128, 64], F32, mcs)
                    imps = T("imps", [128, 64], F32, mcs)
                    selm = T("selm", [128, 64], BF16, mcs)
                    mx8 = T("mx8", [128, 16], F32, mcs)
                    cf = T("cf", [128, 16], F32, mcs)
                    S.op('pool', lambda e: e.memset(vsa[:].rearrange("p a b -> p (a b)"), 1.0), writes=[('vsa', s_) for s_ in range(NSLOT)])
                    S.op('pool', lambda e: e.memset(vwa[:].rearrange("p a b -> p (a b)"), 1.0), writes=[('vwa', s_) for s_ in range(NSLOT)])
                    wk_, wkk = load_wgroup(l, 4)
                    wq_, wqk = load_wgroup(l, 5)
                    wload(8 * l + 6)
                    for t in range(2):
                        S.dma('pool', w2b[:, t, :, :], w2_d[l, t].rearrange("(c p) d -> p c d", p=128), f'cw{t}', writes=['w2b'])
                    S.dma('sp', b1t[:], b1_d[l], 'cw2', writes=['b1t'])
                    S.dma('pool', posb[:], posT_d[l], 'cw3', writes=['posb'])
                    S.dma('sp', vcE[:, :, 64:128], cd['ovl'], 'cw4', writes=[('vcE', 'ov')])
                    def ck_iter(slot):
                        c = {}
                        tsl = slice(slot * 128, (slot + 1) * 128)

                        def s0():
                            c['ps'], c['pk'] = proj_slot(slot, wk_, wkk, ncols=396)

                        def s1():
                            ps, pk = c['ps'], c['pk']
                            S.op('act', lambda e: e.copy(vsa[:, slot, 0:64], ps[:, 256:320]), reads=[pk], writes=[('vsa', slot)])
                            S.op('act', lambda e: e.copy(vwa[:, slot, 0:64], ps[:, 320:384]), reads=[pk], writes=[('vwa', slot)])
                            S.op('act', lambda e: e.activation(gsig[:, slot, :], ps[:, 384:396], AF.Sigmoid), reads=[pk], writes=[('gsig', slot)])
                            kb_, kbk = WB()
                            c['kb'], c['kbk'] = kb_, kbk
                            S.op('dve', lambda e: e.tensor_copy(kb_[:, 0:128], ps[:, 0:128]), reads=[pk], writes=[kbk])
                            rope(ps[:, 128:256].rearrange("p (h c) -> p h c", c=64), pk,
                                 kb_[:, 128:256].rearrange("p (h c) -> p h c", c=64), kbk, 2,
                                 ct['rcc'][:, slot, :], ct['rss'][:, slot, :], ['c_rcc', 'c_rss'])

                        def s2():
                            kb_, kbk = c['kb'], c['kbk']
                            pt, ptk = PT()
                            c['pt'], c['ptk'] = pt, ptk

                            def f(e):
                                e.transpose(pt[0:64, 0, :], kb_[:, 0:64], ident[:])
                                e.transpose(pt[0:64, 3, :], kb_[:, 64:128], ident[:])
                                e.transpose(pt[0:64, 1, :], kb_[:, 128:192], ident[:])
                                return e.transpose(pt[0:64, 2, :], kb_[:, 192:256], ident[:])
                            S.op('pe', f, reads=[kbk, 'c_ident'], writes=[ptk])

                        def s3():
                            pt, ptk = c['pt'], c['ptk']
                            S.op('dve', lambda e: e.tensor_copy(kcrT[:, tsl], pt[0:64, 0, :]), reads=[ptk], writes=[('kcrT', slot)])
                            S.op('act', lambda e: e.copy(vcrT[:, tsl], pt[0:64, 3, :]), reads=[ptk], writes=[('vcrT', slot)])
                            S.op('act', lambda e: e.copy(ksT[:, tsl], pt[0:64, 1, :]), reads=[ptk], writes=[('ksT', slot)])
                            S.op('dve', lambda e: e.tensor_copy(kwT[:, tsl], pt[0:64, 2, :]), reads=[ptk], writes=[('kwT', slot)])
                        return [s0, s1, s2, s3]
                    run_pipeline([ck_iter(slot) for slot in range(NSLOT)], 4)
                    wload(8 * l + 7)
                    allkv = [[('kcrT', s_) for s_ in range(NSLOT)], [('vcrT', s_) for s_ in range(NSLOT)]]
                    bps, bpk = PR()
                    kv3 = [kvT[t][:].rearrange("p (n r) -> p n r", r=16) for t in range(2)]
                    for piece in range(4):
                        w1b = w1bs[piece % 2]
                        w1k = f"w1b{piece % 2}"
                        for t in range(2):
                            S.dma('pool', w1b[:, t, :, :], w1_d[l, t, piece * 512:(piece + 1) * 512, :].rearrange("(l d) c -> d l c", d=64),
                                  f"w1l{piece % 2}{t}", writes=[(w1k, t)])
                        for t in range(2):
                            def f(e, t=t, piece=piece, w1b=w1b):
                                ins = None
                                for li in range(8):
                                    lg = piece * 8 + li
                                    for c in range(2):
                                        o_ = pA[t][:, c * 256:(c + 1) * 256]
                                        w_ = w1b[:, t, li, c * 128:(c + 1) * 128]
                                        first = (lg == 0 and c == 0)
                                        last = (lg == 31)
                                        if lg < 16:
                                            ins = e.matmul(o_, w_, kv3[t][:, 0:256, lg], start=first, stop=False, skip_group_check=True)
                                        else:
                                            ins = e.matmul(o_[:, 0:255], w_, kv3[t][:, 1:256, lg - 16], start=False, stop=last, skip_group_check=True)
                                        ins = e.matmul(bps[:, (t * 2 + c):(t * 2 + c) + 1], w_, posb[:, t, lg:lg + 1],
                                                       start=(t == 0 and first), stop=last, skip_group_check=True)
                                return ins
                            S.op('pe', f, reads=allkv[t] + [(w1k, t), 'posb'], writes=[f"pA{t}", bpk])
                    S.op('dve', lambda e: e.tensor_tensor(biasv[:], bps[:, 0:4], b1t[:], ALU.add), reads=[bpk, 'b1t'], writes=['biasv'])
                    for t in range(2):
                        for c in range(2):
                            j = t * 2 + c
                            (xs_, xsk), (x2_, x2k), (u__, uk) = WF(), WF(), WF()
                            xs, x2, u_ = xs_[:, 0:256], x2_[:, 0:256], u__[:, 0:256]
                            S.op('dve', lambda e, t=t, c=c, j=j, xs=xs: e.tensor_scalar(out=xs, in0=pA[t][:, c * 256:(c + 1) * 256], scalar1=biasv[:, j:j + 1], scalar2=None, op0=ALU.add),
                                 reads=[f"pA{t}", 'biasv'], writes=[xsk])
                            S.op('dve', lambda e, xs=xs, x2=x2: e.tensor_tensor(x2, xs, xs, ALU.mult), reads=[xsk], writes=[x2k])
                            S.op('dve', lambda e, x2=x2: e.tensor_scalar(out=x2, in0=x2, scalar1=0.044715, scalar2=1.0, op0=ALU.mult, op1=ALU.add), reads=[x2k], writes=[x2k])
                            S.op('dve', lambda e, xs=xs, x2=x2, u_=u_: e.tensor_tensor(u_, x2, xs, ALU.mult), reads=[x2k, xsk], writes=[uk])
                            S.op('act', lambda e, u_=u_: e.activation(u_, u_, AF.Sigmoid, scale=1.5957691216057308), reads=[uk], writes=[uk])
                            S.op('dve', lambda e, j=j, xs=xs, u_=u_: e.tensor_tensor(hidT[:, j, :], xs, u_, ALU.mult), reads=[xsk, uk], writes=[('hidT', j)])
                    for nchunk in range(2):
                        ps, pk = PR()

                        def f(e, ps=ps, nchunk=nchunk):
                            for t in range(2):
                                for c in range(2):
                                    ins = e.matmul(ps[:, t * 64:(t + 1) * 64], hidT[:, t * 2 + c, nchunk * 128:(nchunk + 1) * 128], w2b[:, t, c, :],
                                                   start=(c == 0), stop=(c == 1))
                            return ins
                        S.op('pe', f, reads=[('hidT', j) for j in range(4)] + ['w2b'], writes=[pk])
                        S.op('act', lambda e, ps=ps, nchunk=nchunk: e.copy(vcE[:, nchunk, 0:64], ps[:, 64:128]), reads=[pk], writes=[('vcE', nchunk)])
                        kb_, kbk = WB()
                        rope(ps[:, 0:64].rearrange("p (h c) -> p h c", c=64), pk,
                             kb_[:, 0:64].rearrange("p (h c) -> p h c", c=64), kbk, 1,
                             ct['rccc'][:, nchunk, :], ct['rssc'][:, nchunk, :], ['c_rccc', 'c_rssc'])
                        pt, ptk = PT()
                        S.op('pe', lambda e, pt=pt, kb_=kb_: e.transpose(pt[0:64, 0, :], kb_[:, 0:64], ident[:]),
                             reads=[kbk, 'c_ident'], writes=[ptk])
                        S.op('dve', lambda e, pt=pt, nchunk=nchunk: e.tensor_copy(kcT[:, nchunk * 128:(nchunk + 1) * 128], pt[0:64, 0, :]),
                             reads=[ptk], writes=[('kcT', nchunk)])
                    for qg in range(nq_groups):
                        run_pipeline(qs_iters(wq_, wqk, qg, None, None, zt, 'ztc', do_rope=True, pairs=False, qcT=qcT), 4)
                        for i in range(4):
                            m = qg * 4 + i
                            qv = qcT[:, i, :, :].rearrange("p h q -> p (h q)")
                            acc, acck = pA[0], 'pA0'
                            nch = 1 if m < 16 else 2
                            for c in range(nch):
                                ps, pk = PR()
                                S.op('pe', lambda e, ps=ps, c=c, qv=qv: e.matmul(ps[:], kcT[:, c * 128:(c + 1) * 128], qv, start=True, stop=True),
                                     reads=[('kcT', c), ('qcT', i)], writes=[pk])
                                pb, pbk = WB()
                                S.op('act', lambda e, pb=pb, ps=ps: e.activation(pb[:], ps[:], AF.Exp, scale=SCALE), reads=[pk], writes=[pbk])
                                u = m - 16 * c
                                if u < 17:
                                    S.op('dve', lambda e, pb=pb, u=u: e.tensor_tensor(
                                        pb[:].rearrange("p (h c) -> p h c", c=128), pb[:].rearrange("p (h c) -> p h c", c=128),
                                        ct['cv'][:, u, :].unsqueeze(1).to_broadcast([128, 4, 128]), ALU.mult),
                                        reads=[pbk, 'c_cv'], writes=[pbk])
                                av(acc, acck, pb, pbk, lambda h, c=c: vcE[:, c, :], [('vcE', c), ('vcE', 'ov')], c == 0, c == nch - 1, 128)
                            accv = acc[:].rearrange("p (h c) -> p h c", c=128)
                            S.op('dve', lambda e, accv=accv: e.tensor_reduce(out=cf[:, 0:4], in_=accv[:, :, 64:128], axis=AX.X, op=ALU.add), reads=[acck, ('vcE', 'ov')], writes=[('cf', 0)])
                            S.op('dve', lambda e: e.tensor_scalar(out=cf[:, 0:4], in0=cf[:, 0:4], scalar1=1e-30, scalar2=None, op0=ALU.max), reads=[('cf', 0)], writes=[('cf', 0)])
                            S.op('dve', lambda e: e.reciprocal(cf[:, 4:8], cf[:, 0:4]), reads=[('cf', 0)], writes=[('cf', 1)])
                            for h in range(4):
                                if h == 0:
                                    S.op('dve', lambda e, accv=accv: e.tensor_scalar(out=impt[:], in0=accv[:, 0, 64:128], scalar1=cf[:, 4:5], scalar2=None, op0=ALU.mult),
                                         reads=[acck, ('cf', 1)], writes=['impt'])
                                else:
                                    S.op('dve', lambda e, accv=accv, h=h: e.scalar_tensor_tensor(out=impt[:], in0=accv[:, h, 64:128], scalar=cf[:, 4 + h:5 + h], in1=impt[:], op0=ALU.mult, op1=ALU.add),
                                         reads=[acck, ('cf', 1), 'impt'], writes=['impt'])
                            gv = gsig[:, m, :].rearrange("p (h b) -> p h b", b=3)
                            S.op('dve', lambda e, gv=gv: e.tensor_tensor(cf[:, 8:12], cf[:, 4:8], gv[:, :, 0], ALU.mult), reads=[('cf', 1), ('gsig', m)], writes=[('cf', 2)])
                            ocv = ocacc[:].rearrange("p (h c) -> p h c", c=64)
                            S.op('dve', lambda e, accv=accv, ocv=ocv: e.tensor_tensor(ocv, accv[:, :, 0:64], cf[:, 8:12].unsqueeze(2).to_broadcast([128, 4, 64]), ALU.mult),
                                 reads=[acck, ('cf', 2)], writes=['ocacc'])
                            vsl = ct['vt'][:, 64 - 2 * m:128 - 2 * m]
                            if m <= 7:
                                S.op('dve', lambda e, vsl=vsl: e.tensor_copy(selm[:], vsl), reads=['c_vt'], writes=['selm'])
                            else:
                                t1s = ct['t1'][:, 64 - 2 * m:128 - 2 * m]
                                t2s = ct['t2'][:, 64 - 2 * m:128 - 2 * m]
                                S.op('dve', lambda e, t1s=t1s: e.tensor_tensor(imps[:], impt[:], t1s, ALU.mult), reads=['impt', 'c_t1'], writes=['imps'])
                                S.op('dve', lambda e, t2s=t2s: e.tensor_tensor(imps[:], imps[:], t2s, ALU.add), reads=['imps', 'c_t2'], writes=['imps'])
                                S.op('dve', lambda e: e.memset(imps[:, 0:1], BIG), reads=['imps'], writes=['imps'])
                                S.op('dve', lambda e: e.max(out=mx8[:, 0:8], in_=imps[:]), reads=['imps'], writes=[('mx8', 0)])
                                S.op('dve', lambda e: e.match_replace(out=impt[:], in_to_replace=mx8[:, 0:8], in_values=imps[:], imm_value=-3e38),
                                     reads=[('mx8', 0), 'imps'], writes=['impt'])
                                S.op('dve', lambda e: e.max(out=mx8[:, 8:16], in_=impt[:]), reads=['impt'], writes=[('mx8', 1)])
                                S.op('dve', lambda e: e.tensor_scalar(out=imps[:], in0=imps[:], scalar1=mx8[:, 15:16], scalar2=None, op0=ALU.is_ge),
                                     reads=['imps', ('mx8', 1)], writes=['imps'])
                                S.op('dve', lambda e, vsl=vsl: e.tensor_tensor(selm[:], imps[:], vsl, ALU.mult), reads=['imps', 'c_vt'], writes=['selm'])
                            def c_fin(acc, acck, gcol, m=m, gv=gv):
                                accv = acc[:, 0:260].rearrange("p (h c) -> p h c", c=65)
                                S.op('dve', lambda e: e.reciprocal(cf[:, 12:16], accv[:, :, 64]), reads=[acck], writes=[('cf', 3)])
                                S.op('dve', lambda e: e.tensor_tensor(cf[:, 12:16], cf[:, 12:16], gv[:, :, gcol], ALU.mult), reads=[('cf', 3), ('gsig', m)], writes=[('cf', 3)])
                                tmpo, tmpk = WF()
                                S.op('dve', lambda e: e.tensor_tensor(tmpo[:, 0:256].rearrange("p (h c) -> p h c", c=64), accv[:, :, 0:64],
                                                                      cf[:, 12:16].unsqueeze(2).to_broadcast([128, 4, 64]), ALU.mult),
                                     reads=[acck, ('cf', 3)], writes=[tmpk])
                                S.op('pool', lambda e: e.tensor_tensor(ocacc[:], ocacc[:], tmpo[:, 0:256], ALU.add), reads=['ocacc', tmpk], writes=['ocacc'])

                            def c_iter(kind, kb, first, last, m=m, i=i, qv=qv, c_fin=c_fin):
                                c = {}
                                sel = (kind == 'sel')
                                acc, acck = (pA[1], 'pA1') if sel else (pA[0], 'pA0')
                                kT_, kTk = (ksT, 'ksT') if sel else (kwT, 'kwT')
                                vA_, vAk = (vsa, 'vsa') if sel else (vwa, 'vwa')
                                d = m - kb
                                bias = None
                                if (sel and kb == m) or ((not sel) and d == 0):
                                    bias = 'ntri4'
                                elif (not sel) and d == 4:
                                    bias = 'nw44'

                                def s0():
                                    ps, pk = PR()
                                    c['ps'], c['pk'] = ps, pk

                                    def f(e):
                                        ins = e.matmul(ps[:], kT_[:, kb * 128:(kb + 1) * 128], qv, start=True, stop=not bias)
                                        if bias:
                                            ins = e.matmul(ps[:], ident[:], ct[bias][:], start=False, stop=True)
                                        return ins
                                    S.op('pe', f, reads=[(kTk, kb), ('qcT', i), 'c_ident'] + (['c_' + bias] if bias else []), writes=[pk])
                                    if sel:
                                        mp, mpk = PR()
                                        c['mp'], c['mpk'] = mp, mpk

                                        def fm(e):
                                            e.matmul(mp[0:64, 0:128], selm[:, 2 * kb:2 * kb + 1].to_broadcast([128, 64]), ident[:], start=True, stop=True)
                                            return e.matmul(mp[64:128, 0:128], selm[:, 2 * kb + 1:2 * kb + 2].to_broadcast([128, 64]), ident[:], start=True, stop=True)
                                        S.op('pe', fm, reads=['selm', 'c_ident'], writes=[mpk])

                                def s1():
                                    c['pb'], c['pbk'] = WB()
                                    pb, pbk = c['pb'], c['pbk']
                                    S.op('act', lambda e: e.activation(pb[:], c['ps'][:], AF.Exp, scale=SCALE), reads=[c['pk']], writes=[pbk])
                                    if sel:
                                        S.op('dve', lambda e: e.tensor_tensor(
                                            pb[:].rearrange("p (h c) -> p h c", c=128), pb[:].rearrange("p (h c) -> p h c", c=128),
                                            c['mp'][:, 0:128].unsqueeze(1).to_broadcast([128, 4, 128]), ALU.mult),
                                            reads=[pbk, c['mpk']], writes=[pbk])

                                def s2():
                                    av(acc, acck, c['pb'], c['pbk'], lambda h: vA_[:, kb, :], (vAk, kb), first, last, 65)
                                    if last:
                                        c_fin(acc, acck, 1 if sel else 2)
                                        if not sel:
                                            store_mixed(m, 512, lambda: ocacc[:], ['ocacc'], zt, 'ztc', i)
                                return [s0, s1, s2]
                            its = [c_iter('sel', kb, kb == 0, kb == m) for kb in range(0, m + 1)]
                            dmax = min(4, m)
                            its += [c_iter('win', m - d, d == 0, d == dmax) for d in range(0, dmax + 1)]
                            run_pipeline(its, 3)
                    S.barrier(drop_prefixes=('kcrT', 'vcrT', 'ksT', 'kwT', 'vsa', 'vwa', 'gsig', 'kcT', 'vcE', 'qcT', 'ztc', 'w1b', 'w2', 'b1t', 'pos',
                                             'biasv', 'hidT', 'gx', 'ocacc', 'imp', 'selm', 'selb', 'mx8', 'cf'))

            if 'D' in mixers:
                with ExitStack() as mds:
                    kT = T("kdT", [128, 2, S_LEN], BF16, mds)
                    vaug = T("vdaug", [128, NSLOT, 4, 65], BF16, mds)
                    qT2s = [T(f"qT2d{b_}", [128, 2, 4, 2, 128], BF16, mds) for b_ in range(2)]
                    zts = [T(f"ztd{b_}", [128, 4, 256], BF16, mds) for b_ in range(2)]
                    kmf = T("kmf", [128, 2, 16], F32, mds)
                    kmb = T("kmb", [128, 2, 16], BF16, mds)
                    gm = T("gm", [128, 4, 16], F32, mds)
                    gmx = T("gmx", [128, 4, 8], F32, mds)
                    isel = T("isel", [128, 4, 16], BF16, mds)
                    for b_ in range(2):
                        S.op('pool', lambda e, b_=b_: e.memset(qT2s[b_][:].rearrange("p a b c d -> p (a b c d)"), 0.0), writes=[(f'qT2_{b_}', i, j) for i in range(4) for j in range(2)])
                    S.op('pool', lambda e: e.memset(vaug[:].rearrange("p a b c -> p (a b c)"), 1.0), writes=[('vaug', s_) for s_ in range(NSLOT)])
                    wk_, wkk = load_wgroup(l, 6)
                    wq_, wqk = load_wgroup(l, 7)
                    wload(8 * l + 8)
                    def rk_iter(slot, kT_, kTkey, vaug_, wk__, wkk__):
                        c = {}

                        def s0():
                            c['ps'], c['pk'] = proj_slot(slot, wk__, wkk__)

                        def s1():
                            ps, pk = c['ps'], c['pk']
                            S.op('act', lambda e: e.copy(vaug_[:, slot, :, 0:64], ps[:, 256:512].rearrange("p (h c) -> p h c", c=64)),
                                 reads=[pk], writes=[('vaug', slot)])
                            c['kb'], c['kbk'] = WB()
                            rope(ps[:, 0:256].rearrange("p (h c) -> p h c", c=64), pk,
                                 c['kb'][:, 0:256].rearrange("p (h c) -> p h c", c=64), c['kbk'], 4,
                                 ct['rcc'][:, slot, :], ct['rss'][:, slot, :], ['c_rcc', 'c_rss'])

                        def s2():
                            c['pt'], c['ptk'] = qk_transpose(c['kb'], [c['kbk']], None, None, 2)

                        def s3():
                            S.op('dve', lambda e: e.tensor_copy(kT_[:, :, slot * 128:(slot + 1) * 128], c['pt'][:, 0:2, :]),
                                 reads=[c['ptk']], writes=[(kTkey, slot)])
                        return [s0, s1, s2, s3]
                    run_pipeline([rk_iter(slot, kT, 'kT', vaug, wk_, wkk) for slot in range(NSLOT)], 4)
                    wload(8 * l + 9)
                    allk = [('kT', s_) for s_ in range(NSLOT)]
                    for p in range(2):
                        S.op('dve', lambda e, p=p: e.tensor_reduce(out=kmf[:, p, :], in_=kT[:, p, :].rearrange("p (n r) -> p n r", r=256), axis=AX.X, op=ALU.add),
                             reads=allk, writes=[('kmf', p)])
                    S.op('dve', lambda e: e.tensor_scalar(out=kmb[:], in0=kmf[:], scalar1=1.0 / 256, scalar2=None, op0=ALU.mult),
                         reads=[('kmf', 0), ('kmf', 1)], writes=['kmb'])
                    isel2 = [isel, T("isel2", [128, 4, 16], BF16, mds)]

                    def d_prep(m, i, own, qT2, qT2k):
                        isl = isel2[m % 2]
                        islk = f"isel{m % 2}"
                        gp_, gpk = PR()

                        def fg(e):
                            for h in range(4):
                                ins = e.matmul(gp_[:, h * 16:(h + 1) * 16], qT2[:, h // 2, i, h % 2, :], kmb[:, h // 2, :], start=True, stop=True)
                            return ins
                        S.op('pe', fg, reads=[(qT2k, i, 0), (qT2k, i, 1), 'kmb'], writes=[gpk])
                        S.op('dve', lambda e: e.memset(gm[:].rearrange("p a b -> p (a b)"), NEGB), writes=['gm'])
                        S.op('dve', lambda e: e.tensor_copy(gm[:, :, 0:own], gp_[:, 0:64].rearrange("p (h n) -> p h n", n=16)[:, :, 0:own]),
                             reads=[gpk, 'gm'], writes=['gm'])
                        for h in range(4):
                            S.op('dve', lambda e, h=h: e.max(out=gmx[:, h, :], in_=gm[:, h, :]), reads=['gm'], writes=[('gmx', h)])
                        for h in range(4):
                            S.op('dve', lambda e, h=h: e.tensor_scalar(out=gm[:, h, :], in0=gm[:, h, :], scalar1=gmx[:, h, 2:3], scalar2=None, op0=ALU.is_ge),
                                 reads=['gm', ('gmx', h)], writes=['gm'])
                        S.op('dve', lambda e: e.tensor_scalar(out=isl[:].rearrange("p a b -> p (a b)"), in0=gm[:].rearrange("p a b -> p (a b)"), scalar1=-1.0, scalar2=240.0, op0=ALU.add, op1=ALU.mult),
                             reads=['gm'], writes=[islk])

                    def d_iter(m, i, kb, mt, first, last, own, qT2, qT2k, zt, ztk):
                        c = {}
                        acc, acck = pA[m % 2], f"pA{m % 2}"
                        isl = isel2[m % 2]
                        islk = f"isel{m % 2}"

                        def s0():
                            if first and own > 3:
                                d_prep(m, i, own, qT2, qT2k)
                            ps, pk = PR()
                            c['ps'], c['pk'] = ps, pk
                            n_ = kb // 2

                            def f(e):
                                for p in range(2):
                                    ins = e.matmul(ps[:, p * 256:(p + 1) * 256], kT[:, p, kb * 128:(kb + 1) * 128],
                                                   qT2[:, p, i, :, :].rearrange("p a b -> p (a b)"), start=(p == 0), stop=(mt == 'none' and p == 1),
                                                   skip_group_check=True)
                                if mt == 'sel':
                                    for h in range(4):
                                        ins = e.matmul(ps[:, h * 128:(h + 1) * 128], isl[:, h, n_:n_ + 1].to_broadcast([128, 128]), ident[:],
                                                       start=False, stop=(h == 3), skip_group_check=True)
                                elif mt == 'diag':
                                    ins = e.matmul(ps[:], ident[:], ct['ntri4'][:], start=False, stop=True, skip_group_check=True)
                                return ins
                            S.op('pe', f, reads=[('kT', kb), (qT2k, i, 0), (qT2k, i, 1), 'c_ident', 'c_ntri4'] + ([islk] if mt == 'sel' else []), writes=[pk])

                        def s1():
                            c['pb'], c['pbk'] = WB()
                            S.op('act', lambda e: e.activation(c['pb'][:], c['ps'][:], AF.Exp, scale=SCALE), reads=[c['pk']], writes=[c['pbk']])

                        def s2():
                            av(acc, acck, c['pb'], c['pbk'], lambda h: vaug[:, kb, h, :], ('vaug', kb), first, last, 65)
                            if last:
                                o, ok = normalize(acc, acck)
                                store_mixed(m, 768, lambda: o[:, 0:256], [ok], zt, ztk, i)
                        return [s0, s1, s2]
                    run_pipeline(qs_iters(wq_, wqk, 0, qT2s[0], 'qT2_0', zts[0], 'ztd_0', do_rope=True), 4)
                    for qg in range(nq_groups):
                        b_ = qg % 2
                        its = []
                        for i in range(4):
                            m = qg * 4 + i
                            own = m // 2
                            steps = [(kb, 'sel' if own > 3 else 'none') for kb in range(0, 2 * own)]
                            if m % 2 == 1:
                                steps.append((m - 1, 'none'))
                            steps.append((m, 'diag'))
                            for si_, (kb, mt) in enumerate(steps):
                                its.append(d_iter(m, i, kb, mt, si_ == 0, si_ == len(steps) - 1, own, qT2s[b_], f'qT2_{b_}', zts[b_], f'ztd_{b_}'))
                        extra = qs_iters(wq_, wqk, qg + 1, qT2s[1 - b_], f'qT2_{1 - b_}', zts[1 - b_], f'ztd_{1 - b_}', do_rope=True) if qg + 1 < nq_groups else []
                        run_pipeline(interleave(its, extra), 4)
                    S.barrier(drop_prefixes=('kT', 'vaug', 'qT2', 'ztd', 'kmf', 'kmb', 'gm', 'isel'))

            with ExitStack() as ps3:
                if dbg and (mixers != 'ABCD' or nq_groups != 8):
                    break
                wo = T("wo", [128, 8, D], BF16, ps3)
                mx = [T(f"mxl{i}", [128, D], BF16, ps3) for i in range(2)]
                mT = [T(f"mTl{i}", [128, 8, 128], BF16, ps3) for i in range(2)]
                xr = [T(f"xr{i}", [128, D], F32, ps3) for i in range(2)]
                ot = [T(f"ot{i}", [128, D], F32, ps3) for i in range(2)]
                junk3 = T("junk3", [128, D], BF16, ps3)
                st3 = [T(f"st3_{i}", [128, 8], F32, ps3) for i in range(2)]
                wsrc = wout_d[l].rearrange("(k p) c -> p k c", p=128)
                for q4 in range(4):
                    S.dma('pool', wo[:, q4 * 2:(q4 + 1) * 2, :], wsrc[:, q4 * 2:(q4 + 1) * 2, :], f"wol{q4}", writes=[('wo', q4)])
                def p3_iter(slot):
                    i2 = slot % 2
                    tsl = slice(slot * 128, (slot + 1) * 128)
                    stt, stk = st3[i2], f"st3_{i2}"
                    if slot % 2:
                        (y0, y0k), (y1, y1k) = (pR[0], 'pR0'), (pR[1], 'pR1')
                    else:
                        (y0, y0k), (y1, y1k) = (pA[0], 'pA0'), (pA[1], 'pA1')

                    def s0():
                        S.dma('sp', mx[i2][:], mixed_d[tsl, :], f"mxl{i2}", reads=[('mixed', slot, c0) for c0 in (0, 256, 512, 768)], writes=[f"mxl{i2}"])

                    def s1():
                        pt, ptk = PT()

                        def f(e):
                            for k in range(8):
                                ins = e.transpose(pt[:, k, :], mx[i2][:, k * 128:(k + 1) * 128], ident[:])
                            return ins
                        S.op('pe', f, reads=[f"mxl{i2}", 'c_ident'], writes=[ptk])
                        S.op('act', lambda e: e.copy(mT[i2][:], pt[:]), reads=[ptk], writes=[f"mTl{i2}"])

                    def s2():
                        S.dma('sp', xr[i2][:], xin[tsl, :], f"xr{i2}", reads=[(xin_key, slot)], writes=[f"xr{i2}"])

                        def fy(e):
                            for hh, y in enumerate((y0, y1)):
                                for k in range(8):
                                    ins = e.matmul(y[:], mT[i2][:, k, :], wo[:, k, hh * 512:(hh + 1) * 512], start=(k == 0), stop=(k == 7))
                            return ins
                        S.op('pe', fy, reads=[f"mTl{i2}"] + [('wo', q4) for q4 in range(4)], writes=[y0k, y1k])

                    def s3():
                        S.op('act', lambda e: e.activation(junk3[:, 0:512], y0[:], AF.Square, accum_out=stt[:, 0:1]), reads=[y0k], writes=[('junk3', 0), (stk, 0)])
                        S.op('act', lambda e: e.activation(junk3[:, 512:1024], y1[:], AF.Square, accum_out=stt[:, 1:2]), reads=[y1k], writes=[('junk3', 1), (stk, 1)])
                        S.op('dve', lambda e: e.tensor_tensor(stt[:, 2:3], stt[:, 0:1], stt[:, 1:2], ALU.add), reads=[(stk, 0), (stk, 1)], writes=[(stk, 2)])
                        S.op('dve', lambda e: e.tensor_scalar(out=stt[:, 5:6], in0=stt[:, 2:3], scalar1=1.0 / D, scalar2=EPS, op0=ALU.mult, op1=ALU.add), reads=[(stk, 2)], writes=[(stk, 5)])
                        S.op('act', lambda e: e.activation(stt[:, 3:4], stt[:, 5:6], AF.Sqrt), reads=[(stk, 5)], writes=[(stk, 3)])
                        S.op('dve', lambda e: e.reciprocal(stt[:, 4:5], stt[:, 3:4]), reads=[(stk, 3)], writes=[(stk, 4)])
                        for hh, (y, yk) in enumerate(((y0, y0k), (y1, y1k))):
                            S.op('dve', lambda e, y=y, hh=hh: e.scalar_tensor_tensor(out=ot[i2][:, hh * 512:(hh + 1) * 512], in0=y[:], scalar=stt[:, 4:5],
                                                                                   in1=gpt[:, hh * 512:(hh + 1) * 512], op0=ALU.mult, op1=ALU.mult),
                                 reads=[yk, (stk, 4), 'gpt'], writes=[(f"ot{i2}", hh)])
                        S.op('pool', lambda e: e.tensor_tensor(ot[i2][:], ot[i2][:], xr[i2][:], ALU.add),
                             reads=[(f"ot{i2}", 0), (f"ot{i2}", 1), f"xr{i2}"], writes=[(f"ot{i2}", 0), (f"ot{i2}", 1)])
                        S.dma('pool', xout[tsl, :], ot[i2][:], f'outst{i2}', reads=[(f"ot{i2}", 0), (f"ot{i2}", 1)], writes=[(xout_key, slot)])
                    return [s0, s1, s2, s3]
                run_pipeline([p3_iter(slot) for slot in range(NSLOT)], 4)
                S.barrier(drop_prefixes=('wo', 'mxl', 'mTl', 'xr', 'ot', 'junk3', 'st3_'))
        S.finish()
        build.stats = dict(nins=S.nins, nwait=S.nwait, nsem=S.nsem)
    return nc


def _prep_inputs(inputs):
    f = np.float32
    x = np.asarray(inputs['x'], f)
    c = np.asarray(inputs['c'], f)
    w_in = np.asarray(inputs['w_in'], f)
    shared = {}
    shared['npre'] = np.ascontiguousarray(np.broadcast_to(np.asarray(inputs['norm_pre'], f)[:, None, :], (L, 128, D)))
    shared['npost'] = np.ascontiguousarray(np.broadcast_to(np.asarray(inputs['norm_post'], f)[:, None, :], (L, 128, D)))
    shared['bmod'] = np.ascontiguousarray(np.broadcast_to(np.asarray(inputs['b_mod'], f)[:, None, :], (L, 128, 3 * D)))
    shared['wmod'] = np.ascontiguousarray(np.asarray(inputs['w_mod'], f))
    shared['wg'] = np.ascontiguousarray(np.stack([_w_groups(w_in[l]) for l in range(L)], 0))
    shared['wout'] = np.ascontiguousarray(np.asarray(inputs['w_out'], f))
    shared['w1'] = np.ascontiguousarray(np.asarray(inputs['cmp_w1'], f))
    shared['w2'] = np.ascontiguousarray(np.asarray(inputs['cmp_w2'], f))
    b1 = np.asarray(inputs['cmp_b1'], f)
    shared['b1'] = np.ascontiguousarray(b1.reshape(L, 2, 2, 128).transpose(0, 3, 1, 2).reshape(L, 128, 4))
    pos = np.asarray(inputs['cmp_pos'], f)
    shared['posT'] = np.ascontiguousarray(pos.transpose(0, 3, 1, 2))
    for k_, v_ in _consts().items():
        shared['c_' + k_] = v_
    maps = []
    for b in range(x.shape[0]):
        m = dict(shared)
        m['x'] = np.ascontiguousarray(x[b])
        m['cT'] = np.ascontiguousarray(c[b].reshape(8, 128).T)
        maps.append(m)
    return maps


_NC_CACHE = {}


def kernel(**inputs):
    maps = _prep_inputs(inputs)
    if 'nc' not in _NC_CACHE:
        _NC_CACHE['nc'] = build()
    nc = _NC_CACHE['nc']
    res = run_bass_kernel_spmd(nc, maps, core_ids=list(range(len(maps))))
    out = np.stack([np.asarray(r['out'], np.float32) for r in res.results], 0)
    return out
```

```python
import numpy as np
from contextlib import ExitStack
import concourse.bass as bass
import concourse.mybir as mybir
from concourse.bass_utils import run_bass_kernel_spmd
import ml_dtypes

F32 = mybir.dt.float32
BF16 = mybir.dt.bfloat16
AF = mybir.ActivationFunctionType
ALU = mybir.AluOpType
AX = mybir.AxisListType

S_LEN = 4096
D = 1024
NSLOT = 32
L = 2
EPS = 1e-6
SCALE = 0.125
BIG = 1e9
NEGB = -1e30


class Sched:
    EPOCH = 20000

    def __init__(self, nc, es, same_engine_sync=True):
        self.nc = nc
        self.es = es
        self.eng = dict(pe=nc.tensor, act=nc.scalar, dve=nc.vector, pool=nc.gpsimd, sp=nc.sync)
        self.cur = {}
        self.nsem = 0
        self.bufs = {}
        self.seen = {e: {} for e in self.eng}
        self.streams = {}
        self.same = same_engine_sync
        self.nwait = 0
        self.nins = 0
        self.last_tok = {}

    def _newsem(self, name):
        self.nsem += 1
        return self.es.enter_context(self.nc.semaphore(f"s{self.nsem}_{name}"))

    def _tick(self, e):
        c = self.cur.get(e)
        if c is None or c[1] >= self.EPOCH:
            c = [self._newsem(e), 0]
            self.cur[e] = c
        c[1] += 1
        return ('c', c[0], c[1], e)

    def _wait(self, e, tok):
        if tok[0] == 'c':
            _, sem, val, src = tok
            if src == e and (e == 'pe' or not self.same):
                return
        else:
            _, st = tok
            sem = st[0]
            val = 16 * st[1]
        k = sem.name
        if self.seen[e].get(k, 0) >= val:
            return
        self.eng[e].wait_ge(sem, val)
        self.nwait += 1
        self.seen[e][k] = val

    def _deps(self, reads, writes):
        deps = []
        for r in reads:
            b = self.bufs.get(r)
            if b and b['w'] is not None:
                deps.append(b['w'])
        for w in writes:
            b = self.bufs.get(w)
            if b:
                if b['w'] is not None:
                    deps.append(b['w'])
                deps.extend(b['r'].values())
        return deps

    def _record(self, e, tok, reads, writes):
        for r in reads:
            self.bufs.setdefault(r, dict(w=None, r={}))['r'][e] = tok
        for w in writes:
            self.bufs[w] = dict(w=tok, r={})
        self.last_tok[e] = tok

    @staticmethod
    def _is_psum(k):
        return isinstance(k, str) and k[:2] in ('pR', 'pA', 'pT')

    def op(self, e, fn, reads=(), writes=()):
        psr = [r for r in reads if self._is_psum(r)]
        if psr:
            reads = [r for r in reads if not self._is_psum(r)]
            writes = list(writes) + [r for r in psr if r not in writes]
        for t in self._deps(reads, writes):
            self._wait(e, t)
        ins = fn(self.eng[e])
        tok = self._tick(e)
        ins.then_inc(tok[1], 1)
        self.nins += 1
        self._record(e, tok, reads, writes)
        return tok

    def dma(self, e, out, in_, stream, reads=(), writes=(), **kw):
        for t in self._deps(reads, writes):
            self._wait(e, t)
        st = self.streams.get(stream)
        if st is None:
            st = [self._newsem('d' + stream), 0]
            self.streams[stream] = st
        elif st[1] > 0:
            self._wait(e, ('d', st))
        ins = self.eng[e].dma_start(out=out, in_=in_, **kw)
        ins.then_inc(st[0], 16)
        st[1] += 1
        tok = ('d', st)
        self.nins += 1
        self._record('dma:' + stream, tok, reads, writes)
        return tok

    def barrier(self, drop_prefixes=()):
        toks = [t for k, t in self.last_tok.items()]
        for e in self.eng:
            for t in toks:
                self._wait(e, t)
        if drop_prefixes:
            for k in list(self.bufs.keys()):
                ks = k if isinstance(k, str) else k[0]
                if any(ks.startswith(p) for p in drop_prefixes):
                    del self.bufs[k]

    def finish(self):
        for stream, st in self.streams.items():
            self._wait('sp', ('d', st))
        for e, t in list(self.last_tok.items()):
            if not e.startswith('dma:'):
                self._wait('sp', t)


class Rot:
    def __init__(self, items):
        self.items = items
        self.i = 0

    def __call__(self):
        it = self.items[self.i % len(self.items)]
        self.i += 1
        return it


def _consts():
    bf = ml_dtypes.bfloat16
    c = {}
    k = np.arange(128)[:, None]
    q = np.arange(128)[None, :]
    c['ident'] = np.eye(128, dtype=np.float32).astype(bf)
    c['triS'] = (k < q).astype(np.float32).astype(bf)
    c['triI'] = (k <= q).astype(np.float32).astype(bf)
    c['w4'] = (q < k).astype(np.float32).astype(bf)
    c['umat'] = (k >= q).astype(np.float32).astype(bf)
    c['ones'] = np.ones((128, 128), np.float32).astype(bf)
    c['ntri4'] = np.tile(-240.0 * (1.0 - (k <= q).astype(np.float32)), (1, 4)).astype(bf)
    c['nw44'] = np.tile(-240.0 * (1.0 - (q < k).astype(np.float32)), (1, 4)).astype(bf)
    mb = np.zeros((128, 17, 128), np.float32)
    for d in range(17):
        j = 128 * d + q - k
        m = ((j >= 0) & (j <= 128)).astype(np.float32)
        m += ((j >= 0) & (j <= 512) & (j % 4 == 0)).astype(np.float32)
        m += ((j >= 0) & (j <= 2048) & (j % 16 == 0)).astype(np.float32)
        mb[:, d, :] = m
    with np.errstate(divide='ignore'):
        lb = np.where(mb > 0, np.log(np.maximum(mb, 1.0)) / SCALE, -240.0).astype(np.float32)
    hi = lb.astype(bf)
    c['lmbh'] = hi
    c['lmbl'] = (lb[:, 0:5, :] - hi[:, 0:5, :].astype(np.float32)).astype(bf)
    inv_freq = (1.0 / (500000.0 ** (np.arange(0, 16, 2, dtype=np.float32) / 16))).astype(np.float32)

    def tabs(pos):
        ang = pos.astype(np.float32)[:, None] * inv_freq[None, :]
        cs, sn = np.cos(ang).astype(np.float32), np.sin(ang).astype(np.float32)
        return np.concatenate([cs, cs], 1), np.concatenate([-sn, sn], 1)
    cc, ss = tabs(np.arange(S_LEN))
    c['rcc'] = np.ascontiguousarray(cc.reshape(32, 128, 16).transpose(1, 0, 2))
    c['rss'] = np.ascontiguousarray(ss.reshape(32, 128, 16).transpose(1, 0, 2))
    ccc, ssc = tabs(np.arange(256) * 16 + 31)
    c['rccc'] = np.ascontiguousarray(ccc.reshape(2, 128, 16).transpose(1, 0, 2))
    c['rssc'] = np.ascontiguousarray(ssc.reshape(2, 128, 16).transpose(1, 0, 2))
    cv = np.zeros((128, 17, 128), np.float32)
    for u in range(17):
        cv[:, u, :] = (16 * k + 31 - q <= 128 * u)
    c['cv'] = cv.astype(bf)
    n_cmp, n_sel = 255, 64
    c_start = np.arange(n_cmp) * 16
    s_start = np.arange(n_sel) * 64
    ov = np.clip(np.minimum(c_start[:, None] + 32, s_start[None, :] + 64)
                 - np.maximum(c_start[:, None], s_start[None, :]), 0, None) / 32
    ovp = np.zeros((256, 64), np.float32)
    ovp[:255] = ov
    c['ovl'] = np.ascontiguousarray(ovp.reshape(2, 128, 64).transpose(1, 0, 2)).astype(bf)
    qq = np.arange(128)[:, None]
    cidx = np.arange(128)[None, :]
    hi = qq // 64
    V = (cidx - 64 <= hi)
    Fm = (cidx - 64 >= hi - 1)
    c['t1'] = (V & ~Fm).astype(np.float32)
    c['t2'] = (BIG * (V & Fm) + NEGB * (~V)).astype(np.float32)
    c['vt'] = V.astype(np.float32)
    return c


GROUPS = None


def _w_groups(w_in_l):
    o = {}
    names = ['qa', 'ka', 'va', 'za', 'qb', 'kb', 'vb', 'zb', 'qc', 'kcr', 'vcr', 'ksr', 'vsr', 'kwr', 'vwr',
             'gc', 'zc', 'qd', 'kd', 'vd', 'zd']
    sizes = [256] * 8 + [256, 64, 64, 64, 64, 64, 64, 12, 256] + [256] * 4
    off = 0
    for n, s in zip(names, sizes):
        o[n] = w_in_l[:, off:off + s]
        off += s
    assert off == 3980
    pad = np.zeros((1024, 512 - 396), np.float32)
    g = [np.concatenate([o['ka'], o['va']], 1), np.concatenate([o['qa'], o['za']], 1),
         np.concatenate([o['kb'], o['vb']], 1), np.concatenate([o['qb'], o['zb']], 1),
         np.concatenate([o['kcr'], o['vcr'], o['ksr'], o['kwr'], o['vsr'], o['vwr'], o['gc'], pad], 1),
         np.concatenate([o['qc'], o['zc']], 1),
         np.concatenate([o['kd'], o['vd']], 1), np.concatenate([o['qd'], o['zd']], 1)]
    return np.stack(g, 0)


def build(n_layers=2, mixers='ABCD', dbg=False, nq_groups=8):
    nc = bass.Bass("TRN2", target_bir_lowering=False)
    C = _consts()

    def din(name, shape, dt=F32):
        return nc.dram_tensor(name, list(shape), dt, kind="ExternalInput").ap()
    x_d = din("x", [S_LEN, D])
    cT_d = din("cT", [128, 8])
    npre_d = din("npre", [L, 128, D])
    npost_d = din("npost", [L, 128, D])
    bmod_d = din("bmod", [L, 128, 3 * D])
    wmod_d = din("wmod", [L, D, 3 * D])
    wg_d = din("wg", [L, 8, D, 512])
    wout_d = din("wout", [L, D, D])
    w1_d = din("w1", [L, 2, 2048, 256])
    w2_d = din("w2", [L, 2, 256, 64])
    b1_d = din("b1", [L, 128, 4])
    posT_d = din("posT", [L, 64, 2, 32])
    cd = {}
    for k_, v_ in C.items():
        cd[k_] = din("c_" + k_, v_.shape, BF16 if v_.dtype == ml_dtypes.bfloat16 else F32)
    out_d = nc.dram_tensor("out", [S_LEN, D], F32, kind="ExternalOutput").ap()
    x1_d = nc.dram_tensor("x1s", [S_LEN, D], F32, kind="Internal").ap()
    mixed_d = nc.dram_tensor("mixed", [S_LEN, D], BF16, kind="ExternalOutput" if dbg else "Internal").ap()

    with ExitStack() as es:
        S = Sched(nc, es)

        tcount = [0]

        def T(name, shape, dt, st=es):
            tcount[0] += 1
            return st.enter_context(nc.sbuf_tensor(f"{name}_u{tcount[0]}", list(shape), dt))

        def P(name, shape, dt):
            return es.enter_context(nc.psum_tensor(name, list(shape), dt))

        hT = T("hT", [128, 8, S_LEN], BF16)
        Gt = T("Gt", [128, D], F32)
        sht = T("sht", [128, D], F32)
        gpt = T("gpt", [128, D], F32)
        ct = {}
        for k_, v_ in C.items():
            ct[k_] = T("k_" + k_, v_.shape, BF16 if v_.dtype == ml_dtypes.bfloat16 else F32)
            S.dma('sp', ct[k_][:], cd[k_], 'const', writes=['c_' + k_])
        ident = ct['ident']
        wbf = [T(f"wbf{i}", [128, 8, 512], BF16) for i in range(3)]
        wloaded = {}
        wf = [T(f"wf{i}", [128, 512], F32) for i in range(3)]
        wb = [T(f"wb{i}", [128, 512], BF16) for i in range(6)]
        wf_rot = Rot(list(range(3)))
        wb_rot = Rot(list(range(6)))
        qbp = [T(f"qbp{i}", [128, 256], BF16) for i in range(2)]
        qb_rot = Rot([0, 1])
        sm2 = [T(f"sz{i}", [128, 256], F32) for i in range(1)]
        sm2_rot = Rot([0])

        def SM2():
            i = sm2_rot()
            return sm2[i], f"sz{i}"
        sm = [T(f"sm{i}", [128, 64], F32) for i in range(8)]
        sm_rot = Rot(list(range(8)))
        pT = [P(f"pT{i}", [128, 8, 128], BF16) for i in range(2)]
        pR = [P(f"pR{i}", [128, 512], F32) for i in range(4)]
        pA = [P(f"pA{i}", [128, 512], F32) for i in range(2)]
        pT_rot = Rot([0, 1])
        pR_rot = Rot([0, 1, 2, 3])

        def WF():
            i = wf_rot()
            return wf[i], f"wf{i}"

        def WB():
            i = wb_rot()
            return wb[i], f"wb{i}"

        def SM():
            i = sm_rot()
            return sm[i], f"sm{i}"

        def PR():
            i = pR_rot()
            return pR[i], f"pR{i}"

        def PT():
            i = pT_rot()
            return pT[i], f"pT{i}"

        def wload(idx):
            if idx in wloaded or idx >= 8 * n_layers:
                return
            bi = idx % 3
            wt, wk = wbf[bi], f"wbf{bi}"
            src = wg_d[idx // 8, idx % 8].rearrange("(k p) c -> p k c", p=128)
            for half in range(2):
                S.dma('pool', wt[:, half * 4:(half + 1) * 4, :], src[:, half * 4:(half + 1) * 4, :], f"wl{bi}{half}", writes=[(wk, half)])
            wloaded[idx] = (wt, wk)

        def load_wgroup(l, g):
            idx = 8 * l + g
            wload(idx)
            return wloaded[idx]

        def proj_slot(slot, wt, wk, ncols=512):
            ps, pk = PR()

            def f(e):
                for k in range(8):
                    ins = e.matmul(ps[:, 0:ncols], hT[:, k, slot * 128:(slot + 1) * 128], wt[:, k, 0:ncols],
                                   start=(k == 0), stop=(k == 7))
                return ins
            S.op('pe', f, reads=[('hT', slot), (wk, 0), (wk, 1)], writes=[pk])
            return ps, pk

        def rope(src, srckey, dst, dstkey, nh, cc_ap, ss_ap, tabkeys):
            S.op('act', lambda e: e.copy(dst[:, :, 16:64], src[:, :, 16:64]), reads=[srckey], writes=[dstkey])
            ta, tak = SM()
            tb, tbk = SM()
            tav = ta[:, 0:nh * 16].rearrange("p (h c) -> p h c", c=16)
            tbv = tb[:, 0:nh * 16].rearrange("p (h c) -> p h c", c=16)
            S.op('dve', lambda e: e.tensor_tensor(tav, src[:, :, 0:16], cc_ap.unsqueeze(1).to_broadcast([128, nh, 16]), ALU.mult),
                 reads=[srckey] + tabkeys, writes=[tak])
            S.op('dve', lambda e: e.tensor_tensor(tbv[:, :, 0:8], src[:, :, 8:16], ss_ap[:, 0:8].unsqueeze(1).to_broadcast([128, nh, 8]), ALU.mult),
                 reads=[srckey] + tabkeys, writes=[tbk])
            S.op('dve', lambda e: e.tensor_tensor(tbv[:, :, 8:16], src[:, :, 0:8], ss_ap[:, 8:16].unsqueeze(1).to_broadcast([128, nh, 8]), ALU.mult),
                 reads=[srckey] + tabkeys, writes=[tbk])
            S.op('dve', lambda e: e.tensor_tensor(dst[:, :, 0:16], tav, tbv, ALU.add),
                 reads=[tak, tbk], writes=[dstkey])

        def run_pipeline(its, nst):
            n = len(its)
            for step in range(n + nst - 1):
                for k in range(nst):
                    t = step - k
                    if 0 <= t < n and its[t][k] is not None:
                        its[t][k]()

        for l in range(n_layers):
            xin = x_d if l == 0 else x1_d
            xin_key = 'xin' if l == 0 else 'x1'
            xout = out_d if l == n_layers - 1 else x1_d
            xout_key = 'out' if l == n_layers - 1 else 'x1'

            with ExitStack() as ps0:
                cTt = T("cTt", [128, 8], F32, ps0)
                sc = T("sc", [128, 8], F32, ps0)
                crep = T("crep", [128, 8, 128], F32, ps0)
                wm = [T(f"wm{i}", [128, 8, 512], F32, ps0) for i in range(2)]
                modb = T("modb", [128, 3 * D], F32, ps0)
                bmt = T("bmt", [128, 3 * D], F32, ps0)
                npre_t = T("npre_t", [128, D], F32, ps0)
                npost_t = T("npost_t", [128, D], F32, ps0)
                S.dma('sp', cTt[:], cT_d, 'p0a', writes=['cTt'])
                S.dma('sp', bmt[:], bmod_d[l], 'p0b', writes=['bmt'])
                S.dma('sp', npre_t[:], npre_d[l], 'p0c', writes=['npre_t'])
                S.dma('sp', npost_t[:], npost_d[l], 'p0d', writes=['npost_t'])
                S.op('act', lambda e: e.activation(sc[:], cTt[:], AF.Silu), reads=['cTt'], writes=['sc'])
                S.op('dve', lambda e: e.tensor_copy(crep[:], sc[:].unsqueeze(2).to_broadcast([128, 8, 128])), reads=['sc'], writes=['crep'])
                for cg in range(6):
                    wmt, wmk = wm[cg % 2], f"wm{cg % 2}"
                    S.dma('sp', wmt[:], wmod_d[l][:, cg * 512:(cg + 1) * 512].rearrange("(k p) c -> p k c", p=128), wmk, writes=[wmk])
                    ps, pk = PR()

                    def f(e, ps=ps, wmt=wmt):
                        for k in range(8):
                            ins = e.matmul(ps[:], crep[:, k, :], wmt[:, k, :], start=(k == 0), stop=(k == 7))
                        return ins
                    S.op('pe', f, reads=['crep', wmk], writes=[pk])
                    S.op('dve', lambda e, ps=ps, cg=cg: e.tensor_tensor(modb[:, cg * 512:(cg + 1) * 512], ps[:], bmt[:, cg * 512:(cg + 1) * 512], ALU.add),
                         reads=[pk, 'bmt'], writes=[('modb', cg)])
                S.op('act', lambda e: e.copy(sht[:], modb[:, 0:D]), reads=[('modb', 0), ('modb', 1)], writes=['sht'])
                S.op('dve', lambda e: e.scalar_tensor_tensor(out=Gt[:], in0=modb[:, D:2 * D], scalar=1.0, in1=npre_t[:], op0=ALU.add, op1=ALU.mult),
                     reads=[('modb', 2), ('modb', 3), 'npre_t'], writes=['Gt'])
                S.op('dve', lambda e: e.tensor_tensor(gpt[:], modb[:, 2 * D:3 * D], npost_t[:], ALU.mult),
                     reads=[('modb', 4), ('modb', 5), 'npost_t'], writes=['gpt'])
                S.barrier(drop_prefixes=('cTt', 'sc', 'crep', 'wm', 'modb', 'bmt', 'npre_t', 'npost_t'))

            with ExitStack() as ps1:
                xt = [T(f"xt{i}", [128, D], F32, ps1) for i in range(2)]
                junk = T("junk", [128, D], BF16, ps1)
                ht32 = T("ht32", [128, D], F32, ps1)
                hb = [T(f"hb{i}", [128, D], BF16, ps1) for i in range(2)]
                st1 = [T(f"st1_{i}", [128, 4], F32, ps1) for i in range(2)]
                def p1_iter(slot):
                    i2 = slot % 2
                    stt, stk = st1[i2], f"st1_{i2}"
                    c = {}

                    def s0():
                        S.dma('sp', xt[i2][:], xin[slot * 128:(slot + 1) * 128, :], f"xt{i2}", reads=[(xin_key, slot)], writes=[f"xt{i2}"])

                    def s1():
                        S.op('act', lambda e: e.activation(junk[:], xt[i2][:], AF.Square, accum_out=stt[:, 0:1]),
                             reads=[f"xt{i2}"], writes=['junk', (stk, 0)])
                        S.op('dve', lambda e: e.tensor_scalar(out=stt[:, 3:4], in0=stt[:, 0:1], scalar1=1.0 / D, scalar2=EPS, op0=ALU.mult, op1=ALU.add),
                             reads=[(stk, 0)], writes=[(stk, 3)])
                        S.op('act', lambda e: e.activation(stt[:, 1:2], stt[:, 3:4], AF.Sqrt), reads=[(stk, 3)], writes=[(stk, 1)])
                        S.op('dve', lambda e: e.reciprocal(stt[:, 2:3], stt[:, 1:2]), reads=[(stk, 1)], writes=[(stk, 2)])
                        S.op('dve', lambda e: e.scalar_tensor_tensor(out=ht32[:], in0=xt[i2][:], scalar=stt[:, 2:3], in1=Gt[:], op0=ALU.mult, op1=ALU.mult),
                             reads=[f"xt{i2}", (stk, 2), 'Gt'], writes=['ht32'])
                        S.op('pool', lambda e: e.tensor_tensor(hb[i2][:], ht32[:], sht[:], ALU.add),
                             reads=['ht32', 'sht'], writes=[f"hb{i2}"])

                    def s2():
                        pt, ptk = PT()
                        c['pt'], c['ptk'] = pt, ptk

                        def f(e):
                            for k in range(8):
                                ins = e.transpose(pt[:, k, :], hb[i2][:, k * 128:(k + 1) * 128], ident[:])
                            return ins
                        S.op('pe', f, reads=[f"hb{i2}", 'c_ident'], writes=[ptk])

                    def s3():
                        pt, ptk = c['pt'], c['ptk']
                        if slot % 2:
                            S.op('act', lambda e: e.copy(hT[:, :, slot * 128:(slot + 1) * 128], pt[:]), reads=[ptk], writes=[('hT', slot)])
                        else:
                            S.op('dve', lambda e: e.tensor_copy(hT[:, :, slot * 128:(slot + 1) * 128], pt[:]), reads=[ptk], writes=[('hT', slot)])
                    return [s0, s1, s2, s3]
                run_pipeline([p1_iter(slot) for slot in range(NSLOT)], 4)
                S.barrier(drop_prefixes=('xt', 'junk', 'ht32', 'hb', 'st1_'))

            def qk_transpose(src_bf, srckeys, dst_fn, dstkey, npair):
                pt, ptk = PT()

                def f(e):
                    for p in range(npair):
                        ins = e.transpose(pt[:, p, :], src_bf[:, p * 128:(p + 1) * 128], ident[:])
                    return ins
                S.op('pe', f, reads=list(srckeys) + ['c_ident'], writes=[ptk])
                return pt, ptk

            mixrr = [0]

            def store_mixed(m, col0, o_ap_fn, okeys, zt, ztk, i):
                mo, mok = WB()
                S.op('dve', lambda e: e.tensor_tensor(mo[:, 0:256], o_ap_fn(), zt[:, i, :], ALU.mult),
                     reads=list(okeys) + [(ztk, i)], writes=[mok])
                mixrr[0] += 1
                S.dma('pool', mixed_d[m * 128:(m + 1) * 128, col0:col0 + 256], mo[:, 0:256], f'mixst{mixrr[0] % 4}',
                      reads=[mok], writes=[('mixed', m, col0)])

            def qs_iters(wq, wqk, qg, qT2, qT2k, zt, ztk, do_rope, pairs=True, qcT=None):
                def one(i):
                    slot = qg * 4 + i
                    c = {}

                    def s0():
                        c['ps'], c['pk'] = proj_slot(slot, wq, wqk)

                    def s1():
                        ps, pk = c['ps'], c['pk']
                        ez, ezk = SM2()
                        S.op('act', lambda e: e.activation(ez[:], ps[:, 256:512], AF.Exp, scale=-1.0), reads=[pk], writes=[ezk])
                        S.op('dve', lambda e: e.tensor_scalar(out=ez[:], in0=ez[:], scalar1=1.0, scalar2=None, op0=ALU.add), reads=[ezk], writes=[ezk])
                        S.op('dve', lambda e: e.reciprocal(ez[:], ez[:]), reads=[ezk], writes=[ezk])
                        S.op('dve', lambda e: e.tensor_tensor(zt[:, i, :], ps[:, 256:512], ez[:], ALU.mult), reads=[pk, ezk], writes=[(ztk, i)])
                        qi = qb_rot()
                        qb_, qbk = qbp[qi], f"qbp{qi}"
                        c['qb'], c['qbk'] = qb_, qbk
                        if do_rope:
                            rope(ps[:, 0:256].rearrange("p (h c) -> p h c", c=64), pk,
                                 qb_[:, 0:256].rearrange("p (h c) -> p h c", c=64), qbk, 4,
                                 ct['rcc'][:, slot, :], ct['rss'][:, slot, :], ['c_rcc', 'c_rss'])
                        else:
                            S.op('dve', lambda e: e.tensor_copy(qb_[:, 0:256], ps[:, 0:256]), reads=[pk], writes=[qbk])

                    def s2():
                        qb_, qbk = c['qb'], c['qbk']
                        if pairs:
                            c['pt'], c['ptk'] = qk_transpose(qb_, [qbk], None, None, 2)
                        else:
                            pt, ptk = PT()
                            c['pt'], c['ptk'] = pt, ptk

                            def f(e):
                                for h in range(4):
                                    ins = e.transpose(pt[0:64, h, :], qb_[:, h * 64:(h + 1) * 64], ident[:])
                                return ins
                            S.op('pe', f, reads=[qbk, 'c_ident'], writes=[ptk])

                    def s3():
                        pt, ptk = c['pt'], c['ptk']
                        if pairs:
                            S.op('dve', lambda e: e.tensor_copy(qT2[0:64, :, i, 0, :], pt[0:64, 0:2, :]), reads=[ptk], writes=[(qT2k, i, 0)])
                            S.op('act', lambda e: e.copy(qT2[64:128, :, i, 1, :], pt[64:128, 0:2, :]), reads=[ptk], writes=[(qT2k, i, 1)])
                        else:
                            S.op('dve', lambda e: e.tensor_copy(qcT[:, i, :, :], pt[0:64, 0:4, :]), reads=[ptk], writes=[('qcT', i)])
                    return [s0, s1, s2, s3]
                return [one(i) for i in range(4)]

            def interleave(its, extra):
                its = [it + [None] * (4 - len(it)) for it in its]
                if not extra:
                    return its
                n = len(its)
                out = []
                pos = [((j + 1) * n) // (len(extra) + 1) for j in range(len(extra))]
                j = 0
                for t, it in enumerate(its):
                    while j < len(extra) and pos[j] == t:
                        out.append(extra[j])
                        j += 1
                    out.append(it)
                out.extend(extra[j:])
                return out

            def s_pairs(kT, kTk, kb, qT2, qT2k, i):
                ps, pk = PR()

                def f(e):
                    for p in range(2):
                        ins = e.matmul(ps[:, p * 256:(p + 1) * 256], kT[:, p, kb * 128:(kb + 1) * 128],
                                       qT2[:, p, i, :, :].rearrange("p a b -> p (a b)"), start=True, stop=True)
                    return ins
                S.op('pe', f, reads=[(kTk, kb), (qT2k, i, 0), (qT2k, i, 1)], writes=[pk])
                return ps, pk

            def av(acc, acck, pb, pbk, v_fn, vkey, first, last, width):
                def f(e):
                    for h in range(4):
                        ins = e.matmul(acc[:, h * width:(h + 1) * width], pb[:, h * 128:(h + 1) * 128], v_fn(h), start=(first and h == 0), stop=last,
                                       skip_group_check=True)
                    return ins
                S.op('pe', f, reads=[pbk] + (vkey if isinstance(vkey, list) else [vkey]), writes=[acck])

            def normalize(acc, acck, width=65):
                accv = acc[:, 0:4 * width].rearrange("p (h c) -> p h c", c=width)
                rd, rdk = SM()
                S.op('dve', lambda e: e.reciprocal(rd[:, 0:4], accv[:, :, 64]), reads=[acck], writes=[rdk])
                o, ok = WF()
                S.op('dve', lambda e: e.tensor_tensor(o[:, 0:256].rearrange("p (h c) -> p h c", c=64), accv[:, :, 0:64],
                                                      rd[:, 0:4].unsqueeze(2).to_broadcast([128, 4, 64]), ALU.mult),
                     reads=[acck, rdk], writes=[ok])
                return o, ok

            if 'A' in mixers:
                with ExitStack() as ma:
                    kaT = T("kaT", [128, 2, S_LEN], BF16, ma)
                    va = T("va", [128, NSLOT, 256], BF16, ma)
                    qT2s = [T(f"qT2a{b_}", [128, 2, 4, 2, 128], BF16, ma) for b_ in range(2)]
                    zts = [T(f"zta{b_}", [128, 4, 256], BF16, ma) for b_ in range(2)]
                    for b_ in range(2):
                        S.op('pool', lambda e, b_=b_: e.memset(qT2s[b_][:].rearrange("p a b c d -> p (a b c d)"), 0.0), writes=[(f'qT2_{b_}', i, j) for i in range(4) for j in range(2)])
                    wk_, wkk = load_wgroup(l, 0)
                    wq_, wqk = load_wgroup(l, 1)
                    wload(8 * l + 2)
                    def a_k_iter(slot):
                        c = {}

                        def s0():
                            c['ps'], c['pk'] = proj_slot(slot, wk_, wkk)

                        def s1():
                            ps, pk = c['ps'], c['pk']
                            S.op('act', lambda e: e.copy(va[:, slot, :], ps[:, 256:512]), reads=[pk], writes=[('va', slot)])
                            c['kb'], c['kbk'] = WB()
                            S.op('dve', lambda e: e.tensor_copy(c['kb'][:, 0:256], ps[:, 0:256]), reads=[pk], writes=[c['kbk']])

                        def s2():
                            c['pt'], c['ptk'] = qk_transpose(c['kb'], [c['kbk']], None, None, 2)

                        def s3():
                            S.op('dve', lambda e: e.tensor_copy(kaT[:, :, slot * 128:(slot + 1) * 128], c['pt'][:, 0:2, :]),
                                 reads=[c['ptk']], writes=[('kaT', slot)])
                        return [s0, s1, s2, s3]
                    run_pipeline([a_k_iter(slot) for slot in range(NSLOT)], 4)
                    wload(8 * l + 3)
                    wa = [T(f"wa{j}", [128, 512], BF16, ma) for j in range(16)]
                    wa_rot = Rot(list(range(16)))

                    def WA():
                        j = wa_rot()
                        return wa[j], f"wa{j}"

                    def a_iter(m, i, kb, st, qT2, qT2k, zt, ztk):
                        c = {}
                        acc, acck = pA[m % 2], f"pA{m % 2}"
                        diag = (kb == m)

                        def s0():
                            c['ps'], c['pk'] = s_pairs(kaT, 'kaT', kb, qT2, qT2k, i)

                        def s1():
                            ps, pk = c['ps'], c['pk']
                            et, etk = WA()
                            c['et'], c['etk'] = et, etk
                            S.op('act', lambda e: e.activation(et[:], ps[:], AF.Exp, scale=SCALE), reads=[pk], writes=[etk])
                            spb, spbk = WA()
                            S.op('act', lambda e: e.activation(spb[:], et[:], AF.Ln, bias=1.0), reads=[etk], writes=[spbk])
                            if diag:
                                S.op('dve', lambda e: e.tensor_tensor(
                                    spb[:].rearrange("p (h c) -> p h c", c=128), spb[:].rearrange("p (h c) -> p h c", c=128),
                                    ct['triS'][:].unsqueeze(1).to_broadcast([128, 4, 128]), ALU.mult),
                                    reads=[spbk, 'c_triS'], writes=[spbk])
                            bw, bwk = PR()
                            c['bw'], c['bwk'] = bw, bwk
                            lsum, lsumk = st['lsum'], st['lsumk']

                            def fb(e):
                                ins = e.matmul(bw[:], ct['umat'][:], spb[:], start=True, stop=diag)
                                if not diag:
                                    ins = e.matmul(bw[:], ct['ones'][:], lsum[:], start=False, stop=True)
                                return ins
                            S.op('pe', fb, reads=[spbk, 'c_umat', 'c_ones'] + ([lsumk] if not diag else []), writes=[bwk])
                            if kb > 0:
                                if diag:
                                    st['lsum'], st['lsumk'] = spb, spbk
                                else:
                                    nl_, nlk = WA()
                                    S.op('pool', lambda e: e.tensor_tensor(nl_[:], lsum[:], spb[:], ALU.add), reads=[lsumk, spbk], writes=[nlk])
                                    st['lsum'], st['lsumk'] = nl_, nlk

                        def s2():
                            et, etk, bw, bwk = c['et'], c['etk'], c['bw'], c['bwk']
                            xt_, xtk = WA()
                            S.op('act', lambda e: e.activation(xt_[:], bw[:], AF.Exp, scale=-1.0), reads=[bwk], writes=[xtk])
                            ab_, abk = WA()
                            S.op('dve', lambda e: e.tensor_tensor(ab_[:], et[:], xt_[:], ALU.mult), reads=[etk, xtk], writes=[abk])
                            if diag:
                                S.op('dve', lambda e: e.tensor_tensor(
                                    ab_[:].rearrange("p (h c) -> p h c", c=128), ab_[:].rearrange("p (h c) -> p h c", c=128),
                                    ct['triS'][:].unsqueeze(1).to_broadcast([128, 4, 128]), ALU.mult),
                                    reads=[abk, 'c_triS'], writes=[abk])
                            av(acc, acck, ab_, abk, lambda h: va[:, kb, h * 64:(h + 1) * 64], ('va', kb), diag, kb == 0, 64)
                            if kb == 0:
                                store_mixed(m, 0, lambda: acc[:, 0:256], [acck], zt, ztk, i)
                        return [s0, s1, s2]
                    run_pipeline(qs_iters(wq_, wqk, 0, qT2s[0], 'qT2_0', zts[0], 'zta_0', do_rope=False), 4)
                    for qg in range(nq_groups):
                        b_ = qg % 2
                        st = {'lsum': None, 'lsumk': None}
                        its = []
                        for i in range(4):
                            m = qg * 4 + i
                            for kb in range(m, -1, -1):
                                its.append(a_iter(m, i, kb, st, qT2s[b_], f'qT2_{b_}', zts[b_], f'zta_{b_}'))
                        extra = qs_iters(wq_, wqk, qg + 1, qT2s[1 - b_], f'qT2_{1 - b_}', zts[1 - b_], f'zta_{1 - b_}', do_rope=False) if qg + 1 < nq_groups else []
                        run_pipeline(interleave(its, extra), 4)
                    S.barrier(drop_prefixes=('kaT', 'va', 'qT2', 'zta', 'wa'))

            if 'B' in mixers:
                with ExitStack() as mbs:
                    kT = T("kbT", [128, 2, S_LEN], BF16, mbs)
                    vaug = T("vbaug", [128, NSLOT, 4, 65], BF16, mbs)
                    qT2s = [T(f"qT2b{b_}", [128, 2, 4, 2, 128], BF16, mbs) for b_ in range(2)]
                    zts = [T(f"ztb{b_}", [128, 4, 256], BF16, mbs) for b_ in range(2)]
                    for b_ in range(2):
                        S.op('pool', lambda e, b_=b_: e.memset(qT2s[b_][:].rearrange("p a b c d -> p (a b c d)"), 0.0), writes=[(f'qT2_{b_}', i, j) for i in range(4) for j in range(2)])
                    S.op('pool', lambda e: e.memset(vaug[:].rearrange("p a b c -> p (a b c)"), 1.0), writes=[('vaug', s_) for s_ in range(NSLOT)])
                    wk_, wkk = load_wgroup(l, 2)
                    wq_, wqk = load_wgroup(l, 3)
                    wload(8 * l + 4)
                    def rk_iter(slot, kT_, kTkey, vaug_, wk__, wkk__):
                        c = {}

                        def s0():
                            c['ps'], c['pk'] = proj_slot(slot, wk__, wkk__)

                        def s1():
                            ps, pk = c['ps'], c['pk']
                            S.op('act', lambda e: e.copy(vaug_[:, slot, :, 0:64], ps[:, 256:512].rearrange("p (h c) -> p h c", c=64)),
                                 reads=[pk], writes=[('vaug', slot)])
                            c['kb'], c['kbk'] = WB()
                            rope(ps[:, 0:256].rearrange("p (h c) -> p h c", c=64), pk,
                                 c['kb'][:, 0:256].rearrange("p (h c) -> p h c", c=64), c['kbk'], 4,
                                 ct['rcc'][:, slot, :], ct['rss'][:, slot, :], ['c_rcc', 'c_rss'])

                        def s2():
                            c['pt'], c['ptk'] = qk_transpose(c['kb'], [c['kbk']], None, None, 2)

                        def s3():
                            S.op('dve', lambda e: e.tensor_copy(kT_[:, :, slot * 128:(slot + 1) * 128], c['pt'][:, 0:2, :]),
                                 reads=[c['ptk']], writes=[(kTkey, slot)])
                        return [s0, s1, s2, s3]
                    run_pipeline([rk_iter(slot, kT, 'kT', vaug, wk_, wkk) for slot in range(NSLOT)], 4)
                    wload(8 * l + 5)
                    def b_iter(m, i, d, dmax, qT2, qT2k, zt, ztk):
                        c = {}
                        acc, acck = pA[m % 2], f"pA{m % 2}"
                        kb = m - d

                        def s0():
                            ps, pk = PR()
                            c['ps'], c['pk'] = ps, pk

                            def f(e):
                                for p in range(2):
                                    e.matmul(ps[:, p * 256:(p + 1) * 256], kT[:, p, kb * 128:(kb + 1) * 128],
                                             qT2[:, p, i, :, :].rearrange("p a b -> p (a b)"), start=(p == 0), stop=False, skip_group_check=True)
                                for h in range(4):
                                    ins = e.matmul(ps[:, h * 128:(h + 1) * 128], ident[:], ct['lmbh'][:, d, :], start=False, stop=(d > 4 and h == 3), skip_group_check=True)
                                if d <= 4:
                                    for h in range(4):
                                        ins = e.matmul(ps[:, h * 128:(h + 1) * 128], ident[:], ct['lmbl'][:, d, :], start=False, stop=(h == 3), skip_group_check=True)
                                return ins
                            S.op('pe', f, reads=[('kT', kb), (qT2k, i, 0), (qT2k, i, 1), 'c_ident', 'c_lmbh', 'c_lmbl'], writes=[pk])

                        def s1():
                            c['pb'], c['pbk'] = WB()
                            S.op('act', lambda e: e.activation(c['pb'][:], c['ps'][:], AF.Exp, scale=SCALE), reads=[c['pk']], writes=[c['pbk']])

                        def s2():
                            av(acc, acck, c['pb'], c['pbk'], lambda h: vaug[:, kb, h, :], ('vaug', kb), d == 0, d == dmax, 65)
                            if d == dmax:
                                o, ok = normalize(acc, acck)
                                store_mixed(m, 256, lambda: o[:, 0:256], [ok], zt, ztk, i)
                        return [s0, s1, s2]
                    run_pipeline(qs_iters(wq_, wqk, 0, qT2s[0], 'qT2_0', zts[0], 'ztb_0', do_rope=True), 4)
                    for qg in range(nq_groups):
                        b_ = qg % 2
                        its = []
                        for i in range(4):
                            m = qg * 4 + i
                            dmax = min(16, m)
                            for d in range(0, dmax + 1):
                                its.append(b_iter(m, i, d, dmax, qT2s[b_], f'qT2_{b_}', zts[b_], f'ztb_{b_}'))
                        extra = qs_iters(wq_, wqk, qg + 1, qT2s[1 - b_], f'qT2_{1 - b_}', zts[1 - b_], f'ztb_{1 - b_}', do_rope=True) if qg + 1 < nq_groups else []
                        run_pipeline(interleave(its, extra), 4)
                    S.barrier(drop_prefixes=('kT', 'vaug', 'qT2', 'ztb'))

            if 'C' in mixers:
                with ExitStack() as mcs:
                    kcrT = T("kcrT", [64, S_LEN], BF16, mcs)
                    vcrT = T("vcrT", [64, S_LEN], BF16, mcs)
                    kvT = [kcrT, vcrT]
                    ksT = T("ksT", [128, S_LEN], BF16, mcs)
                    kwT = T("kwT", [128, S_LEN], BF16, mcs)
                    vsa = T("vsa", [128, NSLOT, 65], BF16, mcs)
                    vwa = T("vwa", [128, NSLOT, 65], BF16, mcs)
                    gsig = T("gsig", [128, NSLOT, 12], F32, mcs)
                    kcT = T("kcT", [128, 256], BF16, mcs)
                    vcE = T("vcE", [128, 2, 128], BF16, mcs)
                    qT2c = T("qT2c", [128, 2, 4, 2, 128], BF16, mcs)
                    S.op('pool', lambda e: e.memset(qT2c[:].rearrange("p a b c d -> p (a b c d)"), 0.0), writes=[('qT2c', i, j) for i in range(4) for j in range(2)])
                    zt = T("ztc", [128, 4, 256], BF16, mcs)
                    w1bs = [T(f"w1b{j}", [64, 2, 8, 256], BF16, mcs) for j in range(2)]
                    w2b = T("w2b", [128, 2, 2, 64], BF16, mcs)
                    b1t = T("b1t", [128, 4], F32, mcs)
                    posb = T("posb", [64, 2, 32], BF16, mcs)
                    biasv = T("biasv", [128, 4], F32, mcs)
                    hidT = T("hidT", [128, 4, 256], BF16, mcs)
                    ocacc = T("ocacc", [128, 256], F32, mcs)
                    impt = T("impt", [128, 64], F32, mcs)
                    imps = T("imps", [128, 64], F32, mcs)
                    selm = T("selm", [128, 64], BF16, mcs)
                    mx8 = T("mx8", [128, 16], F32, mcs)
                    cf = T("cf", [128, 16], F32, mcs)
                    S.op('pool', lambda e: e.memset(vsa[:].rearrange("p a b -> p (a b)"), 1.0), writes=[('vsa', s_) for s_ in range(NSLOT)])
                    S.op('pool', lambda e: e.memset(vwa[:].rearrange("p a b -> p (a b)"), 1.0), writes=[('vwa', s_) for s_ in range(NSLOT)])
                    wk_, wkk = load_wgroup(l, 4)
                    wq_, wqk = load_wgroup(l, 5)
                    wload(8 * l + 6)
                    for t in range(2):
                        S.dma('pool', w2b[:, t, :, :], w2_d[l, t].rearrange("(c p) d -> p c d", p=128), f'cw{t}', writes=['w2b'])
                    S.dma('sp', b1t[:], b1_d[l], 'cw2', writes=['b1t'])
                    S.dma('pool', posb[:], posT_d[l], 'cw3', writes=['posb'])
                    S.dma('sp', vcE[:, :, 64:128], cd['ovl'], 'cw4', writes=[('vcE', 'ov')])
                    def ck_iter(slot):
                        c = {}
                        tsl = slice(slot * 128, (slot + 1) * 128)

                        def s0():
                            c['ps'], c['pk'] = proj_slot(slot, wk_, wkk, ncols=396)

                        def s1():
                            ps, pk = c['ps'], c['pk']
                            S.op('act', lambda e: e.copy(vsa[:, slot, 0:64], ps[:, 256:320]), reads=[pk], writes=[('vsa', slot)])
                            S.op('act', lambda e: e.copy(vwa[:, slot, 0:64], ps[:, 320:384]), reads=[pk], writes=[('vwa', slot)])
                            S.op('act', lambda e: e.activation(gsig[:, slot, :], ps[:, 384:396], AF.Sigmoid), reads=[pk], writes=[('gsig', slot)])
                            kb_, kbk = WB()
                            c['kb'], c['kbk'] = kb_, kbk
                            S.op('dve', lambda e: e.tensor_copy(kb_[:, 0:128], ps[:, 0:128]), reads=[pk], writes=[kbk])
                            kdup = kb_[:, 128:384].rearrange("p (h c) -> p h c", c=128)
                            rope(ps[:, 128:256].rearrange("p (h c) -> p h c", c=64), pk,
                                 kdup[:, :, 0:64], kbk, 2,
                                 ct['rcc'][:, slot, :], ct['rss'][:, slot, :], ['c_rcc', 'c_rss'])
                            S.op('dve', lambda e: e.tensor_copy(kdup[:, :, 64:128], kdup[:, :, 0:64]), reads=[kbk], writes=[kbk])

                        def s2():
                            kb_, kbk = c['kb'], c['kbk']
                            pt, ptk = PT()
                            c['pt'], c['ptk'] = pt, ptk

                            def f(e):
                                e.transpose(pt[0:64, 0, :], kb_[:, 0:64], ident[:])
                                e.transpose(pt[0:64, 3, :], kb_[:, 64:128], ident[:])
                                e.transpose(pt[:, 1, :], kb_[:, 128:256], ident[:])
                                return e.transpose(pt[:, 2, :], kb_[:, 256:384], ident[:])
                            S.op('pe', f, reads=[kbk, 'c_ident'], writes=[ptk])

                        def s3():
                            pt, ptk = c['pt'], c['ptk']
                            S.op('dve', lambda e: e.tensor_copy(kcrT[:, tsl], pt[0:64, 0, :]), reads=[ptk], writes=[('kcrT', slot)])
                            S.op('act', lambda e: e.copy(vcrT[:, tsl], pt[0:64, 3, :]), reads=[ptk], writes=[('vcrT', slot)])
                            S.op('act', lambda e: e.copy(ksT[:, tsl], pt[:, 1, :]), reads=[ptk], writes=[('ksT', slot)])
                            S.op('dve', lambda e: e.tensor_copy(kwT[:, tsl], pt[:, 2, :]), reads=[ptk], writes=[('kwT', slot)])
                        return [s0, s1, s2, s3]
                    run_pipeline([ck_iter(slot) for slot in range(NSLOT)], 4)
                    wload(8 * l + 7)
                    allkv = [[('kcrT', s_) for s_ in range(NSLOT)], [('vcrT', s_) for s_ in range(NSLOT)]]
                    bps, bpk = PR()
                    kv3 = [kvT[t][:].rearrange("p (n r) -> p n r", r=16) for t in range(2)]
                    for piece in range(4):
                        w1b = w1bs[piece % 2]
                        w1k = f"w1b{piece % 2}"
                        for t in range(2):
                            S.dma('pool', w1b[:, t, :, :], w1_d[l, t, piece * 512:(piece + 1) * 512, :].rearrange("(l d) c -> d l c", d=64),
                                  f"w1l{piece % 2}{t}", writes=[(w1k, t)])
                        for t in range(2):
                            def f(e, t=t, piece=piece, w1b=w1b):
                                ins = None
                                for li in range(8):
                                    lg = piece * 8 + li
                                    for c in range(2):
                                        o_ = pA[t][:, c * 256:(c + 1) * 256]
                                        w_ = w1b[:, t, li, c * 128:(c + 1) * 128]
                                        first = (lg == 0 and c == 0)
                                        last = (lg == 31)
                                        if lg < 16:
                                            ins = e.matmul(o_, w_, kv3[t][:, 0:256, lg], start=first, stop=False, skip_group_check=True)
                                        else:
                                            ins = e.matmul(o_[:, 0:255], w_, kv3[t][:, 1:256, lg - 16], start=False, stop=last, skip_group_check=True)
                                        ins = e.matmul(bps[:, (t * 2 + c):(t * 2 + c) + 1], w_, posb[:, t, lg:lg + 1],
                                                       start=(t == 0 and first), stop=last, skip_group_check=True)
                                return ins
                            S.op('pe', f, reads=allkv[t] + [(w1k, t), 'posb'], writes=[f"pA{t}", bpk])
                    S.op('dve', lambda e: e.tensor_tensor(biasv[:], bps[:, 0:4], b1t[:], ALU.add), reads=[bpk, 'b1t'], writes=['biasv'])
                    for t in range(2):
                        for c in range(2):
                            j = t * 2 + c
                            (xs_, xsk), (x2_, x2k), (u__, uk) = WF(), WF(), WF()
                            xs, x2, u_ = xs_[:, 0:256], x2_[:, 0:256], u__[:, 0:256]
                            S.op('dve', lambda e, t=t, c=c, j=j, xs=xs: e.tensor_scalar(out=xs, in0=pA[t][:, c * 256:(c + 1) * 256], scalar1=biasv[:, j:j + 1], scalar2=None, op0=ALU.add),
                                 reads=[f"pA{t}", 'biasv'], writes=[xsk])
                            S.op('dve', lambda e, xs=xs, x2=x2: e.tensor_tensor(x2, xs, xs, ALU.mult), reads=[xsk], writes=[x2k])
                            S.op('dve', lambda e, x2=x2: e.tensor_scalar(out=x2, in0=x2, scalar1=0.044715, scalar2=1.0, op0=ALU.mult, op1=ALU.add), reads=[x2k], writes=[x2k])
                            S.op('dve', lambda e, xs=xs, x2=x2, u_=u_: e.tensor_tensor(u_, x2, xs, ALU.mult), reads=[x2k, xsk], writes=[uk])
                            S.op('act', lambda e, u_=u_: e.activation(u_, u_, AF.Sigmoid, scale=1.5957691216057308), reads=[uk], writes=[uk])
                            S.op('dve', lambda e, j=j, xs=xs, u_=u_: e.tensor_tensor(hidT[:, j, :], xs, u_, ALU.mult), reads=[xsk, uk], writes=[('hidT', j)])
                    for nchunk in range(2):
                        ps, pk = PR()

                        def f(e, ps=ps, nchunk=nchunk):
                            for t in range(2):
                                for c in range(2):
                                    ins = e.matmul(ps[:, t * 64:(t + 1) * 64], hidT[:, t * 2 + c, nchunk * 128:(nchunk + 1) * 128], w2b[:, t, c, :],
                                                   start=(c == 0), stop=(c == 1))
                            return ins
                        S.op('pe', f, reads=[('hidT', j) for j in range(4)] + ['w2b'], writes=[pk])
                        S.op('act', lambda e, ps=ps, nchunk=nchunk: e.copy(vcE[:, nchunk, 0:64], ps[:, 64:128]), reads=[pk], writes=[('vcE', nchunk)])
                        kb_, kbk = WB()
                        rope(ps[:, 0:64].rearrange("p (h c) -> p h c", c=64), pk,
                             kb_[:, 0:64].rearrange("p (h c) -> p h c", c=64), kbk, 1,
                             ct['rccc'][:, nchunk, :], ct['rssc'][:, nchunk, :], ['c_rccc', 'c_rssc'])
                        S.op('dve', lambda e, kb_=kb_: e.tensor_copy(kb_[:, 64:128], kb_[:, 0:64]), reads=[kbk], writes=[kbk])
                        pt, ptk = PT()
                        S.op('pe', lambda e, pt=pt, kb_=kb_: e.transpose(pt[:, 0, :], kb_[:, 0:128], ident[:]),
                             reads=[kbk, 'c_ident'], writes=[ptk])
                        S.op('dve', lambda e, pt=pt, nchunk=nchunk: e.tensor_copy(kcT[:, nchunk * 128:(nchunk + 1) * 128], pt[:, 0, :]),
                             reads=[ptk], writes=[('kcT', nchunk)])
                    for qg in range(nq_groups):
                        run_pipeline(qs_iters(wq_, wqk, qg, qT2c, 'qT2c', zt, 'ztc', do_rope=True), 4)
                        for i in range(4):
                            m = qg * 4 + i
                            qv = None
                            acc, acck = pA[0], 'pA0'
                            nch = 1 if m < 16 else 2
                            for c in range(nch):
                                ps, pk = PR()
                                def fc(e, ps=ps, c=c, i=i):
                                    for p in range(2):
                                        ins = e.matmul(ps[:, p * 256:(p + 1) * 256], kcT[:, c * 128:(c + 1) * 128],
                                                       qT2c[:, p, i, :, :].rearrange("p a b -> p (a b)"), start=True, stop=True)
                                    return ins
                                S.op('pe', fc, reads=[('kcT', c), ('qT2c', i, 0), ('qT2c', i, 1)], writes=[pk])
                                pb, pbk = WB()
                                S.op('act', lambda e, pb=pb, ps=ps: e.activation(pb[:], ps[:], AF.Exp, scale=SCALE), reads=[pk], writes=[pbk])
                                u = m - 16 * c
                                if u < 17:
                                    S.op('dve', lambda e, pb=pb, u=u: e.tensor_tensor(
                                        pb[:].rearrange("p (h c) -> p h c", c=128), pb[:].rearrange("p (h c) -> p h c", c=128),
                                        ct['cv'][:, u, :].unsqueeze(1).to_broadcast([128, 4, 128]), ALU.mult),
                                        reads=[pbk, 'c_cv'], writes=[pbk])
                                av(acc, acck, pb, pbk, lambda h, c=c: vcE[:, c, :], [('vcE', c), ('vcE', 'ov')], c == 0, c == nch - 1, 128)
                            accv = acc[:].rearrange("p (h c) -> p h c", c=128)
                            S.op('dve', lambda e, accv=accv: e.tensor_reduce(out=cf[:, 0:4], in_=accv[:, :, 64:128], axis=AX.X, op=ALU.add), reads=[acck, ('vcE', 'ov')], writes=[('cf', 0)])
                            S.op('dve', lambda e: e.tensor_scalar(out=cf[:, 0:4], in0=cf[:, 0:4], scalar1=1e-30, scalar2=None, op0=ALU.max), reads=[('cf', 0)], writes=[('cf', 0)])
                            S.op('dve', lambda e: e.reciprocal(cf[:, 4:8], cf[:, 0:4]), reads=[('cf', 0)], writes=[('cf', 1)])
                            for h in range(4):
                                if h == 0:
                                    S.op('dve', lambda e, accv=accv: e.tensor_scalar(out=impt[:], in0=accv[:, 0, 64:128], scalar1=cf[:, 4:5], scalar2=None, op0=ALU.mult),
                                         reads=[acck, ('cf', 1)], writes=['impt'])
                                else:
                                    S.op('dve', lambda e, accv=accv, h=h: e.scalar_tensor_tensor(out=impt[:], in0=accv[:, h, 64:128], scalar=cf[:, 4 + h:5 + h], in1=impt[:], op0=ALU.mult, op1=ALU.add),
                                         reads=[acck, ('cf', 1), 'impt'], writes=['impt'])
                            gv = gsig[:, m, :].rearrange("p (h b) -> p h b", b=3)
                            S.op('dve', lambda e, gv=gv: e.tensor_tensor(cf[:, 8:12], cf[:, 4:8], gv[:, :, 0], ALU.mult), reads=[('cf', 1), ('gsig', m)], writes=[('cf', 2)])
                            ocv = ocacc[:].rearrange("p (h c) -> p h c", c=64)
                            S.op('dve', lambda e, accv=accv, ocv=ocv: e.tensor_tensor(ocv, accv[:, :, 0:64], cf[:, 8:12].unsqueeze(2).to_broadcast([128, 4, 64]), ALU.mult),
                                 reads=[acck, ('cf', 2)], writes=['ocacc'])
                            vsl = ct['vt'][:, 64 - 2 * m:128 - 2 * m]
                            if m <= 7:
                                S.op('dve', lambda e, vsl=vsl: e.tensor_copy(selm[:], vsl), reads=['c_vt'], writes=['selm'])
                            else:
                                t1s = ct['t1'][:, 64 - 2 * m:128 - 2 * m]
                                t2s = ct['t2'][:, 64 - 2 * m:128 - 2 * m]
                                S.op('dve', lambda e, t1s=t1s: e.tensor_tensor(imps[:], impt[:], t1s, ALU.mult), reads=['impt', 'c_t1'], writes=['imps'])
                                S.op('dve', lambda e, t2s=t2s: e.tensor_tensor(imps[:], imps[:], t2s, ALU.add), reads=['imps', 'c_t2'], writes=['imps'])
                                S.op('dve', lambda e: e.memset(imps[:, 0:1], BIG), reads=['imps'], writes=['imps'])
                                S.op('dve', lambda e: e.max(out=mx8[:, 0:8], in_=imps[:]), reads=['imps'], writes=[('mx8', 0)])
                                S.op('dve', lambda e: e.match_replace(out=impt[:], in_to_replace=mx8[:, 0:8], in_values=imps[:], imm_value=-3e38),
                                     reads=[('mx8', 0), 'imps'], writes=['impt'])
                                S.op('dve', lambda e: e.max(out=mx8[:, 8:16], in_=impt[:]), reads=['impt'], writes=[('mx8', 1)])
                                S.op('dve', lambda e: e.tensor_scalar(out=imps[:], in0=imps[:], scalar1=mx8[:, 15:16], scalar2=None, op0=ALU.is_ge),
                                     reads=['imps', ('mx8', 1)], writes=['imps'])
                                S.op('dve', lambda e, vsl=vsl: e.tensor_tensor(selm[:], imps[:], vsl, ALU.mult), reads=['imps', 'c_vt'], writes=['selm'])
                            def c_fin(acc, acck, gcol, m=m, gv=gv):
                                accv = acc[:, 0:260].rearrange("p (h c) -> p h c", c=65)
                                S.op('dve', lambda e: e.reciprocal(cf[:, 12:16], accv[:, :, 64]), reads=[acck], writes=[('cf', 3)])
                                S.op('dve', lambda e: e.tensor_tensor(cf[:, 12:16], cf[:, 12:16], gv[:, :, gcol], ALU.mult), reads=[('cf', 3), ('gsig', m)], writes=[('cf', 3)])
                                tmpo, tmpk = WF()
                                S.op('dve', lambda e: e.tensor_tensor(tmpo[:, 0:256].rearrange("p (h c) -> p h c", c=64), accv[:, :, 0:64],
                                                                      cf[:, 12:16].unsqueeze(2).to_broadcast([128, 4, 64]), ALU.mult),
                                     reads=[acck, ('cf', 3)], writes=[tmpk])
                                S.op('pool', lambda e: e.tensor_tensor(ocacc[:], ocacc[:], tmpo[:, 0:256], ALU.add), reads=['ocacc', tmpk], writes=['ocacc'])

                            def c_iter(kind, kb, first, last, m=m, i=i, qv=qv, c_fin=c_fin):
                                c = {}
                                sel = (kind == 'sel')
                                acc, acck = (pA[1], 'pA1') if sel else (pA[0], 'pA0')
                                kT_, kTk = (ksT, 'ksT') if sel else (kwT, 'kwT')
                                vA_, vAk = (vsa, 'vsa') if sel else (vwa, 'vwa')
                                d = m - kb
                                bias = None
                                if (sel and kb == m) or ((not sel) and d == 0):
                                    bias = 'ntri4'
                                elif (not sel) and d == 4:
                                    bias = 'nw44'

                                def s0():
                                    ps, pk = PR()
                                    c['ps'], c['pk'] = ps, pk

                                    def f(e):
                                        for p in range(2):
                                            ins = e.matmul(ps[:, p * 256:(p + 1) * 256], kT_[:, kb * 128:(kb + 1) * 128],
                                                           qT2c[:, p, i, :, :].rearrange("p a b -> p (a b)"), start=(p == 0), stop=(p == 1 and not bias),
                                                           skip_group_check=True)
                                        if bias:
                                            ins = e.matmul(ps[:], ident[:], ct[bias][:], start=False, stop=True, skip_group_check=True)
                                        return ins
                                    S.op('pe', f, reads=[(kTk, kb), ('qT2c', i, 0), ('qT2c', i, 1), 'c_ident'] + (['c_' + bias] if bias else []), writes=[pk])
                                    if sel:
                                        mp, mpk = PR()
                                        c['mp'], c['mpk'] = mp, mpk

                                        def fm(e):
                                            e.matmul(mp[0:64, 0:128], selm[:, 2 * kb:2 * kb + 1].to_broadcast([128, 64]), ident[:], start=True, stop=True)
                                            return e.matmul(mp[64:128, 0:128], selm[:, 2 * kb + 1:2 * kb + 2].to_broadcast([128, 64]), ident[:], start=True, stop=True)
                                        S.op('pe', fm, reads=['selm', 'c_ident'], writes=[mpk])

                                def s1():
                                    c['pb'], c['pbk'] = WB()
                                    pb, pbk = c['pb'], c['pbk']
                                    S.op('act', lambda e: e.activation(pb[:], c['ps'][:], AF.Exp, scale=SCALE), reads=[c['pk']], writes=[pbk])
                                    if sel:
                                        S.op('dve', lambda e: e.tensor_tensor(
                                            pb[:].rearrange("p (h c) -> p h c", c=128), pb[:].rearrange("p (h c) -> p h c", c=128),
                                            c['mp'][:, 0:128].unsqueeze(1).to_broadcast([128, 4, 128]), ALU.mult),
                                            reads=[pbk, c['mpk']], writes=[pbk])

                                def s2():
                                    av(acc, acck, c['pb'], c['pbk'], lambda h: vA_[:, kb, :], (vAk, kb), first, last, 65)
                                    if last:
                                        c_fin(acc, acck, 1 if sel else 2)
                                        if not sel:
                                            store_mixed(m, 512, lambda: ocacc[:], ['ocacc'], zt, 'ztc', i)
                                return [s0, s1, s2]
                            its = [c_iter('sel', kb, kb == 0, kb == m) for kb in range(0, m + 1)]
                            dmax = min(4, m)
                            its += [c_iter('win', m - d, d == 0, d == dmax) for d in range(0, dmax + 1)]
                            run_pipeline(its, 3)
                    S.barrier(drop_prefixes=('kcrT', 'vcrT', 'ksT', 'kwT', 'vsa', 'vwa', 'gsig', 'kcT', 'vcE', 'qT2c', 'ztc', 'w1b', 'w2', 'b1t', 'pos',
                                             'biasv', 'hidT', 'gx', 'ocacc', 'imp', 'selm', 'selb', 'mx8', 'cf'))

            if 'D' in mixers:
                with ExitStack() as mds:
                    kT = T("kdT", [128, 2, S_LEN], BF16, mds)
                    vaug = T("vdaug", [128, NSLOT, 4, 65], BF16, mds)
                    qT2s = [T(f"qT2d{b_}", [128, 2, 4, 2, 128], BF16, mds) for b_ in range(2)]
                    zts = [T(f"ztd{b_}", [128, 4, 256], BF16, mds) for b_ in range(2)]
                    kmf = T("kmf", [128, 2, 16], F32, mds)
                    kmb = T("kmb", [128, 2, 16], BF16, mds)
                    gm = T("gm", [128, 4, 16], F32, mds)
                    gmx = T("gmx", [128, 4, 8], F32, mds)
                    isel = T("isel", [128, 4, 16], BF16, mds)
                    for b_ in range(2):
                        S.op('pool', lambda e, b_=b_: e.memset(qT2s[b_][:].rearrange("p a b c d -> p (a b c d)"), 0.0), writes=[(f'qT2_{b_}', i, j) for i in range(4) for j in range(2)])
                    S.op('pool', lambda e: e.memset(vaug[:].rearrange("p a b c -> p (a b c)"), 1.0), writes=[('vaug', s_) for s_ in range(NSLOT)])
                    wk_, wkk = load_wgroup(l, 6)
                    wq_, wqk = load_wgroup(l, 7)
                    wload(8 * l + 8)
                    def rk_iter(slot, kT_, kTkey, vaug_, wk__, wkk__):
                        c = {}

                        def s0():
                            c['ps'], c['pk'] = proj_slot(slot, wk__, wkk__)

                        def s1():
                            ps, pk = c['ps'], c['pk']
                            S.op('act', lambda e: e.copy(vaug_[:, slot, :, 0:64], ps[:, 256:512].rearrange("p (h c) -> p h c", c=64)),
                                 reads=[pk], writes=[('vaug', slot)])
                            c['kb'], c['kbk'] = WB()
                            rope(ps[:, 0:256].rearrange("p (h c) -> p h c", c=64), pk,
                                 c['kb'][:, 0:256].rearrange("p (h c) -> p h c", c=64), c['kbk'], 4,
                                 ct['rcc'][:, slot, :], ct['rss'][:, slot, :], ['c_rcc', 'c_rss'])

                        def s2():
                            c['pt'], c['ptk'] = qk_transpose(c['kb'], [c['kbk']], None, None, 2)

                        def s3():
                            S.op('dve', lambda e: e.tensor_copy(kT_[:, :, slot * 128:(slot + 1) * 128], c['pt'][:, 0:2, :]),
                                 reads=[c['ptk']], writes=[(kTkey, slot)])
                        return [s0, s1, s2, s3]
                    run_pipeline([rk_iter(slot, kT, 'kT', vaug, wk_, wkk) for slot in range(NSLOT)], 4)
                    wload(8 * l + 9)
                    allk = [('kT', s_) for s_ in range(NSLOT)]
                    for p in range(2):
                        S.op('dve', lambda e, p=p: e.tensor_reduce(out=kmf[:, p, :], in_=kT[:, p, :].rearrange("p (n r) -> p n r", r=256), axis=AX.X, op=ALU.add),
                             reads=allk, writes=[('kmf', p)])
                    S.op('dve', lambda e: e.tensor_scalar(out=kmb[:], in0=kmf[:], scalar1=1.0 / 256, scalar2=None, op0=ALU.mult),
                         reads=[('kmf', 0), ('kmf', 1)], writes=['kmb'])
                    isel2 = [isel, T("isel2", [128, 4, 16], BF16, mds)]

                    def d_prep(m, i, own, qT2, qT2k):
                        isl = isel2[m % 2]
                        islk = f"isel{m % 2}"
                        gp_, gpk = PR()

                        def fg(e):
                            for h in range(4):
                                ins = e.matmul(gp_[:, h * 16:(h + 1) * 16], qT2[:, h // 2, i, h % 2, :], kmb[:, h // 2, :], start=True, stop=True)
                            return ins
                        S.op('pe', fg, reads=[(qT2k, i, 0), (qT2k, i, 1), 'kmb'], writes=[gpk])
                        S.op('dve', lambda e: e.memset(gm[:].rearrange("p a b -> p (a b)"), NEGB), writes=['gm'])
                        S.op('dve', lambda e: e.tensor_copy(gm[:, :, 0:own], gp_[:, 0:64].rearrange("p (h n) -> p h n", n=16)[:, :, 0:own]),
                             reads=[gpk, 'gm'], writes=['gm'])
                        for h in range(4):
                            S.op('dve', lambda e, h=h: e.max(out=gmx[:, h, :], in_=gm[:, h, :]), reads=['gm'], writes=[('gmx', h)])
                        for h in range(4):
                            S.op('dve', lambda e, h=h: e.tensor_scalar(out=gm[:, h, :], in0=gm[:, h, :], scalar1=gmx[:, h, 2:3], scalar2=None, op0=ALU.is_ge),
                                 reads=['gm', ('gmx', h)], writes=['gm'])
                        S.op('dve', lambda e: e.tensor_scalar(out=isl[:].rearrange("p a b -> p (a b)"), in0=gm[:].rearrange("p a b -> p (a b)"), scalar1=-1.0, scalar2=240.0, op0=ALU.add, op1=ALU.mult),
                             reads=['gm'], writes=[islk])

                    def d_iter(m, i, kb, mt, first, last, own, qT2, qT2k, zt, ztk):
                        c = {}
                        acc, acck = pA[m % 2], f"pA{m % 2}"
                        isl = isel2[m % 2]
                        islk = f"isel{m % 2}"

                        def s0():
                            if first and own > 3:
                                d_prep(m, i, own, qT2, qT2k)
                            ps, pk = PR()
                            c['ps'], c['pk'] = ps, pk
                            n_ = kb // 2

                            def f(e):
                                for p in range(2):
                                    ins = e.matmul(ps[:, p * 256:(p + 1) * 256], kT[:, p, kb * 128:(kb + 1) * 128],
                                                   qT2[:, p, i, :, :].rearrange("p a b -> p (a b)"), start=(p == 0), stop=(mt == 'none' and p == 1),
                                                   skip_group_check=True)
                                if mt == 'sel':
                                    for h in range(4):
                                        ins = e.matmul(ps[:, h * 128:(h + 1) * 128], isl[:, h, n_:n_ + 1].to_broadcast([128, 128]), ident[:],
                                                       start=False, stop=(h == 3), skip_group_check=True)
                                elif mt == 'diag':
                                    ins = e.matmul(ps[:], ident[:], ct['ntri4'][:], start=False, stop=True, skip_group_check=True)
                                return ins
                            S.op('pe', f, reads=[('kT', kb), (qT2k, i, 0), (qT2k, i, 1), 'c_ident', 'c_ntri4'] + ([islk] if mt == 'sel' else []), writes=[pk])

                        def s1():
                            c['pb'], c['pbk'] = WB()
                            S.op('act', lambda e: e.activation(c['pb'][:], c['ps'][:], AF.Exp, scale=SCALE), reads=[c['pk']], writes=[c['pbk']])

                        def s2():
                            av(acc, acck, c['pb'], c['pbk'], lambda h: vaug[:, kb, h, :], ('vaug', kb), first, last, 65)
                            if last:
                                o, ok = normalize(acc, acck)
                                store_mixed(m, 768, lambda: o[:, 0:256], [ok], zt, ztk, i)
                        return [s0, s1, s2]
                    run_pipeline(qs_iters(wq_, wqk, 0, qT2s[0], 'qT2_0', zts[0], 'ztd_0', do_rope=True), 4)
                    for qg in range(nq_groups):
                        b_ = qg % 2
                        its = []
                        for i in range(4):
                            m = qg * 4 + i
                            own = m // 2
                            steps = [(kb, 'sel' if own > 3 else 'none') for kb in range(0, 2 * own)]
                            if m % 2 == 1:
                                steps.append((m - 1, 'none'))
                            steps.append((m, 'diag'))
                            for si_, (kb, mt) in enumerate(steps):
                                its.append(d_iter(m, i, kb, mt, si_ == 0, si_ == len(steps) - 1, own, qT2s[b_], f'qT2_{b_}', zts[b_], f'ztd_{b_}'))
                        extra = qs_iters(wq_, wqk, qg + 1, qT2s[1 - b_], f'qT2_{1 - b_}', zts[1 - b_], f'ztd_{1 - b_}', do_rope=True) if qg + 1 < nq_groups else []
                        run_pipeline(interleave(its, extra), 4)
                    S.barrier(drop_prefixes=('kT', 'vaug', 'qT2', 'ztd', 'kmf', 'kmb', 'gm', 'isel'))

            with ExitStack() as ps3:
                if dbg and (mixers != 'ABCD' or nq_groups != 8):
                    break
                wo = T("wo", [128, 8, D], BF16, ps3)
                mx = [T(f"mxl{i}", [128, D], BF16, ps3) for i in range(2)]
                mT = [T(f"mTl{i}", [128, 8, 128], BF16, ps3) for i in range(2)]
                xr = [T(f"xr{i}", [128, D], F32, ps3) for i in range(2)]
                ot = [T(f"ot{i}", [128, D], F32, ps3) for i in range(2)]
                junk3 = T("junk3", [128, D], BF16, ps3)
                st3 = [T(f"st3_{i}", [128, 8], F32, ps3) for i in range(2)]
                wsrc = wout_d[l].rearrange("(k p) c -> p k c", p=128)
                for q4 in range(4):
                    S.dma('pool', wo[:, q4 * 2:(q4 + 1) * 2, :], wsrc[:, q4 * 2:(q4 + 1) * 2, :], f"wol{q4}", writes=[('wo', q4)])
                def p3_iter(slot):
                    i2 = slot % 2
                    tsl = slice(slot * 128, (slot + 1) * 128)
                    stt, stk = st3[i2], f"st3_{i2}"
                    if slot % 2:
                        (y0, y0k), (y1, y1k) = (pR[0], 'pR0'), (pR[1], 'pR1')
                    else:
                        (y0, y0k), (y1, y1k) = (pA[0], 'pA0'), (pA[1], 'pA1')

                    def s0():
                        S.dma('sp', mx[i2][:], mixed_d[tsl, :], f"mxl{i2}", reads=[('mixed', slot, c0) for c0 in (0, 256, 512, 768)], writes=[f"mxl{i2}"])

                    def s1():
                        pt, ptk = PT()

                        def f(e):
                            for k in range(8):
                                ins = e.transpose(pt[:, k, :], mx[i2][:, k * 128:(k + 1) * 128], ident[:])
                            return ins
                        S.op('pe', f, reads=[f"mxl{i2}", 'c_ident'], writes=[ptk])
                        S.op('act', lambda e: e.copy(mT[i2][:], pt[:]), reads=[ptk], writes=[f"mTl{i2}"])

                    def s2():
                        S.dma('sp', xr[i2][:], xin[tsl, :], f"xr{i2}", reads=[(xin_key, slot)], writes=[f"xr{i2}"])

                        def fy(e):
                            for hh, y in enumerate((y0, y1)):
                                for k in range(8):
                                    ins = e.matmul(y[:], mT[i2][:, k, :], wo[:, k, hh * 512:(hh + 1) * 512], start=(k == 0), stop=(k == 7))
                            return ins
                        S.op('pe', fy, reads=[f"mTl{i2}"] + [('wo', q4) for q4 in range(4)], writes=[y0k, y1k])

                    def s3():
                        S.op('act', lambda e: e.activation(junk3[:, 0:512], y0[:], AF.Square, accum_out=stt[:, 0:1]), reads=[y0k], writes=[('junk3', 0), (stk, 0)])
                        S.op('act', lambda e: e.activation(junk3[:, 512:1024], y1[:], AF.Square, accum_out=stt[:, 1:2]), reads=[y1k], writes=[('junk3', 1), (stk, 1)])
                        S.op('dve', lambda e: e.tensor_tensor(stt[:, 2:3], stt[:, 0:1], stt[:, 1:2], ALU.add), reads=[(stk, 0), (stk, 1)], writes=[(stk, 2)])
                        S.op('dve', lambda e: e.tensor_scalar(out=stt[:, 5:6], in0=stt[:, 2:3], scalar1=1.0 / D, scalar2=EPS, op0=ALU.mult, op1=ALU.add), reads=[(stk, 2)], writes=[(stk, 5)])
                        S.op('act', lambda e: e.activation(stt[:, 3:4], stt[:, 5:6], AF.Sqrt), reads=[(stk, 5)], writes=[(stk, 3)])
                        S.op('dve', lambda e: e.reciprocal(stt[:, 4:5], stt[:, 3:4]), reads=[(stk, 3)], writes=[(stk, 4)])
                        for hh, (y, yk) in enumerate(((y0, y0k), (y1, y1k))):
                            S.op('dve', lambda e, y=y, hh=hh: e.scalar_tensor_tensor(out=ot[i2][:, hh * 512:(hh + 1) * 512], in0=y[:], scalar=stt[:, 4:5],
                                                                                   in1=gpt[:, hh * 512:(hh + 1) * 512], op0=ALU.mult, op1=ALU.mult),
                                 reads=[yk, (stk, 4), 'gpt'], writes=[(f"ot{i2}", hh)])
                        S.op('pool', lambda e: e.tensor_tensor(ot[i2][:], ot[i2][:], xr[i2][:], ALU.add),
                             reads=[(f"ot{i2}", 0), (f"ot{i2}", 1), f"xr{i2}"], writes=[(f"ot{i2}", 0), (f"ot{i2}", 1)])
                        S.dma('pool', xout[tsl, :], ot[i2][:], f'outst{i2}', reads=[(f"ot{i2}", 0), (f"ot{i2}", 1)], writes=[(xout_key, slot)])
                    return [s0, s1, s2, s3]
                run_pipeline([p3_iter(slot) for slot in range(NSLOT)], 4)
                S.barrier(drop_prefixes=('wo', 'mxl', 'mTl', 'xr', 'ot', 'junk3', 'st3_'))
        S.finish()
        build.stats = dict(nins=S.nins, nwait=S.nwait, nsem=S.nsem)
    return nc


def _prep_inputs(inputs):
    f = np.float32
    x = np.asarray(inputs['x'], f)
    c = np.asarray(inputs['c'], f)
    w_in = np.asarray(inputs['w_in'], f)
    shared = {}
    shared['npre'] = np.ascontiguousarray(np.broadcast_to(np.asarray(inputs['norm_pre'], f)[:, None, :], (L, 128, D)))
    shared['npost'] = np.ascontiguousarray(np.broadcast_to(np.asarray(inputs['norm_post'], f)[:, None, :], (L, 128, D)))
    shared['bmod'] = np.ascontiguousarray(np.broadcast_to(np.asarray(inputs['b_mod'], f)[:, None, :], (L, 128, 3 * D)))
    shared['wmod'] = np.ascontiguousarray(np.asarray(inputs['w_mod'], f))
    shared['wg'] = np.ascontiguousarray(np.stack([_w_groups(w_in[l]) for l in range(L)], 0))
    shared['wout'] = np.ascontiguousarray(np.asarray(inputs['w_out'], f))
    shared['w1'] = np.ascontiguousarray(np.asarray(inputs['cmp_w1'], f))
    shared['w2'] = np.ascontiguousarray(np.asarray(inputs['cmp_w2'], f))
    b1 = np.asarray(inputs['cmp_b1'], f)
    shared['b1'] = np.ascontiguousarray(b1.reshape(L, 2, 2, 128).transpose(0, 3, 1, 2).reshape(L, 128, 4))
    pos = np.asarray(inputs['cmp_pos'], f)
    shared['posT'] = np.ascontiguousarray(pos.transpose(0, 3, 1, 2))
    for k_, v_ in _consts().items():
        shared['c_' + k_] = v_
    maps = []
    for b in range(x.shape[0]):
        m = dict(shared)
        m['x'] = np.ascontiguousarray(x[b])
        m['cT'] = np.ascontiguousarray(c[b].reshape(8, 128).T)
        maps.append(m)
    return maps


_NC_CACHE = {}


def kernel(**inputs):
    maps = _prep_inputs(inputs)
    if 'nc' not in _NC_CACHE:
        _NC_CACHE['nc'] = build()
    nc = _NC_CACHE['nc']
    res = run_bass_kernel_spmd(nc, maps, core_ids=list(range(len(maps))))
    out = np.stack([np.asarray(r['out'], np.float32) for r in res.results], 0)
    return out
```

```python
import numpy as np
from contextlib import ExitStack
import concourse.bass as bass
import concourse.mybir as mybir
from concourse.bass_utils import run_bass_kernel_spmd
import ml_dtypes

F32 = mybir.dt.float32
BF16 = mybir.dt.bfloat16
AF = mybir.ActivationFunctionType
ALU = mybir.AluOpType
AX = mybir.AxisListType

S_LEN = 4096
D = 1024
NSLOT = 32
L = 2
EPS = 1e-6
SCALE = 0.125
BIG = 1e9
NEGB = -1e30


class Sched:
    EPOCH = 20000

    def __init__(self, nc, es, same_engine_sync=True):
        self.nc = nc
        self.es = es
        self.eng = dict(pe=nc.tensor, act=nc.scalar, dve=nc.vector, pool=nc.gpsimd, sp=nc.sync)
        self.cur = {}
        self.nsem = 0
        self.bufs = {}
        self.seen = {e: {} for e in self.eng}
        self.streams = {}
        self.same = same_engine_sync
        self.nwait = 0
        self.nins = 0
        self.last_tok = {}

    def _newsem(self, name):
        self.nsem += 1
        return self.es.enter_context(self.nc.semaphore(f"s{self.nsem}_{name}"))

    def _tick(self, e):
        c = self.cur.get(e)
        if c is None or c[1] >= self.EPOCH:
            c = [self._newsem(e), 0]
            self.cur[e] = c
        c[1] += 1
        return ('c', c[0], c[1], e)

    def _wait(self, e, tok):
        if tok[0] == 'c':
            _, sem, val, src = tok
            if src == e and (e == 'pe' or not self.same):
                return
        else:
            _, st = tok
            sem = st[0]
            val = 16 * st[1]
        k = sem.name
        if self.seen[e].get(k, 0) >= val:
            return
        self.eng[e].wait_ge(sem, val)
        self.nwait += 1
        self.seen[e][k] = val

    def _deps(self, reads, writes):
        deps = []
        for r in reads:
            b = self.bufs.get(r)
            if b and b['w'] is not None:
                deps.append(b['w'])
        for w in writes:
            b = self.bufs.get(w)
            if b:
                if b['w'] is not None:
                    deps.append(b['w'])
                deps.extend(b['r'].values())
        return deps

    def _record(self, e, tok, reads, writes):
        for r in reads:
            self.bufs.setdefault(r, dict(w=None, r={}))['r'][e] = tok
        for w in writes:
            self.bufs[w] = dict(w=tok, r={})
        self.last_tok[e] = tok

    @staticmethod
    def _is_psum(k):
        return isinstance(k, str) and k[:2] in ('pR', 'pA', 'pT')

    def op(self, e, fn, reads=(), writes=()):
        psr = [r for r in reads if self._is_psum(r)]
        if psr:
            reads = [r for r in reads if not self._is_psum(r)]
            writes = list(writes) + [r for r in psr if r not in writes]
        for t in self._deps(reads, writes):
            self._wait(e, t)
        ins = fn(self.eng[e])
        tok = self._tick(e)
        ins.then_inc(tok[1], 1)
        self.nins += 1
        self._record(e, tok, reads, writes)
        return tok

    def dma(self, e, out, in_, stream, reads=(), writes=(), **kw):
        for t in self._deps(reads, writes):
            self._wait(e, t)
        st = self.streams.get(stream)
        if st is None:
            st = [self._newsem('d' + stream), 0]
            self.streams[stream] = st
        elif st[1] > 0:
            self._wait(e, ('d', st))
        ins = self.eng[e].dma_start(out=out, in_=in_, **kw)
        ins.then_inc(st[0], 16)
        st[1] += 1
        tok = ('d', st)
        self.nins += 1
        self._record('dma:' + stream, tok, reads, writes)
        return tok

    def barrier(self, drop_prefixes=()):
        toks = [t for k, t in self.last_tok.items()]
        for e in self.eng:
            for t in toks:
                self._wait(e, t)
        if drop_prefixes:
            for k in list(self.bufs.keys()):
                ks = k if isinstance(k, str) else k[0]
                if any(ks.startswith(p) for p in drop_prefixes):
                    del self.bufs[k]

    def finish(self):
        for stream, st in self.streams.items():
            self._wait('sp', ('d', st))
        for e, t in list(self.last_tok.items()):
            if not e.startswith('dma:'):
                self._wait('sp', t)


class Rot:
    def __init__(self, items):
        self.items = items
        self.i = 0

    def __call__(self):
        it = self.items[self.i % len(self.items)]
        self.i += 1
        return it


def _consts():
    bf = ml_dtypes.bfloat16
    c = {}
    k = np.arange(128)[:, None]
    q = np.arange(128)[None, :]
    c['ident'] = np.eye(128, dtype=np.float32).astype(bf)
    c['triS'] = (k < q).astype(np.float32).astype(bf)
    c['triI'] = (k <= q).astype(np.float32).astype(bf)
    c['w4'] = (q < k).astype(np.float32).astype(bf)
    c['umat'] = (k >= q).astype(np.float32).astype(bf)
    c['ones'] = np.ones((128, 128), np.float32).astype(bf)
    c['ntri4'] = np.tile(-240.0 * (1.0 - (k <= q).astype(np.float32)), (1, 4)).astype(bf)
    c['nw44'] = np.tile(-240.0 * (1.0 - (q < k).astype(np.float32)), (1, 4)).astype(bf)
    mb = np.zeros((128, 17, 128), np.float32)
    for d in range(17):
        j = 128 * d + q - k
        m = ((j >= 0) & (j <= 128)).astype(np.float32)
        m += ((j >= 0) & (j <= 512) & (j % 4 == 0)).astype(np.float32)
        m += ((j >= 0) & (j <= 2048) & (j % 16 == 0)).astype(np.float32)
        mb[:, d, :] = m
    with np.errstate(divide='ignore'):
        lb = np.where(mb > 0, np.log(np.maximum(mb, 1.0)) / SCALE, -240.0).astype(np.float32)
    hi = lb.astype(bf)
    c['lmbh'] = hi
    c['lmbl'] = (lb[:, 0:5, :] - hi[:, 0:5, :].astype(np.float32)).astype(bf)
    inv_freq = (1.0 / (500000.0 ** (np.arange(0, 16, 2, dtype=np.float32) / 16))).astype(np.float32)

    def tabs(pos):
        ang = pos.astype(np.float32)[:, None] * inv_freq[None, :]
        cs, sn = np.cos(ang).astype(np.float32), np.sin(ang).astype(np.float32)
        return np.concatenate([cs, cs], 1), np.concatenate([-sn, sn], 1)
    cc, ss = tabs(np.arange(S_LEN))
    c['rcc'] = np.ascontiguousarray(cc.reshape(32, 128, 16).transpose(1, 0, 2))
    c['rss'] = np.ascontiguousarray(ss.reshape(32, 128, 16).transpose(1, 0, 2))
    ccc, ssc = tabs(np.arange(256) * 16 + 31)
    c['rccc'] = np.ascontiguousarray(ccc.reshape(2, 128, 16).transpose(1, 0, 2))
    c['rssc'] = np.ascontiguousarray(ssc.reshape(2, 128, 16).transpose(1, 0, 2))
    cv = np.zeros((128, 17, 128), np.float32)
    for u in range(17):
        cv[:, u, :] = (16 * k + 31 - q <= 128 * u)
    c['cv'] = cv.astype(bf)
    n_cmp, n_sel = 255, 64
    c_start = np.arange(n_cmp) * 16
    s_start = np.arange(n_sel) * 64
    ov = np.clip(np.minimum(c_start[:, None] + 32, s_start[None, :] + 64)
                 - np.maximum(c_start[:, None], s_start[None, :]), 0, None) / 32
    ovp = np.zeros((256, 64), np.float32)
    ovp[:255] = ov
    c['ovl'] = np.ascontiguousarray(ovp.reshape(2, 128, 64).transpose(1, 0, 2)).astype(bf)
    qq = np.arange(128)[:, None]
    cidx = np.arange(128)[None, :]
    hi = qq // 64
    V = (cidx - 64 <= hi)
    Fm = (cidx - 64 >= hi - 1)
    c['t1'] = (V & ~Fm).astype(np.float32)
    c['t2'] = (BIG * (V & Fm) + NEGB * (~V)).astype(np.float32)
    c['vt'] = V.astype(np.float32)
    return c


GROUPS = None


def _w_groups(w_in_l):
    o = {}
    names = ['qa', 'ka', 'va', 'za', 'qb', 'kb', 'vb', 'zb', 'qc', 'kcr', 'vcr', 'ksr', 'vsr', 'kwr', 'vwr',
             'gc', 'zc', 'qd', 'kd', 'vd', 'zd']
    sizes = [256] * 8 + [256, 64, 64, 64, 64, 64, 64, 12, 256] + [256] * 4
    off = 0
    for n, s in zip(names, sizes):
        o[n] = w_in_l[:, off:off + s]
        off += s
    assert off == 3980
    pad = np.zeros((1024, 512 - 396), np.float32)
    g = [np.concatenate([o['ka'], o['va']], 1), np.concatenate([o['qa'], o['za']], 1),
         np.concatenate([o['kb'], o['vb']], 1), np.concatenate([o['qb'], o['zb']], 1),
         np.concatenate([o['kcr'], o['vcr'], o['ksr'], o['kwr'], o['vsr'], o['vwr'], o['gc'], pad], 1),
         np.concatenate([o['qc'], o['zc']], 1),
         np.concatenate([o['kd'], o['vd']], 1), np.concatenate([o['qd'], o['zd']], 1)]
    return np.stack(g, 0)


def build(n_layers=2, mixers='ABCD', dbg=False, nq_groups=8):
    nc = bass.Bass("TRN2", target_bir_lowering=False)
    C = _consts()

    def din(name, shape, dt=F32):
        return nc.dram_tensor(name, list(shape), dt, kind="ExternalInput").ap()
    x_d = din("x", [S_LEN, D])
    cT_d = din("cT", [128, 8])
    npre_d = din("npre", [L, 128, D])
    npost_d = din("npost", [L, 128, D])
    bmod_d = din("bmod", [L, 128, 3 * D])
    wmod_d = din("wmod", [L, D, 3 * D])
    wg_d = din("wg", [L, 8, D, 512])
    wout_d = din("wout", [L, D, D])
    w1_d = din("w1", [L, 2, 2048, 256])
    w2_d = din("w2", [L, 2, 256, 64])
    b1_d = din("b1", [L, 128, 4])
    posT_d = din("posT", [L, 64, 2, 32])
    cd = {}
    for k_, v_ in C.items():
        cd[k_] = din("c_" + k_, v_.shape, BF16 if v_.dtype == ml_dtypes.bfloat16 else F32)
    out_d = nc.dram_tensor("out", [S_LEN, D], F32, kind="ExternalOutput").ap()
    x1_d = nc.dram_tensor("x1s", [S_LEN, D], F32, kind="Internal").ap()
    mixed_d = nc.dram_tensor("mixed", [S_LEN, D], BF16, kind="ExternalOutput" if dbg else "Internal").ap()

    with ExitStack() as es:
        S = Sched(nc, es)

        tcount = [0]

        def T(name, shape, dt, st=es):
            tcount[0] += 1
            return st.enter_context(nc.sbuf_tensor(f"{name}_u{tcount[0]}", list(shape), dt))

        def P(name, shape, dt):
            return es.enter_context(nc.psum_tensor(name, list(shape), dt))

        hT = T("hT", [128, 8, S_LEN], BF16)
        Gt = T("Gt", [128, D], F32)
        sht = T("sht", [128, D], F32)
        gpt = T("gpt", [128, D], F32)
        ct = {}
        for k_, v_ in C.items():
            ct[k_] = T("k_" + k_, v_.shape, BF16 if v_.dtype == ml_dtypes.bfloat16 else F32)
            S.dma('sp', ct[k_][:], cd[k_], 'const', writes=['c_' + k_])
        ident = ct['ident']
        wbf = [T(f"wbf{i}", [128, 8, 512], BF16) for i in range(3)]
        wloaded = {}
        wf = [T(f"wf{i}", [128, 512], F32) for i in range(3)]
        wb = [T(f"wb{i}", [128, 512], BF16) for i in range(6)]
        wf_rot = Rot(list(range(3)))
        wb_rot = Rot(list(range(6)))
        qbp = [T(f"qbp{i}", [128, 256], BF16) for i in range(2)]
        qb_rot = Rot([0, 1])
        sm2 = [T(f"sz{i}", [128, 256], F32) for i in range(1)]
        sm2_rot = Rot([0])

        def SM2():
            i = sm2_rot()
            return sm2[i], f"sz{i}"
        sm = [T(f"sm{i}", [128, 64], F32) for i in range(8)]
        sm_rot = Rot(list(range(8)))
        pT = [P(f"pT{i}", [128, 8, 128], BF16) for i in range(2)]
        pR = [P(f"pR{i}", [128, 512], F32) for i in range(4)]
        pA = [P(f"pA{i}", [128, 512], F32) for i in range(2)]
        pT_rot = Rot([0, 1])
        pR_rot = Rot([0, 1, 2, 3])

        def WF():
            i = wf_rot()
            return wf[i], f"wf{i}"

        def WB():
            i = wb_rot()
            return wb[i], f"wb{i}"

        def SM():
            i = sm_rot()
            return sm[i], f"sm{i}"

        def PR():
            i = pR_rot()
            return pR[i], f"pR{i}"

        def PT():
            i = pT_rot()
            return pT[i], f"pT{i}"

        def wload(idx):
            if idx in wloaded or idx >= 8 * n_layers:
                return
            bi = idx % 3
            wt, wk = wbf[bi], f"wbf{bi}"
            src = wg_d[idx // 8, idx % 8].rearrange("(k p) c -> p k c", p=128)
            for half in range(2):
                S.dma('pool', wt[:, half * 4:(half + 1) * 4, :], src[:, half * 4:(half + 1) * 4, :], f"wl{bi}{half}", writes=[(wk, half)])
            wloaded[idx] = (wt, wk)

        def load_wgroup(l, g):
            idx = 8 * l + g
            wload(idx)
            return wloaded[idx]

        def proj_slot(slot, wt, wk, ncols=512):
            ps, pk = PR()

            def f(e):
                for k in range(8):
                    ins = e.matmul(ps[:, 0:ncols], hT[:, k, slot * 128:(slot + 1) * 128], wt[:, k, 0:ncols],
                                   start=(k == 0), stop=(k == 7))
                return ins
            S.op('pe', f, reads=[('hT', slot), (wk, 0), (wk, 1)], writes=[pk])
            return ps, pk

        def rope(src, srckey, dst, dstkey, nh, cc_ap, ss_ap, tabkeys):
            S.op('act', lambda e: e.copy(dst[:, :, 16:64], src[:, :, 16:64]), reads=[srckey], writes=[dstkey])
            ta, tak = SM()
            tb, tbk = SM()
            tav = ta[:, 0:nh * 16].rearrange("p (h c) -> p h c", c=16)
            tbv = tb[:, 0:nh * 16].rearrange("p (h c) -> p h c", c=16)
            S.op('dve', lambda e: e.tensor_tensor(tav, src[:, :, 0:16], cc_ap.unsqueeze(1).to_broadcast([128, nh, 16]), ALU.mult),
                 reads=[srckey] + tabkeys, writes=[tak])
            S.op('dve', lambda e: e.tensor_tensor(tbv[:, :, 0:8], src[:, :, 8:16], ss_ap[:, 0:8].unsqueeze(1).to_broadcast([128, nh, 8]), ALU.mult),
                 reads=[srckey] + tabkeys, writes=[tbk])
            S.op('dve', lambda e: e.tensor_tensor(tbv[:, :, 8:16], src[:, :, 0:8], ss_ap[:, 8:16].unsqueeze(1).to_broadcast([128, nh, 8]), ALU.mult),
                 reads=[srckey] + tabkeys, writes=[tbk])
            S.op('dve', lambda e: e.tensor_tensor(dst[:, :, 0:16], tav, tbv, ALU.add),
                 reads=[tak, tbk], writes=[dstkey])

        def run_pipeline(its, nst):
            n = len(its)
            for step in range(n + nst - 1):
                for k in range(nst):
                    t = step - k
                    if 0 <= t < n and its[t][k] is not None:
                        its[t][k]()

        for l in range(n_layers):
            xin = x_d if l == 0 else x1_d
            xin_key = 'xin' if l == 0 else 'x1'
            xout = out_d if l == n_layers - 1 else x1_d
            xout_key = 'out' if l == n_layers - 1 else 'x1'

            with ExitStack() as ps0:
                cTt = T("cTt", [128, 8], F32, ps0)
                sc = T("sc", [128, 8], F32, ps0)
                crep = T("crep", [128, 8, 128], BF16, ps0)
                wm = [T(f"wm{i}", [128, 8, 512], BF16, ps0) for i in range(2)]
                modb = T("modb", [128, 3 * D], F32, ps0)
                bmt = T("bmt", [128, 3 * D], F32, ps0)
                npre_t = T("npre_t", [128, D], F32, ps0)
                npost_t = T("npost_t", [128, D], F32, ps0)
                S.dma('sp', cTt[:], cT_d, 'p0a', writes=['cTt'])
                S.dma('sp', bmt[:], bmod_d[l], 'p0b', writes=['bmt'])
                S.dma('sp', npre_t[:], npre_d[l], 'p0c', writes=['npre_t'])
                S.dma('sp', npost_t[:], npost_d[l], 'p0d', writes=['npost_t'])
                S.op('act', lambda e: e.activation(sc[:], cTt[:], AF.Silu), reads=['cTt'], writes=['sc'])
                S.op('dve', lambda e: e.tensor_copy(crep[:], sc[:].unsqueeze(2).to_broadcast([128, 8, 128])), reads=['sc'], writes=['crep'])
                for cg in range(6):
                    wmt, wmk = wm[cg % 2], f"wm{cg % 2}"
                    S.dma('pool', wmt[:], wmod_d[l][:, cg * 512:(cg + 1) * 512].rearrange("(k p) c -> p k c", p=128), wmk, writes=[wmk])
                    ps, pk = PR()

                    def f(e, ps=ps, wmt=wmt):
                        for k in range(8):
                            ins = e.matmul(ps[:], crep[:, k, :], wmt[:, k, :], start=(k == 0), stop=(k == 7))
                        return ins
                    S.op('pe', f, reads=['crep', wmk], writes=[pk])
                    S.op('dve', lambda e, ps=ps, cg=cg: e.tensor_tensor(modb[:, cg * 512:(cg + 1) * 512], ps[:], bmt[:, cg * 512:(cg + 1) * 512], ALU.add),
                         reads=[pk, 'bmt'], writes=[('modb', cg)])
                S.op('act', lambda e: e.copy(sht[:], modb[:, 0:D]), reads=[('modb', 0), ('modb', 1)], writes=['sht'])
                S.op('dve', lambda e: e.scalar_tensor_tensor(out=Gt[:], in0=modb[:, D:2 * D], scalar=1.0, in1=npre_t[:], op0=ALU.add, op1=ALU.mult),
                     reads=[('modb', 2), ('modb', 3), 'npre_t'], writes=['Gt'])
                S.op('dve', lambda e: e.tensor_tensor(gpt[:], modb[:, 2 * D:3 * D], npost_t[:], ALU.mult),
                     reads=[('modb', 4), ('modb', 5), 'npost_t'], writes=['gpt'])
                S.barrier(drop_prefixes=('cTt', 'sc', 'crep', 'wm', 'modb', 'bmt', 'npre_t', 'npost_t'))

            with ExitStack() as ps1:
                xt = [T(f"xt{i}", [128, D], F32, ps1) for i in range(2)]
                junk = T("junk", [128, D], BF16, ps1)
                ht32 = T("ht32", [128, D], F32, ps1)
                hb = [T(f"hb{i}", [128, D], BF16, ps1) for i in range(2)]
                st1 = [T(f"st1_{i}", [128, 4], F32, ps1) for i in range(2)]
                def p1_iter(slot):
                    i2 = slot % 2
                    stt, stk = st1[i2], f"st1_{i2}"
                    c = {}

                    def s0():
                        S.dma('sp', xt[i2][:], xin[slot * 128:(slot + 1) * 128, :], f"xt{i2}", reads=[(xin_key, slot)], writes=[f"xt{i2}"])

                    def s1():
                        S.op('act', lambda e: e.activation(junk[:], xt[i2][:], AF.Square, accum_out=stt[:, 0:1]),
                             reads=[f"xt{i2}"], writes=['junk', (stk, 0)])
                        S.op('dve', lambda e: e.tensor_scalar(out=stt[:, 3:4], in0=stt[:, 0:1], scalar1=1.0 / D, scalar2=EPS, op0=ALU.mult, op1=ALU.add),
                             reads=[(stk, 0)], writes=[(stk, 3)])
                        S.op('act', lambda e: e.activation(stt[:, 1:2], stt[:, 3:4], AF.Sqrt), reads=[(stk, 3)], writes=[(stk, 1)])
                        S.op('dve', lambda e: e.reciprocal(stt[:, 2:3], stt[:, 1:2]), reads=[(stk, 1)], writes=[(stk, 2)])
                        S.op('dve', lambda e: e.scalar_tensor_tensor(out=ht32[:], in0=xt[i2][:], scalar=stt[:, 2:3], in1=Gt[:], op0=ALU.mult, op1=ALU.mult),
                             reads=[f"xt{i2}", (stk, 2), 'Gt'], writes=['ht32'])
                        S.op('pool', lambda e: e.tensor_tensor(hb[i2][:], ht32[:], sht[:], ALU.add),
                             reads=['ht32', 'sht'], writes=[f"hb{i2}"])

                    def s2():
                        pt, ptk = PT()
                        c['pt'], c['ptk'] = pt, ptk

                        def f(e):
                            for k in range(8):
                                ins = e.transpose(pt[:, k, :], hb[i2][:, k * 128:(k + 1) * 128], ident[:])
                            return ins
                        S.op('pe', f, reads=[f"hb{i2}", 'c_ident'], writes=[ptk])

                    def s3():
                        pt, ptk = c['pt'], c['ptk']
                        if slot % 2:
                            S.op('act', lambda e: e.copy(hT[:, :, slot * 128:(slot + 1) * 128], pt[:]), reads=[ptk], writes=[('hT', slot)])
                        else:
                            S.op('dve', lambda e: e.tensor_copy(hT[:, :, slot * 128:(slot + 1) * 128], pt[:]), reads=[ptk], writes=[('hT', slot)])
                    return [s0, s1, s2, s3]
                run_pipeline([p1_iter(slot) for slot in range(NSLOT)], 4)
                S.barrier(drop_prefixes=('xt', 'junk', 'ht32', 'hb', 'st1_'))

            def qk_transpose(src_bf, srckeys, dst_fn, dstkey, npair):
                pt, ptk = PT()

                def f(e):
                    for p in range(npair):
                        ins = e.transpose(pt[:, p, :], src_bf[:, p * 128:(p + 1) * 128], ident[:])
                    return ins
                S.op('pe', f, reads=list(srckeys) + ['c_ident'], writes=[ptk])
                return pt, ptk

            mixrr = [0]

            def store_mixed(m, col0, o_ap_fn, okeys, zt, ztk, i):
                mo, mok = WB()
                S.op('dve', lambda e: e.tensor_tensor(mo[:, 0:256], o_ap_fn(), zt[:, i, :], ALU.mult),
                     reads=list(okeys) + [(ztk, i)], writes=[mok])
                mixrr[0] += 1
                S.dma('pool', mixed_d[m * 128:(m + 1) * 128, col0:col0 + 256], mo[:, 0:256], f'mixst{mixrr[0] % 4}',
                      reads=[mok], writes=[('mixed', m, col0)])

            def qs_iters(wq, wqk, qg, qT2, qT2k, zt, ztk, do_rope, pairs=True, qcT=None):
                def one(i):
                    slot = qg * 4 + i
                    c = {}

                    def s0():
                        c['ps'], c['pk'] = proj_slot(slot, wq, wqk)

                    def s1():
                        ps, pk = c['ps'], c['pk']
                        ez, ezk = SM2()
                        S.op('act', lambda e: e.activation(ez[:], ps[:, 256:512], AF.Exp, scale=-1.0), reads=[pk], writes=[ezk])
                        S.op('dve', lambda e: e.tensor_scalar(out=ez[:], in0=ez[:], scalar1=1.0, scalar2=None, op0=ALU.add), reads=[ezk], writes=[ezk])
                        S.op('dve', lambda e: e.reciprocal(ez[:], ez[:]), reads=[ezk], writes=[ezk])
                        S.op('dve', lambda e: e.tensor_tensor(zt[:, i, :], ps[:, 256:512], ez[:], ALU.mult), reads=[pk, ezk], writes=[(ztk, i)])
                        qi = qb_rot()
                        qb_, qbk = qbp[qi], f"qbp{qi}"
                        c['qb'], c['qbk'] = qb_, qbk
                        if do_rope:
                            rope(ps[:, 0:256].rearrange("p (h c) -> p h c", c=64), pk,
                                 qb_[:, 0:256].rearrange("p (h c) -> p h c", c=64), qbk, 4,
                                 ct['rcc'][:, slot, :], ct['rss'][:, slot, :], ['c_rcc', 'c_rss'])
                        else:
                            S.op('dve', lambda e: e.tensor_copy(qb_[:, 0:256], ps[:, 0:256]), reads=[pk], writes=[qbk])

                    def s2():
                        qb_, qbk = c['qb'], c['qbk']
                        if pairs:
                            c['pt'], c['ptk'] = qk_transpose(qb_, [qbk], None, None, 2)
                        else:
                            pt, ptk = PT()
                            c['pt'], c['ptk'] = pt, ptk

                            def f(e):
                                for h in range(4):
                                    ins = e.transpose(pt[0:64, h, :], qb_[:, h * 64:(h + 1) * 64], ident[:])
                                return ins
                            S.op('pe', f, reads=[qbk, 'c_ident'], writes=[ptk])

                    def s3():
                        pt, ptk = c['pt'], c['ptk']
                        if pairs:
                            S.op('dve', lambda e: e.tensor_copy(qT2[0:64, :, i, 0, :], pt[0:64, 0:2, :]), reads=[ptk], writes=[(qT2k, i, 0)])
                            S.op('act', lambda e: e.copy(qT2[64:128, :, i, 1, :], pt[64:128, 0:2, :]), reads=[ptk], writes=[(qT2k, i, 1)])
                        else:
                            S.op('dve', lambda e: e.tensor_copy(qcT[:, i, :, :], pt[0:64, 0:4, :]), reads=[ptk], writes=[('qcT', i)])
                    return [s0, s1, s2, s3]
                return [one(i) for i in range(4)]

            def interleave(its, extra):
                its = [it + [None] * (4 - len(it)) for it in its]
                if not extra:
                    return its
                n = len(its)
                out = []
                pos = [((j + 1) * n) // (len(extra) + 1) for j in range(len(extra))]
                j = 0
                for t, it in enumerate(its):
                    while j < len(extra) and pos[j] == t:
                        out.append(extra[j])
                        j += 1
                    out.append(it)
                out.extend(extra[j:])
                return out

            def s_pairs(kT, kTk, kb, qT2, qT2k, i):
                ps, pk = PR()

                def f(e):
                    for p in range(2):
                        ins = e.matmul(ps[:, p * 256:(p + 1) * 256], kT[:, p, kb * 128:(kb + 1) * 128],
                                       qT2[:, p, i, :, :].rearrange("p a b -> p (a b)"), start=True, stop=True)
                    return ins
                S.op('pe', f, reads=[(kTk, kb), (qT2k, i, 0), (qT2k, i, 1)], writes=[pk])
                return ps, pk

            def av(acc, acck, pb, pbk, v_fn, vkey, first, last, width):
                def f(e):
                    for h in range(4):
                        ins = e.matmul(acc[:, h * width:(h + 1) * width], pb[:, h * 128:(h + 1) * 128], v_fn(h), start=(first and h == 0), stop=last,
                                       skip_group_check=True)
                    return ins
                S.op('pe', f, reads=[pbk] + (vkey if isinstance(vkey, list) else [vkey]), writes=[acck])

            def normalize(acc, acck, width=65):
                accv = acc[:, 0:4 * width].rearrange("p (h c) -> p h c", c=width)
                rd, rdk = SM()
                S.op('dve', lambda e: e.reciprocal(rd[:, 0:4], accv[:, :, 64]), reads=[acck], writes=[rdk])
                o, ok = WF()
                S.op('dve', lambda e: e.tensor_tensor(o[:, 0:256].rearrange("p (h c) -> p h c", c=64), accv[:, :, 0:64],
                                                      rd[:, 0:4].unsqueeze(2).to_broadcast([128, 4, 64]), ALU.mult),
                     reads=[acck, rdk], writes=[ok])
                return o, ok

            if 'A' in mixers:
                with ExitStack() as ma:
                    kaT = T("kaT", [128, 2, S_LEN], BF16, ma)
                    va = T("va", [128, NSLOT, 256], BF16, ma)
                    qT2s = [T(f"qT2a{b_}", [128, 2, 4, 2, 128], BF16, ma) for b_ in range(2)]
                    zts = [T(f"zta{b_}", [128, 4, 256], BF16, ma) for b_ in range(2)]
                    for b_ in range(2):
                        S.op('pool', lambda e, b_=b_: e.memset(qT2s[b_][:].rearrange("p a b c d -> p (a b c d)"), 0.0), writes=[(f'qT2_{b_}', i, j) for i in range(4) for j in range(2)])
                    wk_, wkk = load_wgroup(l, 0)
                    wq_, wqk = load_wgroup(l, 1)
                    wload(8 * l + 2)
                    def a_k_iter(slot):
                        c = {}

                        def s0():
                            c['ps'], c['pk'] = proj_slot(slot, wk_, wkk)

                        def s1():
                            ps, pk = c['ps'], c['pk']
                            S.op('act', lambda e: e.copy(va[:, slot, :], ps[:, 256:512]), reads=[pk], writes=[('va', slot)])
                            c['kb'], c['kbk'] = WB()
                            S.op('dve', lambda e: e.tensor_copy(c['kb'][:, 0:256], ps[:, 0:256]), reads=[pk], writes=[c['kbk']])

                        def s2():
                            c['pt'], c['ptk'] = qk_transpose(c['kb'], [c['kbk']], None, None, 2)

                        def s3():
                            S.op('dve', lambda e: e.tensor_copy(kaT[:, :, slot * 128:(slot + 1) * 128], c['pt'][:, 0:2, :]),
                                 reads=[c['ptk']], writes=[('kaT', slot)])
                        return [s0, s1, s2, s3]
                    run_pipeline([a_k_iter(slot) for slot in range(NSLOT)], 4)
                    wload(8 * l + 3)
                    wa = [T(f"wa{j}", [128, 512], BF16, ma) for j in range(16)]
                    wa_rot = Rot(list(range(16)))

                    def WA():
                        j = wa_rot()
                        return wa[j], f"wa{j}"

                    def a_iter(m, i, kb, st, qT2, qT2k, zt, ztk):
                        c = {}
                        acc, acck = pA[m % 2], f"pA{m % 2}"
                        diag = (kb == m)

                        def s0():
                            c['ps'], c['pk'] = s_pairs(kaT, 'kaT', kb, qT2, qT2k, i)

                        def s1():
                            ps, pk = c['ps'], c['pk']
                            et, etk = WA()
                            c['et'], c['etk'] = et, etk
                            S.op('act', lambda e: e.activation(et[:], ps[:], AF.Exp, scale=SCALE), reads=[pk], writes=[etk])
                            spb, spbk = WA()
                            S.op('act', lambda e: e.activation(spb[:], et[:], AF.Ln, bias=1.0), reads=[etk], writes=[spbk])
                            if diag:
                                S.op('dve', lambda e: e.tensor_tensor(
                                    spb[:].rearrange("p (h c) -> p h c", c=128), spb[:].rearrange("p (h c) -> p h c", c=128),
                                    ct['triS'][:].unsqueeze(1).to_broadcast([128, 4, 128]), ALU.mult),
                                    reads=[spbk, 'c_triS'], writes=[spbk])
                            bw, bwk = PR()
                            c['bw'], c['bwk'] = bw, bwk
                            lsum, lsumk = st['lsum'], st['lsumk']

                            def fb(e):
                                ins = e.matmul(bw[:], ct['umat'][:], spb[:], start=True, stop=diag)
                                if not diag:
                                    ins = e.matmul(bw[:], ct['ones'][:], lsum[:], start=False, stop=True)
                                return ins
                            S.op('pe', fb, reads=[spbk, 'c_umat', 'c_ones'] + ([lsumk] if not diag else []), writes=[bwk])
                            if kb > 0:
                                if diag:
                                    st['lsum'], st['lsumk'] = spb, spbk
                                else:
                                    nl_, nlk = WA()
                                    S.op('pool', lambda e: e.tensor_tensor(nl_[:], lsum[:], spb[:], ALU.add), reads=[lsumk, spbk], writes=[nlk])
                                    st['lsum'], st['lsumk'] = nl_, nlk

                        def s2():
                            et, etk, bw, bwk = c['et'], c['etk'], c['bw'], c['bwk']
                            xt_, xtk = WA()
                            S.op('act', lambda e: e.activation(xt_[:], bw[:], AF.Exp, scale=-1.0), reads=[bwk], writes=[xtk])
                            ab_, abk = WA()
                            S.op('dve', lambda e: e.tensor_tensor(ab_[:], et[:], xt_[:], ALU.mult), reads=[etk, xtk], writes=[abk])
                            if diag:
                                S.op('dve', lambda e: e.tensor_tensor(
                                    ab_[:].rearrange("p (h c) -> p h c", c=128), ab_[:].rearrange("p (h c) -> p h c", c=128),
                                    ct['triS'][:].unsqueeze(1).to_broadcast([128, 4, 128]), ALU.mult),
                                    reads=[abk, 'c_triS'], writes=[abk])
                            av(acc, acck, ab_, abk, lambda h: va[:, kb, h * 64:(h + 1) * 64], ('va', kb), diag, kb == 0, 64)
                            if kb == 0:
                                store_mixed(m, 0, lambda: acc[:, 0:256], [acck], zt, ztk, i)
                        return [s0, s1, s2]
                    run_pipeline(qs_iters(wq_, wqk, 0, qT2s[0], 'qT2_0', zts[0], 'zta_0', do_rope=False), 4)
                    for qg in range(nq_groups):
                        b_ = qg % 2
                        st = {'lsum': None, 'lsumk': None}
                        its = []
                        for i in range(4):
                            m = qg * 4 + i
                            for kb in range(m, -1, -1):
                                its.append(a_iter(m, i, kb, st, qT2s[b_], f'qT2_{b_}', zts[b_], f'zta_{b_}'))
                        extra = qs_iters(wq_, wqk, qg + 1, qT2s[1 - b_], f'qT2_{1 - b_}', zts[1 - b_], f'zta_{1 - b_}', do_rope=False) if qg + 1 < nq_groups else []
                        run_pipeline(interleave(its, extra), 4)
                    S.barrier(drop_prefixes=('kaT', 'va', 'qT2', 'zta', 'wa'))

            if 'B' in mixers:
                with ExitStack() as mbs:
                    kT = T("kbT", [128, 2, S_LEN], BF16, mbs)
                    vaug = T("vbaug", [128, NSLOT, 4, 65], BF16, mbs)
                    qT2s = [T(f"qT2b{b_}", [128, 2, 4, 2, 128], BF16, mbs) for b_ in range(2)]
                    zts = [T(f"ztb{b_}", [128, 4, 256], BF16, mbs) for b_ in range(2)]
                    for b_ in range(2):
                        S.op('pool', lambda e, b_=b_: e.memset(qT2s[b_][:].rearrange("p a b c d -> p (a b c d)"), 0.0), writes=[(f'qT2_{b_}', i, j) for i in range(4) for j in range(2)])
                    S.op('pool', lambda e: e.memset(vaug[:].rearrange("p a b c -> p (a b c)"), 1.0), writes=[('vaug', s_) for s_ in range(NSLOT)])
                    wk_, wkk = load_wgroup(l, 2)
                    wq_, wqk = load_wgroup(l, 3)
                    wload(8 * l + 4)
                    def rk_iter(slot, kT_, kTkey, vaug_, wk__, wkk__):
                        c = {}

                        def s0():
                            c['ps'], c['pk'] = proj_slot(slot, wk__, wkk__)

                        def s1():
                            ps, pk = c['ps'], c['pk']
                            S.op('act', lambda e: e.copy(vaug_[:, slot, :, 0:64], ps[:, 256:512].rearrange("p (h c) -> p h c", c=64)),
                                 reads=[pk], writes=[('vaug', slot)])
                            c['kb'], c['kbk'] = WB()
                            rope(ps[:, 0:256].rearrange("p (h c) -> p h c", c=64), pk,
                                 c['kb'][:, 0:256].rearrange("p (h c) -> p h c", c=64), c['kbk'], 4,
                                 ct['rcc'][:, slot, :], ct['rss'][:, slot, :], ['c_rcc', 'c_rss'])

                        def s2():
                            c['pt'], c['ptk'] = qk_transpose(c['kb'], [c['kbk']], None, None, 2)

                        def s3():
                            S.op('dve', lambda e: e.tensor_copy(kT_[:, :, slot * 128:(slot + 1) * 128], c['pt'][:, 0:2, :]),
                                 reads=[c['ptk']], writes=[(kTkey, slot)])
                        return [s0, s1, s2, s3]
                    run_pipeline([rk_iter(slot, kT, 'kT', vaug, wk_, wkk) for slot in range(NSLOT)], 4)
                    wload(8 * l + 5)
                    def b_iter(m, i, d, dmax, qT2, qT2k, zt, ztk):
                        c = {}
                        acc, acck = pA[m % 2], f"pA{m % 2}"
                        kb = m - d

                        def s0():
                            ps, pk = PR()
                            c['ps'], c['pk'] = ps, pk

                            def f(e):
                                for p in range(2):
                                    e.matmul(ps[:, p * 256:(p + 1) * 256], kT[:, p, kb * 128:(kb + 1) * 128],
                                             qT2[:, p, i, :, :].rearrange("p a b -> p (a b)"), start=(p == 0), stop=False, skip_group_check=True)
                                for h in range(4):
                                    ins = e.matmul(ps[:, h * 128:(h + 1) * 128], ident[:], ct['lmbh'][:, d, :], start=False, stop=(d > 4 and h == 3), skip_group_check=True)
                                if d <= 4:
                                    for h in range(4):
                                        ins = e.matmul(ps[:, h * 128:(h + 1) * 128], ident[:], ct['lmbl'][:, d, :], start=False, stop=(h == 3), skip_group_check=True)
                                return ins
                            S.op('pe', f, reads=[('kT', kb), (qT2k, i, 0), (qT2k, i, 1), 'c_ident', 'c_lmbh', 'c_lmbl'], writes=[pk])

                        def s1():
                            c['pb'], c['pbk'] = WB()
                            S.op('act', lambda e: e.activation(c['pb'][:], c['ps'][:], AF.Exp, scale=SCALE), reads=[c['pk']], writes=[c['pbk']])

                        def s2():
                            av(acc, acck, c['pb'], c['pbk'], lambda h: vaug[:, kb, h, :], ('vaug', kb), d == 0, d == dmax, 65)
                            if d == dmax:
                                o, ok = normalize(acc, acck)
                                store_mixed(m, 256, lambda: o[:, 0:256], [ok], zt, ztk, i)
                        return [s0, s1, s2]
                    run_pipeline(qs_iters(wq_, wqk, 0, qT2s[0], 'qT2_0', zts[0], 'ztb_0', do_rope=True), 4)
                    for qg in range(nq_groups):
                        b_ = qg % 2
                        its = []
                        for i in range(4):
                            m = qg * 4 + i
                            dmax = min(16, m)
                            for d in range(0, dmax + 1):
                                its.append(b_iter(m, i, d, dmax, qT2s[b_], f'qT2_{b_}', zts[b_], f'ztb_{b_}'))
                        extra = qs_iters(wq_, wqk, qg + 1, qT2s[1 - b_], f'qT2_{1 - b_}', zts[1 - b_], f'ztb_{1 - b_}', do_rope=True) if qg + 1 < nq_groups else []
                        run_pipeline(interleave(its, extra), 4)
                    S.barrier(drop_prefixes=('kT', 'vaug', 'qT2', 'ztb'))

            if 'C' in mixers:
                with ExitStack() as mcs:
                    kcrT = T("kcrT", [64, S_LEN], BF16, mcs)
                    vcrT = T("vcrT", [64, S_LEN], BF16, mcs)
                    kvT = [kcrT, vcrT]
                    ksT = T("ksT", [128, S_LEN], BF16, mcs)
                    kwT = T("kwT", [128, S_LEN], BF16, mcs)
                    vsa = T("vsa", [128, NSLOT, 65], BF16, mcs)
                    vwa = T("vwa", [128, NSLOT, 65], BF16, mcs)
                    gsig = T("gsig", [128, NSLOT, 12], F32, mcs)
                    kcT = T("kcT", [128, 256], BF16, mcs)
                    vcE = T("vcE", [128, 2, 128], BF16, mcs)
                    qT2c = T("qT2c", [128, 2, 4, 2, 128], BF16, mcs)
                    S.op('pool', lambda e: e.memset(qT2c[:].rearrange("p a b c d -> p (a b c d)"), 0.0), writes=[('qT2c', i, j) for i in range(4) for j in range(2)])
                    zt = T("ztc", [128, 4, 256], BF16, mcs)
                    w1bs = [T(f"w1b{j}", [64, 2, 8, 256], BF16, mcs) for j in range(2)]
                    w2b = T("w2b", [128, 2, 2, 64], BF16, mcs)
                    b1t = T("b1t", [128, 4], F32, mcs)
                    posb = T("posb", [64, 2, 32], BF16, mcs)
                    biasv = T("biasv", [128, 4], F32, mcs)
                    hidT = T("hidT", [128, 4, 256], BF16, mcs)
                    ocacc = T("ocacc", [128, 256], F32, mcs)
                    impt = T("impt", [128, 64], F32, mcs)
                    imps = T("imps", [128, 64], F32, mcs)
                    selm = T("selm", [128, 64], BF16, mcs)
                    mx8 = T("mx8", [128, 16], F32, mcs)
                    cf = T("cf", [128, 16], F32, mcs)
                    S.op('pool', lambda e: e.memset(vsa[:].rearrange("p a b -> p (a b)"), 1.0), writes=[('vsa', s_) for s_ in range(NSLOT)])
                    S.op('pool', lambda e: e.memset(vwa[:].rearrange("p a b -> p (a b)"), 1.0), writes=[('vwa', s_) for s_ in range(NSLOT)])
                    wk_, wkk = load_wgroup(l, 4)
                    wq_, wqk = load_wgroup(l, 5)
                    wload(8 * l + 6)
                    for t in range(2):
                        S.dma('pool', w2b[:, t, :, :], w2_d[l, t].rearrange("(c p) d -> p c d", p=128), f'cw{t}', writes=['w2b'])
                    S.dma('sp', b1t[:], b1_d[l], 'cw2', writes=['b1t'])
                    S.dma('pool', posb[:], posT_d[l], 'cw3', writes=['posb'])
                    S.dma('sp', vcE[:, :, 64:128], cd['ovl'], 'cw4', writes=[('vcE', 'ov')])
                    def ck_iter(slot):
                        c = {}
                        tsl = slice(slot * 128, (slot + 1) * 128)

                        def s0():
                            c['ps'], c['pk'] = proj_slot(slot, wk_, wkk, ncols=396)

                        def s1():
                            ps, pk = c['ps'], c['pk']
                            S.op('act', lambda e: e.copy(vsa[:, slot, 0:64], ps[:, 256:320]), reads=[pk], writes=[('vsa', slot)])
                            S.op('act', lambda e: e.copy(vwa[:, slot, 0:64], ps[:, 320:384]), reads=[pk], writes=[('vwa', slot)])
                            S.op('act', lambda e: e.activation(gsig[:, slot, :], ps[:, 384:396], AF.Sigmoid), reads=[pk], writes=[('gsig', slot)])
                            kb_, kbk = WB()
                            c['kb'], c['kbk'] = kb_, kbk
                            S.op('dve', lambda e: e.tensor_copy(kb_[:, 0:128], ps[:, 0:128]), reads=[pk], writes=[kbk])
                            kdup = kb_[:, 128:384].rearrange("p (h c) -> p h c", c=128)
                            rope(ps[:, 128:256].rearrange("p (h c) -> p h c", c=64), pk,
                                 kdup[:, :, 0:64], kbk, 2,
                                 ct['rcc'][:, slot, :], ct['rss'][:, slot, :], ['c_rcc', 'c_rss'])
                            S.op('dve', lambda e: e.tensor_copy(kdup[:, :, 64:128], kdup[:, :, 0:64]), reads=[kbk], writes=[kbk])

                        def s2():
                            kb_, kbk = c['kb'], c['kbk']
                            pt, ptk = PT()
                            c['pt'], c['ptk'] = pt, ptk

                            def f(e):
                                e.transpose(pt[0:64, 0, :], kb_[:, 0:64], ident[:])
                                e.transpose(pt[0:64, 3, :], kb_[:, 64:128], ident[:])
                                e.transpose(pt[:, 1, :], kb_[:, 128:256], ident[:])
                                return e.transpose(pt[:, 2, :], kb_[:, 256:384], ident[:])
                            S.op('pe', f, reads=[kbk, 'c_ident'], writes=[ptk])

                        def s3():
                            pt, ptk = c['pt'], c['ptk']
                            S.op('dve', lambda e: e.tensor_copy(kcrT[:, tsl], pt[0:64, 0, :]), reads=[ptk], writes=[('kcrT', slot)])
                            S.op('act', lambda e: e.copy(vcrT[:, tsl], pt[0:64, 3, :]), reads=[ptk], writes=[('vcrT', slot)])
                            S.op('act', lambda e: e.copy(ksT[:, tsl], pt[:, 1, :]), reads=[ptk], writes=[('ksT', slot)])
                            S.op('dve', lambda e: e.tensor_copy(kwT[:, tsl], pt[:, 2, :]), reads=[ptk], writes=[('kwT', slot)])
                        return [s0, s1, s2, s3]
                    run_pipeline([ck_iter(slot) for slot in range(NSLOT)], 4)
                    wload(8 * l + 7)
                    allkv = [[('kcrT', s_) for s_ in range(NSLOT)], [('vcrT', s_) for s_ in range(NSLOT)]]
                    bps, bpk = PR()
                    kv3 = [kvT[t][:].rearrange("p (n r) -> p n r", r=16) for t in range(2)]
                    for piece in range(4):
                        w1b = w1bs[piece % 2]
                        w1k = f"w1b{piece % 2}"
                        for t in range(2):
                            S.dma('pool', w1b[:, t, :, :], w1_d[l, t, piece * 512:(piece + 1) * 512, :].rearrange("(l d) c -> d l c", d=64),
                                  f"w1l{piece % 2}{t}", writes=[(w1k, t)])
                        for t in range(2):
                            def f(e, t=t, piece=piece, w1b=w1b):
                                ins = None
                                for li in range(8):
                                    lg = piece * 8 + li
                                    for c in range(2):
                                        o_ = pA[t][:, c * 256:(c + 1) * 256]
                                        w_ = w1b[:, t, li, c * 128:(c + 1) * 128]
                                        first = (lg == 0 and c == 0)
                                        last = (lg == 31)
                                        if lg < 16:
                                            ins = e.matmul(o_, w_, kv3[t][:, 0:256, lg], start=first, stop=False, skip_group_check=True)
                                        else:
                                            ins = e.matmul(o_[:, 0:255], w_, kv3[t][:, 1:256, lg - 16], start=False, stop=last, skip_group_check=True)
                                        ins = e.matmul(bps[:, (t * 2 + c):(t * 2 + c) + 1], w_, posb[:, t, lg:lg + 1],
                                                       start=(t == 0 and first), stop=last, skip_group_check=True)
                                return ins
                            S.op('pe', f, reads=allkv[t] + [(w1k, t), 'posb'], writes=[f"pA{t}", bpk])
                    S.op('dve', lambda e: e.tensor_tensor(biasv[:], bps[:, 0:4], b1t[:], ALU.add), reads=[bpk, 'b1t'], writes=['biasv'])
                    for t in range(2):
                        for c in range(2):
                            j = t * 2 + c
                            (xs_, xsk), (x2_, x2k), (u__, uk) = WF(), WF(), WF()
                            xs, x2, u_ = xs_[:, 0:256], x2_[:, 0:256], u__[:, 0:256]
                            S.op('dve', lambda e, t=t, c=c, j=j, xs=xs: e.tensor_scalar(out=xs, in0=pA[t][:, c * 256:(c + 1) * 256], scalar1=biasv[:, j:j + 1], scalar2=None, op0=ALU.add),
                                 reads=[f"pA{t}", 'biasv'], writes=[xsk])
                            S.op('dve', lambda e, xs=xs, x2=x2: e.tensor_tensor(x2, xs, xs, ALU.mult), reads=[xsk], writes=[x2k])
                            S.op('dve', lambda e, x2=x2: e.tensor_scalar(out=x2, in0=x2, scalar1=0.044715, scalar2=1.0, op0=ALU.mult, op1=ALU.add), reads=[x2k], writes=[x2k])
                            S.op('dve', lambda e, xs=xs, x2=x2, u_=u_: e.tensor_tensor(u_, x2, xs, ALU.mult), reads=[x2k, xsk], writes=[uk])
                            S.op('act', lambda e, u_=u_: e.activation(u_, u_, AF.Sigmoid, scale=1.5957691216057308), reads=[uk], writes=[uk])
                            S.op('dve', lambda e, j=j, xs=xs, u_=u_: e.tensor_tensor(hidT[:, j, :], xs, u_, ALU.mult), reads=[xsk, uk], writes=[('hidT', j)])
                    for nchunk in range(2):
                        ps, pk = PR()

                        def f(e, ps=ps, nchunk=nchunk):
                            for t in range(2):
                                for c in range(2):
                                    ins = e.matmul(ps[:, t * 64:(t + 1) * 64], hidT[:, t * 2 + c, nchunk * 128:(nchunk + 1) * 128], w2b[:, t, c, :],
                                                   start=(c == 0), stop=(c == 1))
                            return ins
                        S.op('pe', f, reads=[('hidT', j) for j in range(4)] + ['w2b'], writes=[pk])
                        S.op('act', lambda e, ps=ps, nchunk=nchunk: e.copy(vcE[:, nchunk, 0:64], ps[:, 64:128]), reads=[pk], writes=[('vcE', nchunk)])
                        kb_, kbk = WB()
                        rope(ps[:, 0:64].rearrange("p (h c) -> p h c", c=64), pk,
                             kb_[:, 0:64].rearrange("p (h c) -> p h c", c=64), kbk, 1,
                             ct['rccc'][:, nchunk, :], ct['rssc'][:, nchunk, :], ['c_rccc', 'c_rssc'])
                        S.op('dve', lambda e, kb_=kb_: e.tensor_copy(kb_[:, 64:128], kb_[:, 0:64]), reads=[kbk], writes=[kbk])
                        pt, ptk = PT()
                        S.op('pe', lambda e, pt=pt, kb_=kb_: e.transpose(pt[:, 0, :], kb_[:, 0:128], ident[:]),
                             reads=[kbk, 'c_ident'], writes=[ptk])
                        S.op('dve', lambda e, pt=pt, nchunk=nchunk: e.tensor_copy(kcT[:, nchunk * 128:(nchunk + 1) * 128], pt[:, 0, :]),
                             reads=[ptk], writes=[('kcT', nchunk)])
                    for qg in range(nq_groups):
                        run_pipeline(qs_iters(wq_, wqk, qg, qT2c, 'qT2c', zt, 'ztc', do_rope=True), 4)
                        for i in range(4):
                            m = qg * 4 + i
                            qv = None
                            acc, acck = pA[0], 'pA0'
                            nch = 1 if m < 16 else 2
                            for c in range(nch):
                                ps, pk = PR()
                                def fc(e, ps=ps, c=c, i=i):
                                    for p in range(2):
                                        ins = e.matmul(ps[:, p * 256:(p + 1) * 256], kcT[:, c * 128:(c + 1) * 128],
                                                       qT2c[:, p, i, :, :].rearrange("p a b -> p (a b)"), start=True, stop=True)
                                    return ins
                                S.op('pe', fc, reads=[('kcT', c), ('qT2c', i, 0), ('qT2c', i, 1)], writes=[pk])
                                pb, pbk = WB()
                                S.op('act', lambda e, pb=pb, ps=ps: e.activation(pb[:], ps[:], AF.Exp, scale=SCALE), reads=[pk], writes=[pbk])
                                u = m - 16 * c
                                if u < 17:
                                    S.op('dve', lambda e, pb=pb, u=u: e.tensor_tensor(
                                        pb[:].rearrange("p (h c) -> p h c", c=128), pb[:].rearrange("p (h c) -> p h c", c=128),
                                        ct['cv'][:, u, :].unsqueeze(1).to_broadcast([128, 4, 128]), ALU.mult),
                                        reads=[pbk, 'c_cv'], writes=[pbk])
                                av(acc, acck, pb, pbk, lambda h, c=c: vcE[:, c, :], [('vcE', c), ('vcE', 'ov')], c == 0, c == nch - 1, 128)
                            accv = acc[:].rearrange("p (h c) -> p h c", c=128)
                            S.op('dve', lambda e, accv=accv: e.tensor_reduce(out=cf[:, 0:4], in_=accv[:, :, 64:128], axis=AX.X, op=ALU.add), reads=[acck, ('vcE', 'ov')], writes=[('cf', 0)])
                            S.op('dve', lambda e: e.tensor_scalar(out=cf[:, 0:4], in0=cf[:, 0:4], scalar1=1e-30, scalar2=None, op0=ALU.max), reads=[('cf', 0)], writes=[('cf', 0)])
                            S.op('dve', lambda e: e.reciprocal(cf[:, 4:8], cf[:, 0:4]), reads=[('cf', 0)], writes=[('cf', 1)])
                            for h in range(4):
                                if h == 0:
                                    S.op('dve', lambda e, accv=accv: e.tensor_scalar(out=impt[:], in0=accv[:, 0, 64:128], scalar1=cf[:, 4:5], scalar2=None, op0=ALU.mult),
                                         reads=[acck, ('cf', 1)], writes=['impt'])
                                else:
                                    S.op('dve', lambda e, accv=accv, h=h: e.scalar_tensor_tensor(out=impt[:], in0=accv[:, h, 64:128], scalar=cf[:, 4 + h:5 + h], in1=impt[:], op0=ALU.mult, op1=ALU.add),
                                         reads=[acck, ('cf', 1), 'impt'], writes=['impt'])
                            gv = gsig[:, m, :].rearrange("p (h b) -> p h b", b=3)
                            S.op('dve', lambda e, gv=gv: e.tensor_tensor(cf[:, 8:12], cf[:, 4:8], gv[:, :, 0], ALU.mult), reads=[('cf', 1), ('gsig', m)], writes=[('cf', 2)])
                            ocv = ocacc[:].rearrange("p (h c) -> p h c", c=64)
                            S.op('dve', lambda e, accv=accv, ocv=ocv: e.tensor_tensor(ocv, accv[:, :, 0:64], cf[:, 8:12].unsqueeze(2).to_broadcast([128, 4, 64]), ALU.mult),
                                 reads=[acck, ('cf', 2)], writes=['ocacc'])
                            vsl = ct['vt'][:, 64 - 2 * m:128 - 2 * m]
                            if m <= 7:
                                S.op('dve', lambda e, vsl=vsl: e.tensor_copy(selm[:], vsl), reads=['c_vt'], writes=['selm'])
                            else:
                                t1s = ct['t1'][:, 64 - 2 * m:128 - 2 * m]
                                t2s = ct['t2'][:, 64 - 2 * m:128 - 2 * m]
                                S.op('dve', lambda e, t1s=t1s: e.tensor_tensor(imps[:], impt[:], t1s, ALU.mult), reads=['impt', 'c_t1'], writes=['imps'])
                                S.op('dve', lambda e, t2s=t2s: e.tensor_tensor(imps[:], imps[:], t2s, ALU.add), reads=['imps', 'c_t2'], writes=['imps'])
                                S.op('dve', lambda e: e.memset(imps[:, 0:1], BIG), reads=['imps'], writes=['imps'])
                                S.op('dve', lambda e: e.max(out=mx8[:, 0:8], in_=imps[:]), reads=['imps'], writes=[('mx8', 0)])
                                S.op('dve', lambda e: e.match_replace(out=impt[:], in_to_replace=mx8[:, 0:8], in_values=imps[:], imm_value=-3e38),
                                     reads=[('mx8', 0), 'imps'], writes=['impt'])
                                S.op('dve', lambda e: e.max(out=mx8[:, 8:16], in_=impt[:]), reads=['impt'], writes=[('mx8', 1)])
                                S.op('dve', lambda e: e.tensor_scalar(out=imps[:], in0=imps[:], scalar1=mx8[:, 15:16], scalar2=None, op0=ALU.is_ge),
                                     reads=['imps', ('mx8', 1)], writes=['imps'])
                                S.op('dve', lambda e, vsl=vsl: e.tensor_tensor(selm[:], imps[:], vsl, ALU.mult), reads=['imps', 'c_vt'], writes=['selm'])
                            def c_fin(acc, acck, gcol, m=m, gv=gv):
                                accv = acc[:, 0:260].rearrange("p (h c) -> p h c", c=65)
                                S.op('dve', lambda e: e.reciprocal(cf[:, 12:16], accv[:, :, 64]), reads=[acck], writes=[('cf', 3)])
                                S.op('dve', lambda e: e.tensor_tensor(cf[:, 12:16], cf[:, 12:16], gv[:, :, gcol], ALU.mult), reads=[('cf', 3), ('gsig', m)], writes=[('cf', 3)])
                                tmpo, tmpk = WF()
                                S.op('dve', lambda e: e.tensor_tensor(tmpo[:, 0:256].rearrange("p (h c) -> p h c", c=64), accv[:, :, 0:64],
                                                                      cf[:, 12:16].unsqueeze(2).to_broadcast([128, 4, 64]), ALU.mult),
                                     reads=[acck, ('cf', 3)], writes=[tmpk])
                                S.op('pool', lambda e: e.tensor_tensor(ocacc[:], ocacc[:], tmpo[:, 0:256], ALU.add), reads=['ocacc', tmpk], writes=['ocacc'])

                            def c_iter(kind, kb, first, last, m=m, i=i, qv=qv, c_fin=c_fin):
                                c = {}
                                sel = (kind == 'sel')
                                acc, acck = (pA[1], 'pA1') if sel else (pA[0], 'pA0')
                                kT_, kTk = (ksT, 'ksT') if sel else (kwT, 'kwT')
                                vA_, vAk = (vsa, 'vsa') if sel else (vwa, 'vwa')
                                d = m - kb
                                bias = None
                                if (sel and kb == m) or ((not sel) and d == 0):
                                    bias = 'ntri4'
                                elif (not sel) and d == 4:
                                    bias = 'nw44'

                                def s0():
                                    ps, pk = PR()
                                    c['ps'], c['pk'] = ps, pk

                                    def f(e):
                                        for p in range(2):
                                            ins = e.matmul(ps[:, p * 256:(p + 1) * 256], kT_[:, kb * 128:(kb + 1) * 128],
                                                           qT2c[:, p, i, :, :].rearrange("p a b -> p (a b)"), start=(p == 0), stop=(p == 1 and not bias),
                                                           skip_group_check=True)
                                        if bias:
                                            ins = e.matmul(ps[:], ident[:], ct[bias][:], start=False, stop=True, skip_group_check=True)
                                        return ins
                                    S.op('pe', f, reads=[(kTk, kb), ('qT2c', i, 0), ('qT2c', i, 1), 'c_ident'] + (['c_' + bias] if bias else []), writes=[pk])
                                    if sel:
                                        mp, mpk = PR()
                                        c['mp'], c['mpk'] = mp, mpk

                                        def fm(e):
                                            e.matmul(mp[0:64, 0:128], selm[:, 2 * kb:2 * kb + 1].to_broadcast([128, 64]), ident[:], start=True, stop=True)
                                            return e.matmul(mp[64:128, 0:128], selm[:, 2 * kb + 1:2 * kb + 2].to_broadcast([128, 64]), ident[:], start=True, stop=True)
                                        S.op('pe', fm, reads=['selm', 'c_ident'], writes=[mpk])

                                def s1():
                                    c['pb'], c['pbk'] = WB()
                                    pb, pbk = c['pb'], c['pbk']
                                    S.op('act', lambda e: e.activation(pb[:], c['ps'][:], AF.Exp, scale=SCALE), reads=[c['pk']], writes=[pbk])
                                    if sel:
                                        S.op('dve', lambda e: e.tensor_tensor(
                                            pb[:].rearrange("p (h c) -> p h c", c=128), pb[:].rearrange("p (h c) -> p h c", c=128),
                                            c['mp'][:, 0:128].unsqueeze(1).to_broadcast([128, 4, 128]), ALU.mult),
                                            reads=[pbk, c['mpk']], writes=[pbk])

                                def s2():
                                    av(acc, acck, c['pb'], c['pbk'], lambda h: vA_[:, kb, :], (vAk, kb), first, last, 65)
                                    if last:
                                        c_fin(acc, acck, 1 if sel else 2)
                                        if not sel:
                                            store_mixed(m, 512, lambda: ocacc[:], ['ocacc'], zt, 'ztc', i)
                                return [s0, s1, s2]
                            its = [c_iter('sel', kb, kb == 0, kb == m) for kb in range(0, m + 1)]
                            dmax = min(4, m)
                            its += [c_iter('win', m - d, d == 0, d == dmax) for d in range(0, dmax + 1)]
                            run_pipeline(its, 3)
                    S.barrier(drop_prefixes=('kcrT', 'vcrT', 'ksT', 'kwT', 'vsa', 'vwa', 'gsig', 'kcT', 'vcE', 'qT2c', 'ztc', 'w1b', 'w2', 'b1t', 'pos',
                                             'biasv', 'hidT', 'gx', 'ocacc', 'imp', 'selm', 'selb', 'mx8', 'cf'))

            if 'D' in mixers:
                with ExitStack() as mds:
                    kT = T("kdT", [128, 2, S_LEN], BF16, mds)
                    vaug = T("vdaug", [128, NSLOT, 4, 65], BF16, mds)
                    qT2s = [T(f"qT2d{b_}", [128, 2, 4, 2, 128], BF16, mds) for b_ in range(2)]
                    zts = [T(f"ztd{b_}", [128, 4, 256], BF16, mds) for b_ in range(2)]
                    kmf = T("kmf", [128, 2, 16], F32, mds)
                    kmb = T("kmb", [128, 2, 16], BF16, mds)
                    gm = T("gm", [128, 4, 16], F32, mds)
                    gmx = T("gmx", [128, 4, 8], F32, mds)
                    isel = T("isel", [128, 4, 16], BF16, mds)
                    for b_ in range(2):
                        S.op('pool', lambda e, b_=b_: e.memset(qT2s[b_][:].rearrange("p a b c d -> p (a b c d)"), 0.0), writes=[(f'qT2_{b_}', i, j) for i in range(4) for j in range(2)])
                    S.op('pool', lambda e: e.memset(vaug[:].rearrange("p a b c -> p (a b c)"), 1.0), writes=[('vaug', s_) for s_ in range(NSLOT)])
                    wk_, wkk = load_wgroup(l, 6)
                    wq_, wqk = load_wgroup(l, 7)
                    wload(8 * l + 8)
                    def rk_iter(slot, kT_, kTkey, vaug_, wk__, wkk__):
                        c = {}

                        def s0():
                            c['ps'], c['pk'] = proj_slot(slot, wk__, wkk__)

                        def s1():
                            ps, pk = c['ps'], c['pk']
                            S.op('act', lambda e: e.copy(vaug_[:, slot, :, 0:64], ps[:, 256:512].rearrange("p (h c) -> p h c", c=64)),
                                 reads=[pk], writes=[('vaug', slot)])
                            c['kb'], c['kbk'] = WB()
                            rope(ps[:, 0:256].rearrange("p (h c) -> p h c", c=64), pk,
                                 c['kb'][:, 0:256].rearrange("p (h c) -> p h c", c=64), c['kbk'], 4,
                                 ct['rcc'][:, slot, :], ct['rss'][:, slot, :], ['c_rcc', 'c_rss'])

                        def s2():
                            c['pt'], c['ptk'] = qk_transpose(c['kb'], [c['kbk']], None, None, 2)

                        def s3():
                            S.op('dve', lambda e: e.tensor_copy(kT_[:, :, slot * 128:(slot + 1) * 128], c['pt'][:, 0:2, :]),
                                 reads=[c['ptk']], writes=[(kTkey, slot)])
                        return [s0, s1, s2, s3]
                    run_pipeline([rk_iter(slot, kT, 'kT', vaug, wk_, wkk) for slot in range(NSLOT)], 4)
                    wload(8 * l + 9)
                    allk = [('kT', s_) for s_ in range(NSLOT)]
                    for p in range(2):
                        S.op('dve', lambda e, p=p: e.tensor_reduce(out=kmf[:, p, :], in_=kT[:, p, :].rearrange("p (n r) -> p n r", r=256), axis=AX.X, op=ALU.add),
                             reads=allk, writes=[('kmf', p)])
                    S.op('dve', lambda e: e.tensor_scalar(out=kmb[:], in0=kmf[:], scalar1=1.0 / 256, scalar2=None, op0=ALU.mult),
                         reads=[('kmf', 0), ('kmf', 1)], writes=['kmb'])
                    isel2 = [isel, T("isel2", [128, 4, 16], BF16, mds)]

                    def d_prep(m, i, own, qT2, qT2k):
                        isl = isel2[m % 2]
                        islk = f"isel{m % 2}"
                        gp_, gpk = PR()

                        def fg(e):
                            for h in range(4):
                                ins = e.matmul(gp_[:, h * 16:(h + 1) * 16], qT2[:, h // 2, i, h % 2, :], kmb[:, h // 2, :], start=True, stop=True)
                            return ins
                        S.op('pe', fg, reads=[(qT2k, i, 0), (qT2k, i, 1), 'kmb'], writes=[gpk])
                        S.op('dve', lambda e: e.memset(gm[:].rearrange("p a b -> p (a b)"), NEGB), writes=['gm'])
                        S.op('dve', lambda e: e.tensor_copy(gm[:, :, 0:own], gp_[:, 0:64].rearrange("p (h n) -> p h n", n=16)[:, :, 0:own]),
                             reads=[gpk, 'gm'], writes=['gm'])
                        for h in range(4):
                            S.op('dve', lambda e, h=h: e.max(out=gmx[:, h, :], in_=gm[:, h, :]), reads=['gm'], writes=[('gmx', h)])
                        for h in range(4):
                            S.op('dve', lambda e, h=h: e.tensor_scalar(out=gm[:, h, :], in0=gm[:, h, :], scalar1=gmx[:, h, 2:3], scalar2=None, op0=ALU.is_ge),
                                 reads=['gm', ('gmx', h)], writes=['gm'])
                        S.op('dve', lambda e: e.tensor_scalar(out=isl[:].rearrange("p a b -> p (a b)"), in0=gm[:].rearrange("p a b -> p (a b)"), scalar1=-1.0, scalar2=240.0, op0=ALU.add, op1=ALU.mult),
                             reads=['gm'], writes=[islk])

                    def d_iter(m, i, kb, mt, first, last, own, qT2, qT2k, zt, ztk):
                        c = {}
                        acc, acck = pA[m % 2], f"pA{m % 2}"
                        isl = isel2[m % 2]
                        islk = f"isel{m % 2}"

                        def s0():
                            if first and own > 3:
                                d_prep(m, i, own, qT2, qT2k)
                            ps, pk = PR()
                            c['ps'], c['pk'] = ps, pk
                            n_ = kb // 2

                            def f(e):
                                for p in range(2):
                                    ins = e.matmul(ps[:, p * 256:(p + 1) * 256], kT[:, p, kb * 128:(kb + 1) * 128],
                                                   qT2[:, p, i, :, :].rearrange("p a b -> p (a b)"), start=(p == 0), stop=(mt == 'none' and p == 1),
                                                   skip_group_check=True)
                                if mt == 'sel':
                                    for h in range(4):
                                        ins = e.matmul(ps[:, h * 128:(h + 1) * 128], isl[:, h, n_:n_ + 1].to_broadcast([128, 128]), ident[:],
                                                       start=False, stop=(h == 3), skip_group_check=True)
                                elif mt == 'diag':
                                    ins = e.matmul(ps[:], ident[:], ct['ntri4'][:], start=False, stop=True, skip_group_check=True)
                                return ins
                            S.op('pe', f, reads=[('kT', kb), (qT2k, i, 0), (qT2k, i, 1), 'c_ident', 'c_ntri4'] + ([islk] if mt == 'sel' else []), writes=[pk])

                        def s1():
                            c['pb'], c['pbk'] = WB()
                            S.op('act', lambda e: e.activation(c['pb'][:], c['ps'][:], AF.Exp, scale=SCALE), reads=[c['pk']], writes=[c['pbk']])

                        def s2():
                            av(acc, acck, c['pb'], c['pbk'], lambda h: vaug[:, kb, h, :], ('vaug', kb), first, last, 65)
                            if last:
                                o, ok = normalize(acc, acck)
                                store_mixed(m, 768, lambda: o[:, 0:256], [ok], zt, ztk, i)
                        return [s0, s1, s2]
                    run_pipeline(qs_iters(wq_, wqk, 0, qT2s[0], 'qT2_0', zts[0], 'ztd_0', do_rope=True), 4)
                    for qg in range(nq_groups):
                        b_ = qg % 2
                        its = []
                        for i in range(4):
                            m = qg * 4 + i
                            own = m // 2
                            steps = [(kb, 'sel' if own > 3 else 'none') for kb in range(0, 2 * own)]
                            if m % 2 == 1:
                                steps.append((m - 1, 'none'))
                            steps.append((m, 'diag'))
                            for si_, (kb, mt) in enumerate(steps):
                                its.append(d_iter(m, i, kb, mt, si_ == 0, si_ == len(steps) - 1, own, qT2s[b_], f'qT2_{b_}', zts[b_], f'ztd_{b_}'))
                        extra = qs_iters(wq_, wqk, qg + 1, qT2s[1 - b_], f'qT2_{1 - b_}', zts[1 - b_], f'ztd_{1 - b_}', do_rope=True) if qg + 1 < nq_groups else []
                        run_pipeline(interleave(its, extra), 4)
                    S.barrier(drop_prefixes=('kT', 'vaug', 'qT2', 'ztd', 'kmf', 'kmb', 'gm', 'isel'))

            with ExitStack() as ps3:
                if dbg and (mixers != 'ABCD' or nq_groups != 8):
                    break
                wo = T("wo", [128, 8, D], BF16, ps3)
                mx = [T(f"mxl{i}", [128, D], BF16, ps3) for i in range(2)]
                mT = [T(f"mTl{i}", [128, 8, 128], BF16, ps3) for i in range(2)]
                xr = [T(f"xr{i}", [128, D], F32, ps3) for i in range(2)]
                ot = [T(f"ot{i}", [128, D], F32, ps3) for i in range(2)]
                junk3 = T("junk3", [128, D], BF16, ps3)
                st3 = [T(f"st3_{i}", [128, 8], F32, ps3) for i in range(2)]
                wsrc = wout_d[l].rearrange("(k p) c -> p k c", p=128)
                for q4 in range(4):
                    S.dma('pool', wo[:, q4 * 2:(q4 + 1) * 2, :], wsrc[:, q4 * 2:(q4 + 1) * 2, :], f"wol{q4}", writes=[('wo', q4)])
                def p3_iter(slot):
                    i2 = slot % 2
                    tsl = slice(slot * 128, (slot + 1) * 128)
                    stt, stk = st3[i2], f"st3_{i2}"
                    if slot % 2:
                        (y0, y0k), (y1, y1k) = (pR[0], 'pR0'), (pR[1], 'pR1')
                    else:
                        (y0, y0k), (y1, y1k) = (pA[0], 'pA0'), (pA[1], 'pA1')

                    def s0():
                        S.dma('sp', mx[i2][:], mixed_d[tsl, :], f"mxl{i2}", reads=[('mixed', slot, c0) for c0 in (0, 256, 512, 768)], writes=[f"mxl{i2}"])

                    def s1():
                        pt, ptk = PT()

                        def f(e):
                            for k in range(8):
                                ins = e.transpose(pt[:, k, :], mx[i2][:, k * 128:(k + 1) * 128], ident[:])
                            return ins
                        S.op('pe', f, reads=[f"mxl{i2}", 'c_ident'], writes=[ptk])
                        S.op('act', lambda e: e.copy(mT[i2][:], pt[:]), reads=[ptk], writes=[f"mTl{i2}"])

                    def s2():
                        S.dma('sp', xr[i2][:], xin[tsl, :], f"xr{i2}", reads=[(xin_key, slot)], writes=[f"xr{i2}"])

                        def fy(e):
                            for hh, y in enumerate((y0, y1)):
                                for k in range(8):
                                    ins = e.matmul(y[:], mT[i2][:, k, :], wo[:, k, hh * 512:(hh + 1) * 512], start=(k == 0), stop=(k == 7))
                            return ins
                        S.op('pe', fy, reads=[f"mTl{i2}"] + [('wo', q4) for q4 in range(4)], writes=[y0k, y1k])

                    def s3():
                        S.op('act', lambda e: e.activation(junk3[:, 0:512], y0[:], AF.Square, accum_out=stt[:, 0:1]), reads=[y0k], writes=[('junk3', 0), (stk, 0)])
                        S.op('act', lambda e: e.activation(junk3[:, 512:1024], y1[:], AF.Square, accum_out=stt[:, 1:2]), reads=[y1k], writes=[('junk3', 1), (stk, 1)])
                        S.op('dve', lambda e: e.tensor_tensor(stt[:, 2:3], stt[:, 0:1], stt[:, 1:2], ALU.add), reads=[(stk, 0), (stk, 1)], writes=[(stk, 2)])
                        S.op('dve', lambda e: e.tensor_scalar(out=stt[:, 5:6], in0=stt[:, 2:3], scalar1=1.0 / D, scalar2=EPS, op0=ALU.mult, op1=ALU.add), reads=[(stk, 2)], writes=[(stk, 5)])
                        S.op('act', lambda e: e.activation(stt[:, 3:4], stt[:, 5:6], AF.Sqrt), reads=[(stk, 5)], writes=[(stk, 3)])
                        S.op('dve', lambda e: e.reciprocal(stt[:, 4:5], stt[:, 3:4]), reads=[(stk, 3)], writes=[(stk, 4)])
                        for hh, (y, yk) in enumerate(((y0, y0k), (y1, y1k))):
                            S.op('dve', lambda e, y=y, hh=hh: e.scalar_tensor_tensor(out=ot[i2][:, hh * 512:(hh + 1) * 512], in0=y[:], scalar=stt[:, 4:5],
                                                                                   in1=gpt[:, hh * 512:(hh + 1) * 512], op0=ALU.mult, op1=ALU.mult),
                                 reads=[yk, (stk, 4), 'gpt'], writes=[(f"ot{i2}", hh)])
                        S.op('pool', lambda e: e.tensor_tensor(ot[i2][:], ot[i2][:], xr[i2][:], ALU.add),
                             reads=[(f"ot{i2}", 0), (f"ot{i2}", 1), f"xr{i2}"], writes=[(f"ot{i2}", 0), (f"ot{i2}", 1)])
                        S.dma('pool', xout[tsl, :], ot[i2][:], f'outst{i2}', reads=[(f"ot{i2}", 0), (f"ot{i2}", 1)], writes=[(xout_key, slot)])
                    return [s0, s1, s2, s3]
                run_pipeline([p3_iter(slot) for slot in range(NSLOT)], 4)
                S.barrier(drop_prefixes=('wo', 'mxl', 'mTl', 'xr', 'ot', 'junk3', 'st3_'))
        S.finish()
        build.stats = dict(nins=S.nins, nwait=S.nwait, nsem=S.nsem)
    return nc


def _prep_inputs(inputs):
    f = np.float32
    x = np.asarray(inputs['x'], f)
    c = np.asarray(inputs['c'], f)
    w_in = np.asarray(inputs['w_in'], f)
    shared = {}
    shared['npre'] = np.ascontiguousarray(np.broadcast_to(np.asarray(inputs['norm_pre'], f)[:, None, :], (L, 128, D)))
    shared['npost'] = np.ascontiguousarray(np.broadcast_to(np.asarray(inputs['norm_post'], f)[:, None, :], (L, 128, D)))
    shared['bmod'] = np.ascontiguousarray(np.broadcast_to(np.asarray(inputs['b_mod'], f)[:, None, :], (L, 128, 3 * D)))
    shared['wmod'] = np.ascontiguousarray(np.asarray(inputs['w_mod'], f))
    shared['wg'] = np.ascontiguousarray(np.stack([_w_groups(w_in[l]) for l in range(L)], 0))
    shared['wout'] = np.ascontiguousarray(np.asarray(inputs['w_out'], f))
    shared['w1'] = np.ascontiguousarray(np.asarray(inputs['cmp_w1'], f))
    shared['w2'] = np.ascontiguousarray(np.asarray(inputs['cmp_w2'], f))
    b1 = np.asarray(inputs['cmp_b1'], f)
    shared['b1'] = np.ascontiguousarray(b1.reshape(L, 2, 2, 128).transpose(0, 3, 1, 2).reshape(L, 128, 4))
    pos = np.asarray(inputs['cmp_pos'], f)
    shared['posT'] = np.ascontiguousarray(pos.transpose(0, 3, 1, 2))
    for k_, v_ in _consts().items():
        shared['c_' + k_] = v_
    maps = []
    for b in range(x.shape[0]):
        m = dict(shared)
        m['x'] = np.ascontiguousarray(x[b])
        m['cT'] = np.ascontiguousarray(c[b].reshape(8, 128).T)
        maps.append(m)
    return maps


_NC_CACHE = {}


def kernel(**inputs):
    maps = _prep_inputs(inputs)
    if 'nc' not in _NC_CACHE:
        _NC_CACHE['nc'] = build()
    nc = _NC_CACHE['nc']
    res = run_bass_kernel_spmd(nc, maps, core_ids=list(range(len(maps))))
    out = np.stack([np.asarray(r['out'], np.float32) for r in res.results], 0)
    return out
```
